# Optimizing a Trainium2 kernel written in Bass

```python
import math
import jax
import jax.numpy as jnp
from jax import lax
import numpy as np

D_MODEL = 1024
BATCH = 8
SEQ = 2048
DEPTH = 4

N_MIXERS = 4
PLE_DIM = 256
ALPHA = (2 * DEPTH) ** 0.25
BETA = (8 * DEPTH) ** -0.25
LN_EPS = 1e-5
RMS_EPS = 1e-6

FOX_HEADS = 16
FOX_HEAD_DIM = D_MODEL // FOX_HEADS
Q_BLOCK = 128

LRU_WIDTH = 1280
LRU_BLOCKS = 10
LRU_BLOCK_DIM = LRU_WIDTH // LRU_BLOCKS
LRU_CONV = 4
LRU_C = 8.0

CONF_KERNEL = 31

GDN_HEADS = 8
GDN_HEAD_DIM = D_MODEL // GDN_HEADS
GDN_CONV = 4
GDN_CHUNK = 64

D_FF = 2816
N_EXPERTS = 8
TOP_K = 2
D_EXPERT = 3584
MOE_BLOCK = 256

N_FOX = len(range(0, DEPTH, N_MIXERS))
N_LRU = len(range(1, DEPTH, N_MIXERS))
N_CONV = len(range(2, DEPTH, N_MIXERS))
N_GDN = len(range(3, DEPTH, N_MIXERS))
N_DENSE = len(range(0, DEPTH, 2))
N_MOE = len(range(1, DEPTH, 2))

kernel_name = "hybrid_fox_rglru_conformer_gdn_moe_trunk"


def _layer_norm(x, g, b):
    xf = x.astype(jnp.float32)
    mu = jnp.mean(xf, axis=-1, keepdims=True)
    var = jnp.mean(jnp.square(xf - mu), axis=-1, keepdims=True)
    return ((xf - mu) * lax.rsqrt(var + LN_EPS) * g + b).astype(x.dtype)


def _rms_norm(x, g):
    xf = x.astype(jnp.float32)
    return xf * lax.rsqrt(jnp.mean(jnp.square(xf), axis=-1, keepdims=True) + RMS_EPS) * g


def _l2_normalize(x):
    return x * lax.rsqrt(jnp.sum(jnp.square(x), axis=-1, keepdims=True) + 1e-6)


def _causal_depthwise_conv(x, w):
    k = w.shape[0]
    xp = jnp.pad(x, ((0, 0), (k - 1, 0), (0, 0)))
    return lax.conv_general_dilated(xp, w[:, None, :].astype(x.dtype), (1,), "VALID",
                                    dimension_numbers=("NWC", "WIO", "NWC"),
                                    feature_group_count=x.shape[-1])


def _swiglu(h, w_gu, w_down):
    gate, up = jnp.split(h @ w_gu, 2, axis=-1)
    return (jax.nn.silu(gate) * up) @ w_down


def _fox_attention(x, w_in, b_f, w_out):
    B, S, _ = x.shape
    H, Dh = FOX_HEADS, FOX_HEAD_DIM
    q, k, v, f_logit = jnp.split(x @ w_in, [D_MODEL, 2 * D_MODEL, 3 * D_MODEL], axis=-1)
    to_heads = lambda t: t.reshape(B, S, H, Dh).transpose(0, 2, 1, 3)
    q, k, v = to_heads(q), to_heads(k), to_heads(v)
    log_f = jax.nn.log_sigmoid(f_logit.astype(jnp.float32) + b_f.astype(jnp.float32))
    c = jnp.cumsum(log_f, axis=1).transpose(0, 2, 1)
    scale = Dh ** -0.5
    outs = []
    for start in range(0, S, Q_BLOCK):
        end = start + Q_BLOCK
        s = jnp.einsum("bhqd,bhkd->bhqk", q[:, :, start:end], k[:, :, :end]).astype(jnp.float32) * scale
        s = s + c[:, :, start:end, None] - c[:, :, None, :end]
        causal = (start + jnp.arange(Q_BLOCK))[:, None] >= jnp.arange(end)[None, :]
        s = jnp.where(causal, s, -jnp.inf)
        pr = jax.nn.softmax(s, axis=-1).astype(v.dtype)
        outs.append(jnp.einsum("bhqk,bhkd->bhqd", pr, v[:, :, :end]))
    o = jnp.concatenate(outs, axis=2).transpose(0, 2, 1, 3).reshape(B, S, H * Dh)
    return o @ w_out


def _linear_recurrence_combine(left, right):
    a_l, b_l = left
    a_r, b_r = right
    return a_l * a_r, a_r * b_l + b_r


def _rglru_block(x, w_in, conv_w, conv_b, w_a, b_a, w_x, b_x, lam, w_out):
    B, S, _ = x.shape
    gate_in, rec_in = jnp.split(x @ w_in, 2, axis=-1)
    u = _causal_depthwise_conv(rec_in, conv_w) + conv_b
    ub = u.reshape(B, S, LRU_BLOCKS, LRU_BLOCK_DIM)
    r = jax.nn.sigmoid(jnp.einsum("bsni,nij->bsnj", ub, w_a).reshape(B, S, LRU_WIDTH) + b_a)
    i = jax.nn.sigmoid(jnp.einsum("bsni,nij->bsnj", ub, w_x).reshape(B, S, LRU_WIDTH) + b_x)
    log_a = -LRU_C * r.astype(jnp.float32) * jax.nn.softplus(-lam.astype(jnp.float32))
    a = jnp.exp(log_a)
    b = jnp.sqrt(-jnp.expm1(2.0 * log_a)) * (i * u).astype(jnp.float32)
    _, h = lax.associative_scan(_linear_recurrence_combine, (a, b), axis=1)
    y = jax.nn.gelu(gate_in) * h.astype(x.dtype)
    return y @ w_out


def _conformer_conv(x, w_in, b_in, dw_w, dw_b, ln_g, ln_b, w_out, b_out):
    val, gate = jnp.split(x @ w_in + b_in, 2, axis=-1)
    h = val * jax.nn.sigmoid(gate)
    h = _causal_depthwise_conv(h, dw_w) + dw_b
    h = jax.nn.silu(_layer_norm(h, ln_g, ln_b))
    return h @ w_out + b_out


def _chunk_gated_delta_rule(q, k, v, g, beta):
    B, H, S, Dk = q.shape
    Dv = v.shape[-1]
    C = GDN_CHUNK
    N = S // C
    rs = lambda t: t.reshape(B, H, N, C, *t.shape[3:])
    q, k, v, g, beta = rs(q), rs(k), rs(v), rs(g), rs(beta)
    g = jnp.cumsum(g, axis=-1)
    k_beta = k * beta[..., None]
    v_beta = v * beta[..., None]
    tril = jnp.tril(jnp.ones((C, C), dtype=bool))
    strict = jnp.tril(jnp.ones((C, C), dtype=bool), -1)
    diff = g[..., :, None] - g[..., None, :]
    decay = jnp.where(tril, jnp.exp(jnp.where(tril, diff, 0.0)), 0.0)
    lower = jnp.where(strict, jnp.einsum("bhncd,bhnsd->bhncs", k_beta, k) * decay, 0.0)
    eye = jnp.eye(C, dtype=jnp.float32)
    rhs = jnp.concatenate([v_beta, k_beta * jnp.exp(g)[..., None]], axis=-1)
    solved = lax.linalg.triangular_solve(eye + lower, rhs, left_side=True, lower=True, unit_diagonal=True)
    u, w = jnp.split(solved, [Dv], axis=-1)
    attn_intra = jnp.where(tril, jnp.einsum("bhncd,bhnsd->bhncs", q, k) * decay, 0.0)
    g_last = g[..., -1]
    k_to_end = k * jnp.exp(g_last[..., None] - g)[..., None]

    def step(state, inp):
        q_c, u_c, w_c, attn_c, g_c, gl_c, kend_c = inp
        v_new = u_c - jnp.einsum("bhcd,bhdv->bhcv", w_c, state)
        o_c = (jnp.einsum("bhcd,bhdv->bhcv", q_c * jnp.exp(g_c)[..., None], state)
               + jnp.einsum("bhcs,bhsv->bhcv", attn_c, v_new))
        state = state * jnp.exp(gl_c)[..., None, None] + jnp.einsum("bhcd,bhcv->bhdv", kend_c, v_new)
        return state, o_c

    xs = tuple(jnp.moveaxis(t, 2, 0) for t in (q, u, w, attn_intra, g, g_last, k_to_end))
    state0 = jnp.zeros((B, H, Dk, Dv), jnp.float32)
    _, o = lax.scan(step, state0, xs)
    return jnp.moveaxis(o, 0, 2).reshape(B, H, S, Dv)


def _gated_deltanet(x, w_in, conv_w, a_log, dt_bias, norm_g, w_out):
    B, S, _ = x.shape
    H, Dh = GDN_HEADS, GDN_HEAD_DIM
    HD = H * Dh
    qkv, z, beta_logit, a_logit = jnp.split(x @ w_in, [3 * HD, 4 * HD, 4 * HD + H], axis=-1)
    qkv = jax.nn.silu(_causal_depthwise_conv(qkv, conv_w))
    to_heads = lambda t: t.reshape(B, S, H, Dh).transpose(0, 2, 1, 3).astype(jnp.float32)
    q, k, v = (to_heads(t) for t in jnp.split(qkv, 3, axis=-1))
    q = _l2_normalize(q) * (Dh ** -0.5)
    k = _l2_normalize(k)
    beta = jax.nn.sigmoid(beta_logit.astype(jnp.float32)).transpose(0, 2, 1)
    g = (-jnp.exp(a_log.astype(jnp.float32))
         * jax.nn.softplus(a_logit.astype(jnp.float32) + dt_bias.astype(jnp.float32))).transpose(0, 2, 1)
    o = _chunk_gated_delta_rule(q, k, v, g, beta).transpose(0, 2, 1, 3)
    o = _rms_norm(o, norm_g.astype(jnp.float32)) * jax.nn.silu(z.reshape(B, S, H, Dh).astype(jnp.float32))
    return o.reshape(B, S, HD).astype(x.dtype) @ w_out


def _moe_swiglu(x, w_router, b_router, w_gu, w_down):
    B, S, D = x.shape
    xt = x.reshape(B * S, D)
    T = B * S
    logits = (xt @ w_router).astype(jnp.float32) + b_router.astype(jnp.float32)
    top_logit, top_idx = lax.top_k(logits, TOP_K)
    top_w = jax.nn.softmax(top_logit, axis=-1)
    flat_e = top_idx.reshape(-1)
    flat_tok = jnp.repeat(jnp.arange(T, dtype=jnp.int32), TOP_K)
    flat_w = top_w.reshape(-1)
    order = jnp.argsort(flat_e)
    sorted_e = flat_e[order]
    counts = jnp.bincount(flat_e, length=N_EXPERTS)
    padded = ((counts + MOE_BLOCK - 1) // MOE_BLOCK) * MOE_BLOCK
    pad_end = jnp.cumsum(padded)
    pad_start = pad_end - padded
    start = jnp.cumsum(counts) - counts
    dest = pad_start[sorted_e] + (jnp.arange(T * TOP_K) - start[sorted_e])
    n_rows = (-(-(T * TOP_K) // MOE_BLOCK) + N_EXPERTS) * MOE_BLOCK
    n_blocks = n_rows // MOE_BLOCK
    row_tok = jnp.zeros((n_rows,), jnp.int32).at[dest].set(flat_tok[order])
    row_w = jnp.zeros((n_rows,), jnp.float32).at[dest].set(flat_w[order])
    block_e = jnp.minimum(jnp.searchsorted(pad_end, jnp.arange(n_blocks) * MOE_BLOCK, side="right"),
                          N_EXPERTS - 1)
    xs = xt[row_tok].reshape(n_blocks, MOE_BLOCK, D)

    def expert_block(args):
        xb, e = args
        return _swiglu(xb, w_gu[e], w_down[e])

    y = lax.map(expert_block, (xs, block_e)).reshape(n_rows, D)
    out = jnp.zeros_like(xt).at[row_tok].add(y * row_w[:, None].astype(y.dtype))
    return out.reshape(B, S, D)


def setup_inputs(seed: int = 0) -> dict:
    key = jax.random.key(seed)
    ks = iter(jax.random.split(key, 64))
    f32 = jnp.float32

    def nrm(shape, scale):
        return jax.random.normal(next(ks), shape, f32) * scale

    def unif(shape, lo, hi):
        return jax.random.uniform(next(ks), shape, f32, lo, hi)

    D = D_MODEL
    HD = GDN_HEADS * GDN_HEAD_DIM
    inp = {}
    inp["x"] = nrm((BATCH, SEQ, D), 1.0)
    inp["p"] = nrm((DEPTH, BATCH, SEQ, PLE_DIM), 1.0)
    inp["ln_mix_g"] = 1.0 + nrm((DEPTH, D), 0.05)
    inp["ln_mix_b"] = nrm((DEPTH, D), 0.02)
    inp["ln_ffn_g"] = 1.0 + nrm((DEPTH, D), 0.05)
    inp["ln_ffn_b"] = nrm((DEPTH, D), 0.02)
    inp["ple_w"] = nrm((DEPTH, PLE_DIM, D), PLE_DIM ** -0.5 * BETA)
    inp["ple_gate_w"] = nrm((DEPTH, D, D), D ** -0.5)
    inp["fox_w_in"] = nrm((N_FOX, D, 3 * D + FOX_HEADS), D ** -0.5)
    inp["fox_b_f"] = unif((N_FOX, FOX_HEADS), 2.0, 5.0)
    inp["fox_w_out"] = nrm((N_FOX, D, D), D ** -0.5 * BETA)
    inp["lru_w_in"] = nrm((N_LRU, D, 2 * LRU_WIDTH), D ** -0.5)
    inp["lru_conv_w"] = nrm((N_LRU, LRU_CONV, LRU_WIDTH), LRU_CONV ** -0.5)
    inp["lru_conv_b"] = nrm((N_LRU, LRU_WIDTH), 0.02)
    inp["lru_w_a"] = nrm((N_LRU, LRU_BLOCKS, LRU_BLOCK_DIM, LRU_BLOCK_DIM), LRU_BLOCK_DIM ** -0.5)
    inp["lru_b_a"] = nrm((N_LRU, LRU_WIDTH), 0.02)
    inp["lru_w_x"] = nrm((N_LRU, LRU_BLOCKS, LRU_BLOCK_DIM, LRU_BLOCK_DIM), LRU_BLOCK_DIM ** -0.5)
    inp["lru_b_x"] = nrm((N_LRU, LRU_WIDTH), 0.02)
    a0 = unif((N_LRU, LRU_WIDTH), 0.9, 0.999)
    s0 = a0 ** (1.0 / LRU_C)
    inp["lru_lambda"] = jnp.log(s0) - jnp.log1p(-s0)
    inp["lru_w_out"] = nrm((N_LRU, LRU_WIDTH, D), LRU_WIDTH ** -0.5 * BETA)
    inp["cv_w_in"] = nrm((N_CONV, D, 2 * D), D ** -0.5)
    inp["cv_b_in"] = nrm((N_CONV, 2 * D), 0.02)
    inp["cv_dw_w"] = nrm((N_CONV, CONF_KERNEL, D), CONF_KERNEL ** -0.5)
    inp["cv_dw_b"] = nrm((N_CONV, D), 0.02)
    inp["cv_ln_g"] = 1.0 + nrm((N_CONV, D), 0.05)
    inp["cv_ln_b"] = nrm((N_CONV, D), 0.02)
    inp["cv_w_out"] = nrm((N_CONV, D, D), D ** -0.5 * BETA)
    inp["cv_b_out"] = nrm((N_CONV, D), 0.02)
    inp["gdn_w_in"] = jnp.concatenate([nrm((N_GDN, D, 4 * HD + GDN_HEADS), D ** -0.5),
                                       nrm((N_GDN, D, GDN_HEADS), 0.1 * D ** -0.5)], axis=-1)
    inp["gdn_conv_w"] = nrm((N_GDN, GDN_CONV, 3 * HD), GDN_CONV ** -0.5)
    inp["gdn_a_log"] = jnp.log(unif((N_GDN, GDN_HEADS), 1.0, 16.0))
    dt = jnp.exp(unif((N_GDN, GDN_HEADS), math.log(1e-3), math.log(1e-1)))
    inp["gdn_dt_bias"] = dt + jnp.log(-jnp.expm1(-dt))
    inp["gdn_norm_g"] = 1.0 + nrm((N_GDN, GDN_HEAD_DIM), 0.05)
    inp["gdn_w_out"] = nrm((N_GDN, HD, D), HD ** -0.5 * BETA)
    inp["ffn_w_gu"] = nrm((N_DENSE, D, 2 * D_FF), D ** -0.5)
    inp["ffn_w_down"] = nrm((N_DENSE, D_FF, D), D_FF ** -0.5 * BETA)
    inp["moe_w_router"] = nrm((N_MOE, D, N_EXPERTS), D ** -0.5)
    inp["moe_b_router"] = nrm((N_MOE, N_EXPERTS), 0.01)
    inp["moe_w_gu"] = nrm((N_MOE, N_EXPERTS, D, 2 * D_EXPERT), D ** -0.5)
    inp["moe_w_down"] = nrm((N_MOE, N_EXPERTS, D_EXPERT, D), D_EXPERT ** -0.5 * BETA)
    return inp


def reference(x, p, ln_mix_g, ln_mix_b, ln_ffn_g, ln_ffn_b, ple_w, ple_gate_w,
              fox_w_in, fox_b_f, fox_w_out,
              lru_w_in, lru_conv_w, lru_conv_b, lru_w_a, lru_b_a, lru_w_x, lru_b_x, lru_lambda, lru_w_out,
              cv_w_in, cv_b_in, cv_dw_w, cv_dw_b, cv_ln_g, cv_ln_b, cv_w_out, cv_b_out,
              gdn_w_in, gdn_conv_w, gdn_a_log, gdn_dt_bias, gdn_norm_g, gdn_w_out,
              ffn_w_gu, ffn_w_down,
              moe_w_router, moe_b_router, moe_w_gu, moe_w_down):
    for i in range(DEPTH):
        m = i % N_MIXERS
        j = i // N_MIXERS
        if m == 0:
            mix = _fox_attention(x, fox_w_in[j], fox_b_f[j], fox_w_out[j])
        elif m == 1:
            mix = _rglru_block(x, lru_w_in[j], lru_conv_w[j], lru_conv_b[j], lru_w_a[j], lru_b_a[j],
                               lru_w_x[j], lru_b_x[j], lru_lambda[j], lru_w_out[j])
        elif m == 2:
            mix = _conformer_conv(x, cv_w_in[j], cv_b_in[j], cv_dw_w[j], cv_dw_b[j],
                                  cv_ln_g[j], cv_ln_b[j], cv_w_out[j], cv_b_out[j])
        else:
            mix = _gated_deltanet(x, gdn_w_in[j], gdn_conv_w[j], gdn_a_log[j], gdn_dt_bias[j],
                                  gdn_norm_g[j], gdn_w_out[j])
        x = _layer_norm(ALPHA * x + mix, ln_mix_g[i], ln_mix_b[i])
        if i % 2 == 0:
            ff = _swiglu(x, ffn_w_gu[i // 2], ffn_w_down[i // 2])
        else:
            ff = _moe_swiglu(x, moe_w_router[i // 2], moe_b_router[i // 2], moe_w_gu[i // 2], moe_w_down[i // 2])
        x = _layer_norm(ALPHA * x + ff, ln_ffn_g[i], ln_ffn_b[i])
        gate = jax.nn.sigmoid(x @ ple_gate_w[i])
        x = x + gate * (p[i] @ ple_w[i])
    return x
```

```python
import os
import numpy as np
from contextlib import ExitStack
import concourse.bass as bass
import concourse.mybir as mybir
from concourse.bass_utils import run_bass_kernel_spmd

F32 = mybir.dt.float32
BF16 = mybir.dt.bfloat16
AF = mybir.ActivationFunctionType
ALU = mybir.AluOpType

ENGS = ("tensor", "vector", "scalar", "gpsimd", "sync")
EPOCH = 16000
N_DMA_SEMS = 24
NO_SAME_ENG_WAIT = bool(int(os.environ.get("KB_NOSAME", "0")))

S = 2048
D = 1024
NT = 16
NB = 4
KC = 8
DEPTH = 4
ALPHA = float((2 * DEPTH) ** 0.25)
LN_EPS = 1e-5
NEG = -30000.0


class Op:
    __slots__ = ("eng", "fn", "reads", "writes", "is_dma", "idx", "sem", "val", "deps", "prev_val", "barrier")

    def __init__(self, eng, fn, reads, writes, is_dma):
        self.eng = eng
        self.fn = fn
        self.reads = reads
        self.writes = writes
        self.is_dma = is_dma
        self.deps = ()
        self.barrier = False


class Prog:
    def __init__(self, nc):
        self.nc = nc
        self.ops = []

    def op(self, eng, fn, reads=(), writes=()):
        o = Op(eng, fn, tuple(reads), tuple(writes), False)
        self.ops.append(o)
        return o

    def dma(self, eng, fn, reads=(), writes=()):
        o = Op(eng, fn, tuple(reads), tuple(writes), True)
        self.ops.append(o)
        return o

    def barrier(self):
        o = Op(None, None, (), (), False)
        o.barrier = True
        self.ops.append(o)

    def emit(self, stack):
        nc = self.nc
        ops = self.ops
        last_w = {}
        readers = {}
        eng_count = {e: 0 for e in ENGS}
        cur_sem = {}
        n_dma = {e: 0 for e in ENGS}
        per_eng = {e: [] for e in ENGS}
        for o in ops:
            if o.barrier:
                snap = dict(cur_sem)
                for e in ENGS:
                    per_eng[e].append(("bar", snap))
                last_w = {}
                readers = {}
                continue
            deps = set()
            for r in o.reads:
                w = last_w.get(r)
                if w is not None:
                    deps.add(w)
            for r in o.writes:
                w = last_w.get(r)
                if w is not None:
                    deps.add(w)
                for rd in readers.get(r, ()):
                    deps.add(rd)
            deps.discard(o)
            o.deps = deps
            for r in o.reads:
                readers.setdefault(r, []).append(o)
            for r in o.writes:
                last_w[r] = o
                readers[r] = []
            if o.is_dma:
                nd = n_dma[o.eng]
                o.sem = ("dma_" + o.eng, nd % N_DMA_SEMS)
                o.val = 16 * (nd // N_DMA_SEMS + 1)
                o.prev_val = o.val - 16
                n_dma[o.eng] = nd + 1
            else:
                eng_count[o.eng] += 1
                s = eng_count[o.eng]
                o.sem = (o.eng, (s - 1) // EPOCH)
                o.val = (s - 1) % EPOCH + 1
            cur_sem[o.sem] = o.val
            per_eng[o.eng].append(o)
        sems = {}
        for o in ops:
            if not o.barrier and o.sem not in sems:
                sems[o.sem] = stack.enter_context(nc.semaphore("s_%s_%d" % o.sem))
        final = dict(cur_sem)

        def make_body(ename):
            def body(e):
                seen = {}

                def wait(s, v):
                    if seen.get(s, 0) >= v:
                        return
                    seen[s] = v
                    e.wait_ge(sems[s], v)

                for o in per_eng[ename]:
                    if isinstance(o, tuple):
                        for s, v in o[1].items():
                            wait(s, v)
                        continue
                    need = {}
                    for d in o.deps:
                        if (not d.is_dma) and d.eng == "tensor" and ename == "tensor" and not o.is_dma:
                            continue
                        if NO_SAME_ENG_WAIT and (not d.is_dma) and (not o.is_dma) and d.eng == ename and ename in ("vector", "scalar"):
                            continue
                        need[d.sem] = max(need.get(d.sem, 0), d.val)
                    if o.is_dma and o.prev_val > 0:
                        need[o.sem] = max(need.get(o.sem, 0), o.prev_val)
                    for s, v in need.items():
                        wait(s, v)
                    ins = o.fn(e)
                    ins.then_inc(sems[o.sem], 16 if o.is_dma else 1)
                if ename == "sync":
                    for s, v in final.items():
                        wait(s, v)
            return body

        with nc.Block() as block:
            for ename in ENGS:
                getattr(block, ename)(make_body(ename))


INPUT_SHAPES = {
    "x": (S, D), "p": (4, S, 256),
    "ple_w": (4, 256, 1024), "ple_gate_w": (4, 1024, 1024),
    "fox_w_in": (1, 1024, 3088), "fox_w_out": (1, 1024, 1024),
    "lru_w_in": (1, 1024, 2560), "lru_w_a": (1, 10, 128, 128), "lru_w_x": (1, 10, 128, 128),
    "lru_w_out": (1, 1280, 1024),
    "cv_w_in": (1, 1024, 2048), "cv_w_out": (1, 1024, 1024),
    "gdn_w_in": (1, 1024, 4112), "gdn_w_out": (1, 1024, 1024),
    "ffn_w_gu": (2, 1024, 5632), "ffn_w_down": (2, 2816, 1024),
    "moe_w_gu": (2, 8, 1024, 7168), "moe_w_down": (2, 8, 3584, 1024),
}

BC_LAYOUT = {}
_off = 0
for _n, _w in [("ln_mix_g", 4096), ("ln_mix_b", 4096), ("ln_ffn_g", 4096), ("ln_ffn_b", 4096),
               ("fox_b_f", 16), ("cv_b_out", 1024), ("gdn_a_log", 8), ("gdn_dt_bias", 8),
               ("gdn_norm_g", 128), ("moe_b_router", 16), ("moe_w_routerT", 2 * 8 * 1024)]:
    BC_LAYOUT[_n] = (_off, _w)
    _off += _w
BC_W = _off
COL_LAYOUT = {}
_off = 0
for _n, _w in [("lru_conv_w", 40), ("lru_conv_b", 10), ("lru_b_a", 10), ("lru_b_x", 10), ("lru_lambda", 10),
               ("cv_b_in", 16), ("cv_dw_w", 8 * 31), ("cv_dw_b", 8), ("cv_ln_g", 8), ("cv_ln_b", 8),
               ("gdn_conv_w", 24 * 4)]:
    COL_LAYOUT[_n] = (_off, _w)
    _off += _w
COL_W = _off
CM_IDENT, CM_U, CM_MLOW, CM_MUP, CM_STRICT, CM_UBIG = 0, 128, 256, 384, 512, 640
CM_W = 640 + 2048
CAP = 640
NS = CAP // 128
CM_IOTA = CM_W
CM_SLOT = CM_W + CAP
CM_W = CM_W + CAP + 8


def host_consts():
    r = np.arange(128)[:, None]
    c = np.arange(128)[None, :]
    cm = np.zeros((128, CM_W), np.float32)
    cm[:, CM_IDENT:CM_IDENT + 128] = (r == c)
    cm[:, CM_U:CM_U + 128] = (r <= c)
    cm[:, CM_MLOW:CM_MLOW + 128] = np.where(c <= r, 0.0, NEG)
    cm[:, CM_MUP:CM_MUP + 128] = np.where(r <= c, 0.0, NEG)
    cm[:, CM_STRICT:CM_STRICT + 128] = (c < r)
    t = np.arange(512)[None, :]
    for jj in range(4):
        cm[:, CM_UBIG + jj * 512:CM_UBIG + (jj + 1) * 512] = ((jj * 128 + r) <= t)
    cm[:, CM_IOTA:CM_IOTA + CAP] = np.arange(CAP)[None, :]
    for s_ in range(8):
        cm[:, CM_SLOT + s_] = np.arange(128) + 128 * s_
    return cm


class KB:
    def __init__(self, n_layers, mixers=(0, 1, 2, 3), ffns=(0, 1, 0, 1)):
        self.n_layers = n_layers
        self.mixers = mixers
        self.ffns = ffns
        self.nc = bass.Bass("TRN2", target_bir_lowering=False)
        nc = self.nc
        self.dr = {}
        for n, shp in INPUT_SHAPES.items():
            self.dr[n] = nc.dram_tensor(n, list(shp), F32, kind="ExternalInput").ap()
        self.dr["bc"] = nc.dram_tensor("bc", [128, BC_W], F32, kind="ExternalInput").ap()
        self.dr["col"] = nc.dram_tensor("col", [128, COL_W], F32, kind="ExternalInput").ap()
        self.dr["cm"] = nc.dram_tensor("cm", [128, CM_W], F32, kind="ExternalInput").ap()
        self.y = nc.dram_tensor("y", [S, D], F32, kind="ExternalOutput").ap()
        self.P = Prog(nc)
        self.ps_rr = 0
        self.slot_rr = 0
        self.uid = 0

    def T(self, st, name, shape, dt=F32):
        self.uid += 1
        return st.enter_context(self.nc.sbuf_tensor("sb%d_%s" % (self.uid, name), list(shape), dt))

    def psum(self):
        i = self.ps_rr % self.n_rot
        self.ps_rr += 1
        return self.ps[i], ("ps", i)

    def rot(self, key, n):
        v = getattr(self, "_rot_" + key, 0)
        setattr(self, "_rot_" + key, v + 1)
        return v % n

    def wload(self, src3, nk, ncol):
        i = self.slot_rr % self.n_slots
        self.slot_rr += 1
        sl = self.slots[i]
        dst = sl[:, 0:nk * ncol].rearrange("p (k c) -> p k c", k=nk)
        res = ("slot", i)
        self.P.dma("gpsimd", lambda e: e.dma_start(out=dst, in_=src3), writes=[res])
        return dst, res

    def wload_narrow(self, st, name, W, c0, ncol):
        stg = self.T(st, name + "_stg", [128, KC, ncol])
        wb = self.T(st, name + "_bf", [128, KC, ncol], BF16)
        src = W[:, c0:c0 + ncol].rearrange("(k p) c -> p k c", p=128)
        self.P.dma("sync", lambda e: e.dma_start(out=stg[:], in_=src), writes=[name + "_stg"])
        self.P.op("vector", lambda e: e.tensor_copy(out=wb[:], in_=stg[:]), reads=[name + "_stg"], writes=[name + "_bf"])
        return wb, name + "_bf"

    def wview(self, W, r0, nk, c0, ncol):
        return W[r0:r0 + nk * 128, c0:c0 + ncol].rearrange("(k p) c -> p k c", p=128)

    def mm(self, out_ap, out_res, pairs, reads):
        pairs = list(pairs)

        def fn(e):
            n = len(pairs)
            ins = None
            for i, (l, r) in enumerate(pairs):
                ins = e.matmul(out_ap, lhsT=l, rhs=r, start=(i == 0), stop=(i == n - 1))
            return ins
        self.P.op("tensor", fn, reads=reads, writes=[out_res])

    def xb_res(self, tb):
        return [("xb", c, tb) for c in range(KC)]

    def build(self):
        nc, P = self.nc, self.P
        with ExitStack() as st:
            self.st = st
            self.ps = [st.enter_context(nc.psum_tensor("ps%d" % i, [128, 512], F32)) for i in range(8)]
            self.n_rot = 8
            self.xtm = self.T(st, "xtm", [128, NT, D])
            self.xb = self.T(st, "xb", [128, KC, S], BF16)
            self.n_slots = 4
            self.slots = [self.T(st, "slot%d" % i, [128, 4096], BF16) for i in range(self.n_slots)]
            self.cm = self.T(st, "cm", [128, 640])
            self.ones_f = self.T(st, "ones_f", [128, 128])
            self.ones_b = self.T(st, "ones_b", [128, 128], BF16)
            self.lnw = self.T(st, "lnw", [128, 2, D])
            self.small = self.T(st, "small", [128, 8, 32])
            self.ln_st6 = self.T(st, "ln_st6", [128, NT, 12])
            self.ln_mv = self.T(st, "ln_mv", [128, NT, 2])
            self.ln_sd = self.T(st, "ln_sd", [128, 2, NT])
            self.ident = self.cm[:, CM_IDENT:CM_IDENT + 128]
            xtm = self.xtm
            P.dma("sync", lambda e: e.dma_start(out=self.cm[:], in_=self.dr["cm"][:, 0:640]), writes=["cm"])
            P.op("vector", lambda e: e.memset(self.ones_f[:], 1.0), writes=["ones_f"])
            P.op("vector", lambda e: e.memset(self.ones_b[:], 1.0), writes=["ones_b"])
            xsrc = self.dr["x"].rearrange("(t p) d -> p t d", p=128)
            for t in range(NT):
                P.dma("sync", lambda e, t=t: e.dma_start(out=xtm[:, t, :], in_=xsrc[:, t, :]), writes=[("xtm", t)])
            self.rebuild_xb()
            for l in range(self.n_layers):
                self.layer(l)
            for t in range(NT):
                ysrc = self.y.rearrange("(t p) d -> p t d", p=128)
                P.dma("sync", lambda e, t=t: e.dma_start(out=ysrc[:, t, :], in_=xtm[:, t, :]), reads=[("xtm", t)], writes=[("y", t)])
            P.emit(st)
        return nc

    def rebuild_xb(self):
        P = self.P
        xtm, xb = self.xtm, self.xb
        for tb in range(NB):
            for c in range(KC):
                ps, pr = self.psum()

                def fn(e, ps=ps, tb=tb, c=c):
                    ins = None
                    for j in range(4):
                        ins = e.transpose(out=ps[:, j * 128:(j + 1) * 128], in_=xtm[:, tb * 4 + j, c * 128:(c + 1) * 128], identity=self.ident)
                    return ins
                P.op("tensor", fn, reads=[("xtm", tb * 4 + j) for j in range(4)] + ["cm"], writes=[pr])
                dst = xb[:, c, tb * 512:(tb + 1) * 512]
                if (tb * KC + c) % 2 == 0:
                    P.op("scalar", lambda e, ps=ps, dst=dst: e.copy(out=dst, in_=ps[:]), reads=[pr], writes=[("xb", c, tb)])
                else:
                    P.op("vector", lambda e, ps=ps, dst=dst: e.tensor_copy(out=dst, in_=ps[:]), reads=[pr], writes=[("xb", c, tb)])

    def layer_norm(self, which):
        P = self.P
        xtm = self.xtm
        g = self.lnw[:, 0, :]
        b = self.lnw[:, 1, :]
        bc = self.dr["bc"]
        st6, mv, sd = self.ln_st6, self.ln_mv, self.ln_sd
        for i, n in enumerate([["ln_mix_g", "ln_mix_b"], ["ln_ffn_g", "ln_ffn_b"]][which]):
            o0 = BC_LAYOUT[n][0] + self.cur_layer * D
            P.dma("sync", lambda e, i=i, o0=o0: e.dma_start(out=self.lnw[:, i, :], in_=bc[:, o0:o0 + D]), reads=["lnw"], writes=["lnw"])
        for t in range(NT):
            xt = xtm[:, t, :]
            P.op("vector", lambda e, t=t, xt=xt: e.bn_stats(out=st6[:, t, 0:6], in_=xt[:, 0:512]), reads=[("xtm", t), ("lnst", t)], writes=[("lnst", t)])
            P.op("vector", lambda e, t=t, xt=xt: e.bn_stats(out=st6[:, t, 6:12], in_=xt[:, 512:1024]), reads=[("xtm", t), ("lnst", t)], writes=[("lnst", t)])
            P.op("vector", lambda e, t=t: e.bn_aggr(out=mv[:, t, :], in_=st6[:, t, :]), reads=[("lnst", t), "lnmv"], writes=["lnmv"])
        P.op("scalar", lambda e: e.activation(out=sd[:, 0, :], in_=mv[:, :, 1], func=AF.Sqrt, bias=LN_EPS, scale=1.0), reads=["lnmv", "lnsd"], writes=["lnsd"])
        P.op("vector", lambda e: e.reciprocal(out=sd[:, 1, :], in_=sd[:, 0, :]), reads=["lnsd"], writes=["lnsd"])
        for t in range(NT):
            xt = xtm[:, t, :]
            P.op("vector", lambda e, t=t, xt=xt: e.scalar_tensor_tensor(out=xt, in0=xt, scalar=mv[:, t, 0:1], in1=g, op0=ALU.subtract, op1=ALU.mult),
                 reads=[("xtm", t), "lnmv", "lnw"], writes=[("xtm", t)])
            P.op("scalar", lambda e, t=t, xt=xt: e.activation(out=xt, in_=xt, func=AF.Copy, scale=sd[:, 1, t:t + 1]), reads=[("xtm", t), "lnsd"], writes=[("xtm", t)])
            P.op("gpsimd", lambda e, xt=xt: e.tensor_tensor(out=xt, in0=xt, in1=b, op=ALU.add), reads=[("xtm", t), "lnw"], writes=[("xtm", t)])

    def out_proj_acc(self, yT, yres, W, cc, first=False, nk=1):
        P = self.P
        xtm = self.xtm
        for f in range(2):
            wv, wr = self.wload(self.wview(W, cc * 128, nk, f * 512, 512), nk, 512)
            for t in range(NT):
                ps, pr = self.psum()
                if nk == 1:
                    pairs = [(yT[:, t * 128:(t + 1) * 128], wv[:, 0, :])]
                else:
                    pairs = [(yT[:, k, t * 128:(t + 1) * 128], wv[:, k, :]) for k in range(nk)]
                self.mm(ps[:, :], pr, pairs, reads=[wr] + list(yres(t)))
                xs = xtm[:, t, f * 512:(f + 1) * 512]
                if first:
                    P.op("vector", lambda e, xs=xs, ps=ps: e.scalar_tensor_tensor(out=xs, in0=xs, scalar=ALPHA, in1=ps[:], op0=ALU.mult, op1=ALU.add),
                         reads=[pr, ("xtm", t)], writes=[("xtm", t)])
                else:
                    P.op("vector", lambda e, xs=xs, ps=ps: e.tensor_tensor(out=xs, in0=xs, in1=ps[:], op=ALU.add),
                         reads=[pr, ("xtm", t)], writes=[("xtm", t)])

    def out_proj_full(self, yT, nk, yres, W):
        P = self.P
        xtm = self.xtm
        for f in range(2):
            wv, wr = self.wload(self.wview(W, 0, nk, f * 512, 512), nk, 512)
            for t in range(NT):
                ps, pr = self.psum()
                self.mm(ps[:, :], pr, [(yT[:, k, t * 128:(t + 1) * 128], wv[:, k, :]) for k in range(nk)], reads=[wr] + list(yres(t)))
                xs = xtm[:, t, f * 512:(f + 1) * 512]
                P.op("vector", lambda e, xs=xs, ps=ps: e.scalar_tensor_tensor(out=xs, in0=xs, scalar=ALPHA, in1=ps[:], op0=ALU.mult, op1=ALU.add),
                     reads=[pr, ("xtm", t)], writes=[("xtm", t)])

    def layer(self, l):
        P = self.P
        self.cur_layer = l
        m = self.mixers[l]
        if m == 0:
            self.fox()
        elif m == 1:
            self.lru()
        elif m == 2:
            self.conformer()
        elif m == 3:
            self.gdn()
        if m >= 0:
            self.layer_norm(0)
            self.rebuild_xb()
        P.barrier()
        fk = self.ffns[l]
        if fk == 0:
            with ExitStack() as st:
                self.ffn_alloc(st)
                self.ffn(self.dr["ffn_w_gu"][l // 2], self.dr["ffn_w_down"][l // 2], 2816, None)
            self.layer_norm(1)
            self.rebuild_xb()
        elif fk == 2:
            self.scale_x()
            self.layer_norm(1)
            self.rebuild_xb()
        elif fk == 1:
            self.moe(l // 2)
            self.layer_norm(1)
            self.rebuild_xb()
        P.barrier()
        self.ple(l)
        if l != self.n_layers - 1:
            self.rebuild_xb()
        P.barrier()

    def scale_x(self):
        for t in range(NT):
            xt = self.xtm[:, t, :]
            self.P.op("gpsimd", lambda e, xt=xt: e.tensor_scalar(out=xt, in0=xt, scalar1=ALPHA, scalar2=None, op0=ALU.mult),
                      reads=[("xtm", t)], writes=[("xtm", t)])

    def ffn_alloc(self, st):
        self.hT = [self.T(st, "hT%d" % i, [128, 4, S], BF16) for i in range(2)]
        self.sg = [self.T(st, "sg%d" % i, [128, 512]) for i in range(3)]

    def ffn(self, Wgu, Wd, F, gate):
        P = self.P
        xb, xtm = self.xb, self.xtm
        ng = (F + 511) // 512
        for g in range(ng):
            hc = min(512, F - g * 512)
            nj = hc // 128
            wg, wgr = self.wload(self.wview(Wgu, 0, 8, g * 512, hc), 8, hc)
            wu, wur = self.wload(self.wview(Wgu, 0, 8, F + g * 512, hc), 8, hc)
            wd, wdr = self.wload(self.wview(Wd, g * 512, nj, 0, 1024), nj, 1024)
            hi = self.rot("hT", 2)
            hT = self.hT[hi]
            for j in range(nj):
                for n in range(NB):
                    pg, pgr = self.psum()
                    self.mm(pg[:, :], pgr, [(wg[:, k, j * 128:(j + 1) * 128], xb[:, k, n * 512:(n + 1) * 512]) for k in range(KC)],
                            reads=[wgr] + self.xb_res(n))
                    pu, pur = self.psum()
                    self.mm(pu[:, :], pur, [(wu[:, k, j * 128:(j + 1) * 128], xb[:, k, n * 512:(n + 1) * 512]) for k in range(KC)],
                            reads=[wur] + self.xb_res(n))
                    si = self.rot("sg", 3)
                    sg = self.sg[si]
                    P.op("scalar", lambda e, sg=sg, pg=pg: e.activation(out=sg[:], in_=pg[:], func=AF.Silu), reads=[pgr], writes=[("sg", si)])
                    hd = hT[:, j, n * 512:(n + 1) * 512]
                    P.op("vector", lambda e, hd=hd, sg=sg, pu=pu: e.tensor_tensor(out=hd, in0=sg[:], in1=pu[:], op=ALU.mult),
                         reads=[("sg", si), pur], writes=[("hT", hi, j, n)])
            for t in range(NT):
                for f in range(2):
                    po, por = self.psum()
                    self.mm(po[:, :], por, [(hT[:, j, t * 128:(t + 1) * 128], wd[:, j, f * 512:(f + 1) * 512]) for j in range(nj)],
                            reads=[wdr] + [("hT", hi, j, t // 4) for j in range(nj)])
                    xs = xtm[:, t, f * 512:(f + 1) * 512]
                    if gate is None and g == 0:
                        P.op("vector", lambda e, xs=xs, po=po: e.scalar_tensor_tensor(out=xs, in0=xs, scalar=ALPHA, in1=po[:], op0=ALU.mult, op1=ALU.add),
                             reads=[por, ("xtm", t)], writes=[("xtm", t)])
                    elif gate is None:
                        P.op("vector", lambda e, xs=xs, po=po: e.tensor_tensor(out=xs, in0=xs, in1=po[:], op=ALU.add),
                             reads=[por, ("xtm", t)], writes=[("xtm", t)])
                    else:
                        ga, gr = gate(t)
                        P.op("vector", lambda e, xs=xs, po=po, ga=ga: e.scalar_tensor_tensor(out=xs, in0=po[:], scalar=ga, in1=xs, op0=ALU.mult, op1=ALU.add),
                             reads=[por, ("xtm", t), gr], writes=[("xtm", t)])

    def moe(self, li):
        P = self.P
        xtm = self.xtm
        bc = self.dr["bc"]
        U = self.cm[:, CM_U:CM_U + 128]
        with ExitStack() as st0:
            gates = self.T(st0, "gates", [128, NT, 8])
            maskt = self.T(st0, "maskt", [128, NT, 8])
            posm = self.T(st0, "posm", [128, NT, 8])
            with ExitStack() as st:
                wr = self.T(st, "wrT", [128, 8, D])
                junk = self.T(st, "junk", [128, D])
                lg = self.T(st, "lg", [128, NT, 8])
                rs = self.T(st, "rs", [128, NT, 32])
                br = self.T(st, "br", [128, 8])
                o0 = BC_LAYOUT["moe_w_routerT"][0] + li * 8 * D
                P.dma("sync", lambda e: e.dma_start(out=wr[:], in_=bc[:, o0:o0 + 8 * D].rearrange("p (e d) -> p e d", e=8)), writes=["wrT"])
                o1 = BC_LAYOUT["moe_b_router"][0] + li * 8
                P.dma("sync", lambda e: e.dma_start(out=br[:], in_=bc[:, o1:o1 + 8]), writes=["br"])
                for t in range(NT):
                    for ex in range(8):
                        P.op("vector", lambda e, t=t, ex=ex: e.scalar_tensor_tensor(out=junk[:], in0=xtm[:, t, :], scalar=1.0, in1=wr[:, ex, :], op0=ALU.mult, op1=ALU.mult,
                                                                                   accum_out=lg[:, t, ex:ex + 1]),
                             reads=[("xtm", t), "wrT"], writes=["junk", ("lg", t)])
                    r = rs[:, t, :]
                    lt = lg[:, t, :]
                    R = [("lg", t)]
                    P.op("vector", lambda e, lt=lt: e.tensor_tensor(out=lt, in0=lt, in1=br[:], op=ALU.add), reads=R + ["br"], writes=R)
                    P.op("vector", lambda e, lt=lt, r=r: e.max(out=r[:, 0:8], in_=lt), reads=R, writes=R)
                    P.op("vector", lambda e, lt=lt, t=t, r=r: e.tensor_scalar(out=maskt[:, t, :], in0=lt, scalar1=r[:, 1:2], scalar2=None, op0=ALU.is_ge), reads=R, writes=R + [("maskt", t)])
                    P.op("vector", lambda e, r=r: e.tensor_scalar(out=r[:, 16:17], in0=r[:, 0:1], scalar1=-1.0, scalar2=None, op0=ALU.mult), reads=R, writes=R)
                    P.op("scalar", lambda e, lt=lt, r=r: e.activation(out=r[:, 24:32], in_=lt, func=AF.Exp, bias=r[:, 16:17], scale=1.0), reads=R, writes=R)
                    P.op("vector", lambda e, r=r, t=t: e.tensor_tensor(out=r[:, 24:32], in0=r[:, 24:32], in1=maskt[:, t, :], op=ALU.mult), reads=R + [("maskt", t)], writes=R)
                    P.op("vector", lambda e, r=r: e.reduce_sum(out=r[:, 17:18], in_=r[:, 24:32], axis=mybir.AxisListType.X), reads=R, writes=R)
                    P.op("vector", lambda e, r=r: e.reciprocal(out=r[:, 18:19], in_=r[:, 17:18]), reads=R, writes=R)
                    P.op("vector", lambda e, r=r, t=t: e.tensor_scalar(out=gates[:, t, :], in0=r[:, 24:32], scalar1=r[:, 18:19], scalar2=None, op0=ALU.mult),
                         reads=R, writes=[("gates", t)])
                for t in range(NT):
                    ps, pr = self.psum()
                    pairs = [(self.ones_f[:], maskt[:, j, :]) for j in range(t)] + [(U, maskt[:, t, :])]
                    self.mm(ps[:, 0:8], pr, pairs, reads=["ones_f", "cm"] + [("maskt", j) for j in range(t + 1)])
                    P.op("vector", lambda e, ps=ps, t=t: e.tensor_tensor(out=posm[:, t, :], in0=ps[:, 0:8], in1=maskt[:, t, :], op=ALU.mult), reads=[pr, ("maskt", t)], writes=[("posm", t)])
                    P.op("vector", lambda e, t=t: e.tensor_scalar(out=posm[:, t, :], in0=posm[:, t, :], scalar1=-1.0, scalar2=None, op0=ALU.add), reads=[("posm", t)], writes=[("posm", t)])
            P.barrier()
            xtb = self.xb[:, :, :].rearrange("p c s -> p (c s)").rearrange("p (t d) -> p t d", t=NT)
            for t in range(NT):
                if t % 2 == 0:
                    P.op("scalar", lambda e, t=t: e.copy(out=xtb[:, t, :], in_=xtm[:, t, :]), reads=[("xtm", t)], writes=[("xtb", t)])
                else:
                    P.op("vector", lambda e, t=t: e.tensor_copy(out=xtb[:, t, :], in_=xtm[:, t, :]), reads=[("xtm", t)], writes=[("xtb", t)])
            self.scale_x()
            with ExitStack() as st:
                selbuf = self.T(st, "selbuf", [128, NT * CAP], BF16)
                Sel = selbuf[:, :].rearrange("p (t c) -> p t c", t=NT)
                SelT = selbuf[:, :].rearrange("p (s t) -> p s t", s=NS)
                XgT = self.T(st, "XgT", [128, KC, CAP], BF16)
                Yb = XgT[:, :, :].rearrange("p c s -> p (c s)").rearrange("p (s d) -> p s d", s=NS)
                hT = self.T(st, "hTr", [128, 4, CAP], BF16)
                Yacc = self.T(st, "Yacc", [128, NS, D])
                sgl = [self.T(st, "sgr%d" % i, [128, 512]) for i in range(2)]
                iota = self.T(st, "iota", [128, CAP])
                slotid = self.T(st, "slotid", [128, 8])
                dgl = [self.T(st, "dgr%d" % i, [128, 128]) for i in range(2)]
                P.dma("sync", lambda e: e.dma_start(out=iota[:], in_=self.dr["cm"][:, CM_IOTA:CM_IOTA + CAP]), writes=["iota"])
                P.dma("sync", lambda e: e.dma_start(out=slotid[:], in_=self.dr["cm"][:, CM_SLOT:CM_SLOT + 8]), writes=["slotid"])
                blocks = [(0, 512), (512, CAP - 512)]
                ecnt = 0
                for ex in range(8):
                    Wgu = self.dr["moe_w_gu"][li, ex]
                    Wd = self.dr["moe_w_down"][li, ex]
                    F = 3584
                    for t in range(NT):
                        P.op("vector", lambda e, t=t, ex=ex: e.tensor_scalar(out=Sel[:, t, :], in0=iota[:], scalar1=posm[:, t, ex:ex + 1], scalar2=None, op0=ALU.is_equal),
                             reads=["iota", ("posm", t), "selbuf"], writes=["selbuf"])
                    for c in range(KC):
                        for (b0, bn) in blocks:
                            ps, pr = self.psum()
                            self.mm(ps[:, 0:bn], pr, [(xtb[:, t, c * 128:(c + 1) * 128], Sel[:, t, b0:b0 + bn]) for t in range(NT)],
                                    reads=["selbuf"] + [("xtb", t) for t in range(NT)])
                            ecnt += 1
                            if ecnt % 2 == 0:
                                P.op("scalar", lambda e, ps=ps, c=c, b0=b0, bn=bn: e.copy(out=XgT[:, c, b0:b0 + bn], in_=ps[:, 0:bn]), reads=[pr, "XgT"], writes=["XgT"])
                            else:
                                P.op("vector", lambda e, ps=ps, c=c, b0=b0, bn=bn: e.tensor_copy(out=XgT[:, c, b0:b0 + bn], in_=ps[:, 0:bn]), reads=[pr, "XgT"], writes=["XgT"])
                    ng = F // 512
                    for g in range(ng):
                        wg, wgr = self.wload(self.wview(Wgu, 0, 8, g * 512, 512), 8, 512)
                        wu, wur = self.wload(self.wview(Wgu, 0, 8, F + g * 512, 512), 8, 512)
                        wd, wdr = self.wload(self.wview(Wd, g * 512, 4, 0, 1024), 4, 1024)
                        for j in range(4):
                            for (b0, bn) in blocks:
                                pg, pgr = self.psum()
                                self.mm(pg[:, 0:bn], pgr, [(wg[:, k, j * 128:(j + 1) * 128], XgT[:, k, b0:b0 + bn]) for k in range(KC)], reads=[wgr, "XgT"])
                                pu, pur = self.psum()
                                self.mm(pu[:, 0:bn], pur, [(wu[:, k, j * 128:(j + 1) * 128], XgT[:, k, b0:b0 + bn]) for k in range(KC)], reads=[wur, "XgT"])
                                si = self.rot("sgr", 2)
                                sg = sgl[si]
                                P.op("scalar", lambda e, sg=sg, pg=pg, bn=bn: e.activation(out=sg[:, 0:bn], in_=pg[:, 0:bn], func=AF.Silu), reads=[pgr], writes=[("sgr", si)])
                                P.op("vector", lambda e, sg=sg, pu=pu, j=j, b0=b0, bn=bn: e.tensor_tensor(out=hT[:, j, b0:b0 + bn], in0=sg[:, 0:bn], in1=pu[:, 0:bn], op=ALU.mult),
                                     reads=[("sgr", si), pur], writes=[("hTr", j)])
                        for s_ in range(NS):
                            for f in range(2):
                                po, por = self.psum()
                                self.mm(po[:, :], por, [(hT[:, j, s_ * 128:(s_ + 1) * 128], wd[:, j, f * 512:(f + 1) * 512]) for j in range(4)],
                                        reads=[wdr] + [("hTr", j) for j in range(4)])
                                ys = Yacc[:, s_, f * 512:(f + 1) * 512]
                                if g == 0:
                                    P.op("vector", lambda e, ys=ys, po=po: e.tensor_copy(out=ys, in_=po[:]), reads=[por, ("Yacc", s_)], writes=[("Yacc", s_)])
                                else:
                                    P.op("vector", lambda e, ys=ys, po=po: e.tensor_tensor(out=ys, in0=ys, in1=po[:], op=ALU.add), reads=[por, ("Yacc", s_)], writes=[("Yacc", s_)])
                    for s_ in range(NS):
                        P.op("scalar", lambda e, s_=s_: e.copy(out=Yb[:, s_, :], in_=Yacc[:, s_, :]), reads=[("Yacc", s_), "XgT"], writes=["XgT"])
                    for tb in range(NB):
                        ps, pr = self.psum()
                        for j in range(4):
                            t = tb * 4 + j
                            di = self.rot("dgr", 2)
                            dg = dgl[di]
                            P.op("gpsimd", lambda e, dg=dg, t=t, ex=ex: e.tensor_scalar(out=dg[:], in0=self.ident, scalar1=posm[:, t, ex:ex + 1], scalar2=None, op0=ALU.mult),
                                 reads=["cm", ("posm", t), ("dgr", di)], writes=[("dgr", di)])
                            P.op("tensor", lambda e, ps=ps, dg=dg, j=j: e.matmul(ps[:, j * 128:(j + 1) * 128], lhsT=self.ones_f[:], rhs=dg[:], start=True, stop=True),
                                 reads=["ones_f", ("dgr", di)], writes=[pr])
                        for s_ in range(NS):
                            P.op("vector", lambda e, ps=ps, s_=s_, tb=tb: e.tensor_scalar(out=SelT[:, s_, tb * 512:(tb + 1) * 512], in0=ps[:, :], scalar1=slotid[:, s_:s_ + 1], scalar2=None, op0=ALU.is_equal),
                                 reads=[pr, "slotid", "selbuf"], writes=["selbuf"])
                    for t in range(NT):
                        for f in range(2):
                            po, por = self.psum()
                            self.mm(po[:, :], por, [(SelT[:, s_, t * 128:(t + 1) * 128], Yb[:, s_, f * 512:(f + 1) * 512]) for s_ in range(NS)], reads=["selbuf", "XgT"])
                            xs = xtm[:, t, f * 512:(f + 1) * 512]
                            P.op("vector", lambda e, xs=xs, po=po, t=t, ex=ex: e.scalar_tensor_tensor(out=xs, in0=po[:], scalar=gates[:, t, ex:ex + 1], in1=xs, op0=ALU.mult, op1=ALU.add),
                                 reads=[por, ("xtm", t), ("gates", t)], writes=[("xtm", t)])
            P.barrier()

    def ple(self, l):
        P = self.P
        xb, xtm = self.xb, self.xtm
        with ExitStack() as st:
            ptm = self.T(st, "ptm", [128, NT, 256])
            pT = self.T(st, "pT", [128, 2, S], BF16)
            tmp = [self.T(st, "pletmp%d" % i, [128, 512]) for i in range(3)]
            psrc = self.dr["p"][l].rearrange("(t p) f -> p t f", p=128)
            P.dma("sync", lambda e: e.dma_start(out=ptm[:], in_=psrc), writes=["ptm"])
            for tb in range(NB):
                for c in range(2):
                    ps, pr = self.psum()

                    def fn(e, ps=ps, tb=tb, c=c):
                        ins = None
                        for j in range(4):
                            ins = e.transpose(out=ps[:, j * 128:(j + 1) * 128], in_=ptm[:, tb * 4 + j, c * 128:(c + 1) * 128], identity=self.ident)
                        return ins
                    P.op("tensor", fn, reads=["ptm", "cm"], writes=[pr])
                    dst = pT[:, c, tb * 512:(tb + 1) * 512]
                    P.op("scalar", lambda e, ps=ps, dst=dst: e.copy(out=dst, in_=ps[:]), reads=[pr], writes=[("pT", c, tb)])
            wp, wpr = self.wload(self.wview(self.dr["ple_w"][l], 0, 2, 0, 1024), 2, 1024)
            for f in range(2):
                wg, wgr = self.wload(self.wview(self.dr["ple_gate_w"][l], 0, 8, f * 512, 512), 8, 512)
                for t in range(NT):
                    pg, pgr = self.psum()
                    self.mm(pg[:, :], pgr, [(xb[:, k, t * 128:(t + 1) * 128], wg[:, k, :]) for k in range(KC)], reads=[wgr] + self.xb_res(t // 4))
                    pp, ppr = self.psum()
                    self.mm(pp[:, :], ppr, [(pT[:, c, t * 128:(t + 1) * 128], wp[:, c, f * 512:(f + 1) * 512]) for c in range(2)],
                            reads=[wpr, ("pT", 0, t // 4), ("pT", 1, t // 4)])
                    ti = self.rot("pletmp", 3)
                    tm = tmp[ti]
                    P.op("scalar", lambda e, tm=tm, pg=pg: e.activation(out=tm[:], in_=pg[:], func=AF.Sigmoid), reads=[pgr], writes=[("pletmp", ti)])
                    P.op("vector", lambda e, tm=tm, pp=pp: e.tensor_tensor(out=tm[:], in0=tm[:], in1=pp[:], op=ALU.mult), reads=[("pletmp", ti), ppr], writes=[("pletmp", ti)])
                    xs = xtm[:, t, f * 512:(f + 1) * 512]
                    P.op("vector", lambda e, xs=xs, tm=tm: e.tensor_tensor(out=xs, in0=xs, in1=tm[:], op=ALU.add), reads=[("pletmp", ti), ("xtm", t)], writes=[("xtm", t)])

    def fox(self):
        P = self.P
        xb, xtm = self.xb, self.xtm
        W = self.dr["fox_w_in"][0]
        bc = self.dr["bc"]
        self.n_rot = 4
        with ExitStack() as st:
            oT = self.T(st, "oT", [128, 2, S], BF16)
            self.ubig = self.T(st, "ubig", [128, 4, 512], BF16)
            P.dma("gpsimd", lambda e: e.dma_start(out=self.ubig[:], in_=self.dr["cm"][:, CM_UBIG:CM_UBIG + 2048].rearrange("p (j t) -> p j t", j=4)), writes=["ubig"])
            vpad = self.T(st, "vpad", [128, NT, 2, 128], BF16)
            qT = self.T(st, "qT", [128, 2, S], BF16)
            kT = self.T(st, "kT", [128, S], BF16)
            P.op("vector", lambda e: e.memset(qT[:], 0.0), writes=["qTz"])
            pT = [self.T(st, "pexp%d" % i, [128, 512], BF16) for i in range(4)]
            rec = [self.T(st, "rec%d" % i, [128, 512]) for i in range(2)]
            lf = self.T(st, "lf", [128, NT, 16])
            negc = self.T(st, "negc", [128, NT, 16])
            kap = self.T(st, "kap", [128, NB, 16])
            biasT = self.T(st, "biasT", [128, NB, NT, 16])
            bfb = self.T(st, "bfb", [128, 16])
            zt = [self.T(st, "zt%d" % i, [128, 16]) for i in range(2)]
            oneslr = self.T(st, "oneslr", [128, 2, 128], BF16)
            o0 = BC_LAYOUT["fox_b_f"][0]
            P.dma("sync", lambda e: e.dma_start(out=bfb[:], in_=bc[:, o0:o0 + 16]), writes=["bfb"])
            P.op("vector", lambda e: e.memset(vpad[:], 0.0), writes=["vpad"])
            P.op("vector", lambda e: e.memset(oneslr[:], 0.0), writes=["oneslr"])
            P.op("vector", lambda e: e.memset(oneslr[:, 0, 0:64], 1.0), reads=["oneslr"], writes=["oneslr"])
            P.op("vector", lambda e: e.memset(oneslr[:, 1, 64:128], 1.0), reads=["oneslr"], writes=["oneslr"])
            wf, wfr = self.wload_narrow(st, "foxwf", W, 3072, 16)
            for t in range(NT):
                ps, pr = self.psum()
                self.mm(ps[:, 0:16], pr, [(xb[:, k, t * 128:(t + 1) * 128], wf[:, k, :]) for k in range(KC)], reads=[wfr] + self.xb_res(t // 4))
                z = zt[t % 2]
                zr = ("zt", t % 2)
                P.op("vector", lambda e, z=z, ps=ps: e.tensor_tensor(out=z[:], in0=ps[:, 0:16], in1=bfb[:], op=ALU.add), reads=[pr, "bfb"], writes=[zr])
                P.op("scalar", lambda e, z=z: e.activation(out=z[:], in_=z[:], func=AF.Exp, scale=-1.0), reads=[zr], writes=[zr])
                P.op("scalar", lambda e, z=z, t=t: e.activation(out=lf[:, t, :], in_=z[:], func=AF.Ln, bias=1.0, scale=1.0), reads=[zr], writes=[("lf", t)])
            STOP = int(os.environ.get("KB_STOP", "99"))
            if STOP <= 1:
                self.n_rot = 8
                P.barrier()
                return
            U = self.cm[:, CM_U:CM_U + 128]
            for t in range(NT):
                ps, pr = self.psum()
                pairs = [(self.ones_f[:], lf[:, j, :]) for j in range(t)] + [(U, lf[:, t, :])]
                self.mm(ps[:, 0:16], pr, pairs, reads=["ones_f", "cm"] + [("lf", j) for j in range(t + 1)])
                P.op("vector", lambda e, ps=ps, t=t: e.tensor_copy(out=negc[:, t, :], in_=ps[:, 0:16]), reads=[pr], writes=[("negc", t)])
            for n in range(NB):
                ps, pr = self.psum()
                pairs = [(self.ones_f[:], lf[:, j, :]) for j in range(4 * n + 2)]
                self.mm(ps[:, 0:16], pr, pairs, reads=["ones_f"] + [("lf", j) for j in range(4 * n + 2)])
                P.op("vector", lambda e, ps=ps, n=n: e.tensor_copy(out=kap[:, n, :], in_=ps[:, 0:16]), reads=[pr], writes=[("kap", n)])
                for j in range(4 * n + 4):
                    P.op("vector", lambda e, n=n, j=j: e.tensor_tensor(out=biasT[:, n, j, :], in0=negc[:, j, :], in1=kap[:, n, :], op=ALU.subtract),
                         reads=[("negc", j), ("kap", n)], writes=[("biasT", n, j)])
            if STOP <= 2:
                self.n_rot = 8
                P.barrier()
                return
            for c in range(KC):
                if STOP <= 5 and c >= 1:
                    break
                wq, wqr = self.wload(self.wview(W, 0, 8, c * 128, 128), 8, 128)
                wk, wkr = self.wload(self.wview(W, 0, 8, 1024 + c * 128, 128), 8, 128)
                wv, wvr = self.wload(self.wview(W, 0, 8, 2048 + c * 128, 128), 8, 128)
                for n in range(NB):
                    ps, pr = self.psum()
                    self.mm(ps[:, :], pr, [(wq[:, k, :], xb[:, k, n * 512:(n + 1) * 512]) for k in range(KC)], reads=[wqr] + self.xb_res(n))
                    P.op("vector", lambda e, ps=ps, n=n: e.tensor_scalar(out=qT[0:64, 0, n * 512:(n + 1) * 512], in0=ps[0:64, :], scalar1=0.125, scalar2=None, op0=ALU.mult), reads=[pr, "qTz"], writes=[("qT", n, 0)])
                    P.op("vector", lambda e, ps=ps, n=n: e.tensor_scalar(out=qT[64:128, 1, n * 512:(n + 1) * 512], in0=ps[64:128, :], scalar1=0.125, scalar2=None, op0=ALU.mult), reads=[pr, "qTz"], writes=[("qT", n, 1)])
                    ps, pr = self.psum()
                    self.mm(ps[:, :], pr, [(wk[:, k, :], xb[:, k, n * 512:(n + 1) * 512]) for k in range(KC)], reads=[wkr] + self.xb_res(n))
                    P.op("vector", lambda e, ps=ps, n=n: e.tensor_copy(out=kT[:, n * 512:(n + 1) * 512], in_=ps[:]), reads=[pr], writes=[("kT", n)])
                for t in range(NT):
                    ps, pr = self.psum()
                    self.mm(ps[:, 0:128], pr, [(xb[:, k, t * 128:(t + 1) * 128], wv[:, k, :]) for k in range(KC)], reads=[wvr] + self.xb_res(t // 4))
                    P.op("vector", lambda e, ps=ps, t=t: e.tensor_copy(out=vpad[:, t, 0, 0:64], in_=ps[:, 0:64]), reads=[pr, "vpad"], writes=[("vpad", t, 0)])
                    P.op("vector", lambda e, ps=ps, t=t: e.tensor_copy(out=vpad[:, t, 1, 64:128], in_=ps[:, 64:128]), reads=[pr, "vpad"], writes=[("vpad", t, 1)])
                if STOP <= 3:
                    break
                for n in range(NB):
                    if STOP <= 4 and n >= 1:
                        break
                    pi = self.rot("foxacc", 2)
                    num, numr = self.ps[4 + 2 * pi], ("ps", 4 + 2 * pi)
                    den, denr = self.ps[5 + 2 * pi], ("ps", 5 + 2 * pi)
                    nkt = 4 * n + 4
                    total = 2 * nkt
                    steps = [(hh, j) for hh in range(2) for j in range(nkt)]

                    def emit_S(k, n=n, c=c):
                        hh, j = steps[k]
                        sp, spr = self.psum()
                        self.mm(sp[:, :], spr, [(kT[:, j * 128:(j + 1) * 128], qT[:, hh, n * 512:(n + 1) * 512])], reads=[("kT", j // 4), ("qT", n, hh), "qTz"])
                        ei = self.rot("pexp", 4)
                        pe = pT[ei]
                        per = ("pexp", ei)
                        bcol = biasT[:, n, j, 2 * c + hh:2 * c + hh + 1]
                        P.op("scalar", lambda e, pe=pe, sp=sp, bcol=bcol: e.activation(out=pe[:], in_=sp[:], func=AF.Exp, bias=bcol, scale=1.0),
                             reads=[spr, ("biasT", n, j)], writes=[per])
                        if j >= 4 * n:
                            P.op("vector", lambda e, pe=pe, jj=j - 4 * n: e.tensor_tensor(out=pe[:], in0=pe[:], in1=self.ubig[:, jj, :], op=ALU.mult),
                                 reads=[per, "ubig"], writes=[per])
                        return pe, per
                    LA = 2
                    q_ = [emit_S(k) for k in range(min(LA, total))]
                    for idx in range(total):
                        if idx + LA < total:
                            q_.append(emit_S(idx + LA))
                        pe, per = q_[idx]
                        hh, j = steps[idx]
                        first, last = (idx == 0), (idx == total - 1)

                        def fn(e, num=num, den=den, pe=pe, j=j, hh=hh, first=first, last=last):
                            e.matmul(num[:, :], lhsT=vpad[:, j, hh, :], rhs=pe[:], start=first, stop=last)
                            return e.matmul(den[:, :], lhsT=oneslr[:, hh, :], rhs=pe[:], start=first, stop=last)
                        P.op("tensor", fn, reads=[per, ("vpad", j, hh), "vpad", "oneslr"], writes=[numr, denr])
                    rc = rec[pi]
                    P.op("vector", lambda e, rc=rc, den=den: e.reciprocal(out=rc[:], in_=den[:]), reads=[denr], writes=[("rec", pi)])
                    P.op("vector", lambda e, rc=rc, num=num, c=c, n=n: e.tensor_tensor(out=oT[:, c % 2, n * 512:(n + 1) * 512], in0=num[:], in1=rc[:], op=ALU.mult),
                         reads=[numr, ("rec", pi)], writes=[("oT", c % 2, n)])
                if c % 2 == 1:
                    self.out_proj_acc(oT, lambda t: [("oT", 0, t // 4), ("oT", 1, t // 4)], self.dr["fox_w_out"][0], c - 1, first=(c == 1), nk=2)
            self.n_rot = 8
        P.barrier()

    def lru(self):
        P = self.P
        xb = self.xb
        W = self.dr["lru_w_in"][0]
        Wo = self.dr["lru_w_out"][0]
        col = self.dr["col"]
        B4 = lambda nm: [(nm, n) for n in range(NB)]
        with ExitStack() as st:
            cv = self.T(st, "lrucol", [128, 80])
            sp8 = self.T(st, "sp8", [128, 10])
            recp = self.T(st, "recp", [128, 3 + S])
            u = self.T(st, "lru_u", [128, S])
            ub = self.T(st, "lru_ub", [128, S], BF16)
            ra = self.T(st, "lru_ra", [128, S])
            ib = self.T(st, "lru_ib", [128, S])
            y2 = self.T(st, "lru_y", [128, 2, S], BF16)
            wab = self.T(st, "lru_wab", [128, 2, 128], BF16)
            o0 = COL_LAYOUT["lru_conv_w"][0]
            P.dma("sync", lambda e: e.dma_start(out=cv[:], in_=col[:, o0:o0 + 80]), writes=["lrucol"])
            P.op("scalar", lambda e: e.activation(out=sp8[:], in_=cv[:, 70:80], func=AF.Exp, scale=-1.0), reads=["lrucol"], writes=["sp8"])
            P.op("scalar", lambda e: e.activation(out=sp8[:], in_=sp8[:], func=AF.Ln, bias=1.0, scale=1.0), reads=["sp8"], writes=["sp8"])
            P.op("vector", lambda e: e.tensor_scalar(out=sp8[:], in0=sp8[:], scalar1=-8.0, scalar2=None, op0=ALU.mult), reads=["sp8"], writes=["sp8"])
            P.op("vector", lambda e: e.memset(recp[:, 0:3], 0.0), writes=["recpad"])
            for cc in range(10):
                y = y2[:, cc % 2, :]
                yk = "y%d" % (cc % 2)
                wg, wgr = self.wload(self.wview(W, 0, 8, cc * 128, 128), 8, 128)
                wr_, wrr = self.wload(self.wview(W, 0, 8, 1280 + cc * 128, 128), 8, 128)
                P.dma("gpsimd", lambda e, cc=cc: e.dma_start(out=wab[:, 0, :], in_=self.dr["lru_w_a"][0, cc]), writes=[("wab", 0)])
                P.dma("gpsimd", lambda e, cc=cc: e.dma_start(out=wab[:, 1, :], in_=self.dr["lru_w_x"][0, cc]), writes=[("wab", 1)])
                for n in range(NB):
                    bs = slice(n * 512, (n + 1) * 512)
                    ps, pr = self.psum()
                    self.mm(ps[:, :], pr, [(wg[:, k, :], xb[:, k, bs]) for k in range(KC)], reads=[wgr] + self.xb_res(n))
                    P.op("scalar", lambda e, ps=ps, bs=bs, y=y: e.activation(out=y[:, bs], in_=ps[:], func=AF.Gelu_apprx_tanh), reads=[pr], writes=[(yk, n)])
                    ps, pr = self.psum()
                    self.mm(ps[:, :], pr, [(wr_[:, k, :], xb[:, k, bs]) for k in range(KC)], reads=[wrr] + self.xb_res(n))
                    P.op("vector", lambda e, ps=ps, n=n: e.tensor_copy(out=recp[:, 3 + n * 512:3 + (n + 1) * 512], in_=ps[:]), reads=[pr], writes=[("recp", n)])
                cw = lambda k: cv[:, cc * 4 + k:cc * 4 + k + 1]
                cb = cv[:, 40 + cc:41 + cc]
                P.op("vector", lambda e, c0=cw(0), cb=cb: e.tensor_scalar(out=u[:], in0=recp[:, 0:S], scalar1=c0, scalar2=cb, op0=ALU.mult, op1=ALU.add),
                     reads=B4("recp") + ["recpad", "lrucol"], writes=["u"])
                for k in range(1, 4):
                    P.op("vector", lambda e, k=k, ck=cw(k): e.scalar_tensor_tensor(out=u[:], in0=recp[:, k:k + S], scalar=ck, in1=u[:], op0=ALU.mult, op1=ALU.add),
                         reads=B4("recp") + ["recpad", "lrucol", "u"], writes=["u"])
                P.op("scalar", lambda e: e.copy(out=ub[:], in_=u[:]), reads=["u"], writes=["ub"])
                for n in range(NB):
                    bs = slice(n * 512, (n + 1) * 512)
                    ps, pr = self.psum()
                    self.mm(ps[:, :], pr, [(wab[:, 0, :], ub[:, bs])], reads=[("wab", 0), "ub"])
                    P.op("scalar", lambda e, ps=ps, bs=bs, b=cv[:, 50 + cc:51 + cc]: e.activation(out=ra[:, bs], in_=ps[:], func=AF.Sigmoid, bias=b, scale=1.0),
                         reads=[pr, "lrucol"], writes=[("ra", n)])
                    ps, pr = self.psum()
                    self.mm(ps[:, :], pr, [(wab[:, 1, :], ub[:, bs])], reads=[("wab", 1), "ub"])
                    P.op("scalar", lambda e, ps=ps, bs=bs, b=cv[:, 60 + cc:61 + cc]: e.activation(out=ib[:, bs], in_=ps[:], func=AF.Sigmoid, bias=b, scale=1.0),
                         reads=[pr, "lrucol"], writes=[("ib", n)])
                P.op("scalar", lambda e, sc=sp8[:, cc:cc + 1]: e.activation(out=ra[:], in_=ra[:], func=AF.Exp, scale=sc), reads=B4("ra") + ["sp8"], writes=B4("ra"))
                P.op("vector", lambda e: e.tensor_tensor(out=ib[:], in0=ib[:], in1=u[:], op=ALU.mult), reads=B4("ib") + ["u"], writes=B4("ib"))
                P.op("vector", lambda e: e.tensor_tensor(out=u[:], in0=ra[:], in1=ra[:], op=ALU.mult), reads=B4("ra") + ["u"], writes=["u"])
                P.op("scalar", lambda e: e.activation(out=u[:], in_=u[:], func=AF.Sqrt, bias=1.0, scale=-1.0), reads=["u"], writes=["u"])
                P.op("vector", lambda e: e.tensor_tensor(out=ib[:], in0=ib[:], in1=u[:], op=ALU.mult), reads=B4("ib") + ["u"], writes=B4("ib"))
                P.op("vector", lambda e: e.tensor_tensor_scan(out=recp[:, 3:3 + S], data0=ra[:], data1=ib[:], initial=0.0, op0=ALU.mult, op1=ALU.add),
                     reads=B4("ra") + B4("ib") + B4("recp"), writes=B4("recp"))
                P.op("vector", lambda e, y=y: e.tensor_tensor(out=y, in0=y, in1=recp[:, 3:3 + S], op=ALU.mult), reads=B4(yk) + B4("recp"), writes=B4(yk))
                if cc % 2 == 1:
                    self.out_proj_acc(y2, lambda t: [("y0", t // 4), ("y1", t // 4)], Wo, cc - 1, first=(cc == 1), nk=2)
        P.barrier()

    def conformer(self):
        P = self.P
        xb, xtm = self.xb, self.xtm
        W = self.dr["cv_w_in"][0]
        Wo = self.dr["cv_w_out"][0]
        col = self.dr["col"]
        bc = self.dr["bc"]
        B4 = lambda nm: [(nm, n) for n in range(NB)]
        with ExitStack() as st:
            cvc = self.T(st, "cvcol", [128, 288])
            hpad = self.T(st, "hpad", [128, 30 + S], BF16)
            dgk = self.T(st, "dgk", [128, 31, 128], BF16)
            sgt = [self.T(st, "cvsg%d" % i, [128, 512]) for i in range(2)]
            sq = [self.T(st, "cvsq%d" % i, [128, 512], BF16) for i in range(2)]
            convb = self.T(st, "convb", [128, KC, S], BF16)
            mean = self.T(st, "cvmean", [128, 512])
            rstd = self.T(st, "cvrstd", [128, 512])
            bo = self.T(st, "cvbo", [128, D])
            o0 = COL_LAYOUT["cv_b_in"][0]
            P.dma("sync", lambda e: e.dma_start(out=cvc[:], in_=col[:, o0:o0 + 288]), writes=["cvcol"])
            o1 = BC_LAYOUT["cv_b_out"][0]
            P.dma("sync", lambda e: e.dma_start(out=bo[:], in_=bc[:, o1:o1 + D]), writes=["cvbo"])
            P.op("vector", lambda e: e.memset(hpad[:, 0:30], 0.0), writes=["hpadz"])
            for cc in range(KC):
                wv, wvr = self.wload(self.wview(W, 0, 8, cc * 128, 128), 8, 128)
                wg, wgr = self.wload(self.wview(W, 0, 8, 1024 + cc * 128, 128), 8, 128)
                for n in range(NB):
                    bs = slice(n * 512, (n + 1) * 512)
                    pv, pvr = self.psum()
                    self.mm(pv[:, :], pvr, [(wv[:, k, :], xb[:, k, bs]) for k in range(KC)], reads=[wvr] + self.xb_res(n))
                    pg, pgr = self.psum()
                    self.mm(pg[:, :], pgr, [(wg[:, k, :], xb[:, k, bs]) for k in range(KC)], reads=[wgr] + self.xb_res(n))
                    si = self.rot("cvsg", 2)
                    sg = sgt[si]
                    P.op("scalar", lambda e, sg=sg, pg=pg, b=cvc[:, 8 + cc:9 + cc]: e.activation(out=sg[:], in_=pg[:], func=AF.Sigmoid, bias=b, scale=1.0),
                         reads=[pgr, "cvcol"], writes=[("cvsg", si)])
                    P.op("vector", lambda e, sg=sg, pv=pv, n=n, b=cvc[:, cc:cc + 1]: e.scalar_tensor_tensor(out=hpad[:, 30 + n * 512:30 + (n + 1) * 512], in0=pv[:], scalar=b, in1=sg[:], op0=ALU.add, op1=ALU.mult),
                         reads=[pvr, ("cvsg", si), "cvcol"], writes=[("hpad", n)])
                dw = lambda k: cvc[:, 16 + cc * 31 + k:16 + cc * 31 + k + 1]
                db = cvc[:, 264 + cc:265 + cc]
                for k in range(31):
                    eng = "vector" if k % 2 == 0 else "gpsimd"
                    P.op(eng, lambda e, k=k, dk=dw(k): e.tensor_scalar(out=dgk[:, k, :], in0=self.ident, scalar1=dk, scalar2=None, op0=ALU.mult),
                         reads=["cm", "cvcol", ("dgk", k)], writes=[("dgk", k)])
                for n in range(NB):
                    ps, pr = self.psum()
                    self.mm(ps[:, :], pr, [(dgk[:, k, :], hpad[:, k + n * 512:k + (n + 1) * 512]) for k in range(31)],
                            reads=[("dgk", k) for k in range(31)] + B4("hpad") + ["hpadz"])
                    P.op("vector", lambda e, ps=ps, n=n, cc=cc, db=db: e.tensor_scalar(out=convb[:, cc, n * 512:(n + 1) * 512], in0=ps[:], scalar1=db, scalar2=None, op0=ALU.add),
                         reads=[pr, "cvcol"], writes=[("convb", cc, n)])
            for n in range(NB):
                bs = slice(n * 512, (n + 1) * 512)
                psum_, psr = self.psum()
                self.mm(psum_[:, :], psr, [(self.ones_b[:], convb[:, cc, bs]) for cc in range(KC)], reads=["ones_b"] + [("convb", cc, n) for cc in range(KC)])
                psq, pqr = self.psum()
                for cc in range(KC):
                    qi = self.rot("cvsq", 2)
                    P.op("scalar", lambda e, qi=qi, cc=cc, bs=bs: e.activation(out=sq[qi][:], in_=convb[:, cc, bs], func=AF.Square), reads=[("convb", cc, n)], writes=[("cvsq", qi)])
                    P.op("tensor", lambda e, qi=qi, cc=cc, psq=psq: e.matmul(psq[:, :], lhsT=self.ones_b[:], rhs=sq[qi][:], start=(cc == 0), stop=(cc == KC - 1)),
                         reads=["ones_b", ("cvsq", qi)], writes=[pqr])
                P.op("scalar", lambda e, psum_=psum_: e.activation(out=mean[:], in_=psum_[:], func=AF.Copy, scale=1.0 / D), reads=[psr], writes=["cvmean"])
                P.op("vector", lambda e: e.tensor_tensor(out=rstd[:], in0=mean[:], in1=mean[:], op=ALU.mult), reads=["cvmean"], writes=["cvrstd"])
                P.op("vector", lambda e, psq=psq: e.scalar_tensor_tensor(out=rstd[:], in0=psq[:], scalar=1.0 / D, in1=rstd[:], op0=ALU.mult, op1=ALU.subtract),
                     reads=[pqr, "cvrstd"], writes=["cvrstd"])
                P.op("scalar", lambda e: e.activation(out=rstd[:], in_=rstd[:], func=AF.Sqrt, bias=LN_EPS, scale=1.0), reads=["cvrstd"], writes=["cvrstd"])
                P.op("vector", lambda e: e.reciprocal(out=rstd[:], in_=rstd[:]), reads=["cvrstd"], writes=["cvrstd"])
                for cc in range(KC):
                    si = self.rot("cvsg", 2)
                    tm = sgt[si]
                    P.op("vector", lambda e, tm=tm, cc=cc, bs=bs: e.tensor_tensor(out=tm[:], in0=convb[:, cc, bs], in1=mean[:], op=ALU.subtract),
                         reads=[("convb", cc, n), "cvmean"], writes=[("cvsg", si)])
                    P.op("vector", lambda e, tm=tm: e.tensor_tensor(out=tm[:], in0=tm[:], in1=rstd[:], op=ALU.mult), reads=[("cvsg", si), "cvrstd"], writes=[("cvsg", si)])
                    P.op("scalar", lambda e, tm=tm, cc=cc, bs=bs: e.activation(out=convb[:, cc, bs], in_=tm[:], func=AF.Silu, bias=cvc[:, 280 + cc:281 + cc], scale=cvc[:, 272 + cc:273 + cc]),
                         reads=[("cvsg", si), "cvcol"], writes=[("convb", cc, n)])
            self.out_proj_full(convb, KC, lambda t: [("convb", cc, t // 4) for cc in range(KC)], Wo)
            for t in range(NT):
                xt = xtm[:, t, :]
                P.op("gpsimd", lambda e, xt=xt: e.tensor_tensor(out=xt, in0=xt, in1=bo[:], op=ALU.add), reads=[("xtm", t), "cvbo"], writes=[("xtm", t)])
        P.barrier()

    def gdn(self):
        P = self.P
        xb, xtm = self.xb, self.xtm
        W = self.dr["gdn_w_in"][0]
        Wo = self.dr["gdn_w_out"][0]
        col = self.dr["col"]
        bc = self.dr["bc"]
        cm = self.cm
        ident = self.ident
        U = cm[:, CM_U:CM_U + 128]
        mlow = cm[:, CM_MLOW:CM_MLOW + 128]
        mup = cm[:, CM_MUP:CM_MUP + 128]
        strict = cm[:, CM_STRICT:CM_STRICT + 128]
        ones_f = self.ones_f
        B4 = lambda nm: [(nm, n) for n in range(NB)]
        with ExitStack() as st:
            T = lambda nm, shp, dt=F32: self.T(st, "g_" + nm, shp, dt)
            gcol = T("col", [128, 96])
            hb = T("hb", [128, 144])
            beta = T("beta", [128, NT, 8])
            gt = T("gt", [128, NT, 8])
            gc = T("gc", [128, NT, 8])
            ngc = T("ngc", [128, NT, 8])
            egc = T("egc", [128, NT, 8])
            bexp = T("bexp", [128, NT, 8])
            glast = T("glast", [128, NT, 8])
            kendc = T("kendc", [128, NT, 8])
            eglast = T("eglast", [128, NT, 8])
            nea = T("nea", [128, 8])
            cpad = T("cpad", [128, 3 + S])
            cf = T("cf", [128, S])
            otm = cf[:, :].rearrange("p (t d) -> p t d", t=NT)
            qT = T("qT", [128, S])
            kT = T("kT", [128, S])
            vtm = T("vtm", [128, NT, 128], BF16)
            ktm = T("ktm", [128, NT, 128], BF16)
            ohT = T("ohT", [128, S], BF16)
            ssr = T("ssr", [128, 512])
            sqf = T("sqf", [128, 512])
            Sst = T("S", [128, 128])
            mats = {}
            for nm in ["dg", "ndg", "t1", "Dl", "L", "t2", "Du", "R0", "R1", "P0", "P1", "Y0", "Y1",
                       "vb", "kbg", "kend", "nwT", "vnew", "tmpo", "sz", "on"]:
                mats[nm] = T("m_" + nm, [128, 128])
            self.n_slots = 2
            self.slot_rr = 0
            l1a = self.slots[2][:, :].bitcast(F32)
            l1b = self.slots[3][:, :].bitcast(F32)
            lane1 = {}
            for q_, nm in enumerate(["dg", "ndg", "t1", "Dl", "L", "t2", "Du", "R0", "R1", "P0", "P1", "Y0", "Y1"]):
                src_ = l1a if q_ < 8 else l1b
                q2 = q_ % 8
                lane1[nm] = src_[:, q2 * 128:(q2 + 1) * 128]
            TTs = l1b[:, 5 * 128:9 * 128].rearrange("p (a b d) -> p a b d", a=2, b=2)
            ATs = l1b[:, 9 * 128:13 * 128].rearrange("p (a b d) -> p a b d", a=2, b=2)
            o0 = COL_LAYOUT["gdn_conv_w"][0]
            P.dma("sync", lambda e: e.dma_start(out=gcol[:], in_=col[:, o0:o0 + 96]), writes=["gcol"])
            o1 = BC_LAYOUT["gdn_a_log"][0]
            P.dma("sync", lambda e: e.dma_start(out=hb[:], in_=bc[:, o1:o1 + 144]), writes=["hb"])
            P.op("scalar", lambda e: e.activation(out=nea[:], in_=hb[:, 0:8], func=AF.Exp), reads=["hb"], writes=["nea"])
            P.op("vector", lambda e: e.tensor_scalar(out=nea[:], in0=nea[:], scalar1=-1.0, scalar2=None, op0=ALU.mult), reads=["nea"], writes=["nea"])
            P.op("vector", lambda e: e.memset(cpad[:, 0:3], 0.0), writes=["cpadz"])
            wba, wbar = self.wload_narrow(st, "gdnwba", W, 4096, 16)
            for t in range(NT):
                ps, pr = self.psum()
                self.mm(ps[:, 0:16], pr, [(xb[:, k, t * 128:(t + 1) * 128], wba[:, k, :]) for k in range(KC)], reads=[wbar] + self.xb_res(t // 4))
                R = [("gsc", t)]
                P.op("vector", lambda e, ps=ps, t=t: e.tensor_copy(out=beta[:, t, :], in_=ps[:, 0:8]), reads=[pr], writes=R)
                P.op("vector", lambda e, ps=ps, t=t: e.tensor_tensor(out=gt[:, t, :], in0=ps[:, 8:16], in1=hb[:, 8:16], op=ALU.add), reads=[pr, "hb"] + R, writes=R)
                P.op("scalar", lambda e, t=t: e.activation(out=beta[:, t, :], in_=beta[:, t, :], func=AF.Sigmoid), reads=R, writes=R)
                P.op("scalar", lambda e, t=t: e.activation(out=gt[:, t, :], in_=gt[:, t, :], func=AF.Exp), reads=R, writes=R)
                P.op("scalar", lambda e, t=t: e.activation(out=gt[:, t, :], in_=gt[:, t, :], func=AF.Ln, bias=1.0, scale=1.0), reads=R, writes=R)
                P.op("vector", lambda e, t=t: e.tensor_tensor(out=gt[:, t, :], in0=gt[:, t, :], in1=nea[:], op=ALU.mult), reads=R + ["nea"], writes=R)
                ps, pr = self.psum()
                self.mm(ps[:, 0:8], pr, [(U, gt[:, t, :])], reads=R + ["cm"])
                ps2, pr2 = self.psum()
                self.mm(ps2[:, 0:8], pr2, [(ones_f[:], gt[:, t, :])], reads=R + ["ones_f"])
                P.op("vector", lambda e, ps=ps, t=t: e.tensor_copy(out=gc[:, t, :], in_=ps[:, 0:8]), reads=[pr] + R, writes=R)
                P.op("vector", lambda e, ps2=ps2, t=t: e.tensor_copy(out=glast[:, t, :], in_=ps2[:, 0:8]), reads=[pr2] + R, writes=R)
                P.op("vector", lambda e, t=t: e.tensor_scalar(out=ngc[:, t, :], in0=gc[:, t, :], scalar1=-1.0, scalar2=None, op0=ALU.mult), reads=R, writes=R)
                P.op("scalar", lambda e, t=t: e.activation(out=egc[:, t, :], in_=gc[:, t, :], func=AF.Exp), reads=R, writes=R)
                P.op("vector", lambda e, t=t: e.tensor_tensor(out=bexp[:, t, :], in0=egc[:, t, :], in1=beta[:, t, :], op=ALU.mult), reads=R, writes=R)
                P.op("vector", lambda e, t=t: e.tensor_tensor(out=kendc[:, t, :], in0=glast[:, t, :], in1=gc[:, t, :], op=ALU.subtract), reads=R, writes=R)
                P.op("scalar", lambda e, t=t: e.activation(out=kendc[:, t, :], in_=kendc[:, t, :], func=AF.Exp), reads=R, writes=R)
                P.op("scalar", lambda e, t=t: e.activation(out=eglast[:, t, :], in_=glast[:, t, :], func=AF.Exp), reads=R, writes=R)
            for h in range(8):
                for which in range(3):
                    wv, wvr = self.wload(self.wview(W, 0, 8, which * 1024 + h * 128, 128), 8, 128)
                    for n in range(NB):
                        bs = slice(n * 512, (n + 1) * 512)
                        ps, pr = self.psum()
                        self.mm(ps[:, :], pr, [(wv[:, k, :], xb[:, k, bs]) for k in range(KC)], reads=[wvr] + self.xb_res(n))
                        P.op("scalar", lambda e, ps=ps, n=n: e.copy(out=cpad[:, 3 + n * 512:3 + (n + 1) * 512], in_=ps[:]), reads=[pr], writes=[("cpad", n)])
                    cidx = which * 8 + h
                    cw = lambda k: gcol[:, cidx * 4 + k:cidx * 4 + k + 1]
                    P.op("vector", lambda e, c0=cw(0): e.tensor_scalar(out=cf[:], in0=cpad[:, 0:S], scalar1=c0, scalar2=None, op0=ALU.mult),
                         reads=B4("cpad") + ["cpadz", "gcol"], writes=["cf"])
                    for k in range(1, 4):
                        P.op("vector", lambda e, k=k, ck=cw(k): e.scalar_tensor_tensor(out=cf[:], in0=cpad[:, k:k + S], scalar=ck, in1=cf[:], op0=ALU.mult, op1=ALU.add),
                             reads=B4("cpad") + ["cpadz", "gcol", "cf"], writes=["cf"])
                    P.op("scalar", lambda e: e.activation(out=cf[:], in_=cf[:], func=AF.Silu), reads=["cf"], writes=["cf"])
                    if which < 2:
                        dstT = qT if which == 0 else kT
                        dres = "qT" if which == 0 else "kT"
                        for n in range(NB):
                            bs = slice(n * 512, (n + 1) * 512)
                            P.op("vector", lambda e, bs=bs: e.tensor_tensor(out=sqf[:], in0=cf[:, bs], in1=cf[:, bs], op=ALU.mult), reads=["cf"], writes=["sqf"])
                            ps, pr = self.psum()
                            self.mm(ps[:, :], pr, [(ones_f[:], sqf[:])], reads=["ones_f", "sqf"])
                            P.op("scalar", lambda e, ps=ps: e.activation(out=ssr[:], in_=ps[:], func=AF.Sqrt, bias=1e-6, scale=1.0), reads=[pr], writes=["ssr"])
                            P.op("vector", lambda e: e.reciprocal(out=ssr[:], in_=ssr[:]), reads=["ssr"], writes=["ssr"])
                            sc = (128.0 ** -0.5) if which == 0 else 1.0
                            P.op("vector", lambda e, bs=bs, dstT=dstT, sc=sc: e.scalar_tensor_tensor(out=dstT[:, bs], in0=cf[:, bs], scalar=sc, in1=ssr[:], op0=ALU.mult, op1=ALU.mult),
                                 reads=["cf", "ssr"], writes=[(dres, n)])
                        if which == 1:
                            for tb in range(NB):
                                ps, pr = self.psum()

                                def fn(e, ps=ps, tb=tb):
                                    ins = None
                                    for j in range(4):
                                        ins = e.transpose(out=ps[:, j * 128:(j + 1) * 128], in_=kT[:, (tb * 4 + j) * 128:(tb * 4 + j + 1) * 128], identity=ident)
                                    return ins
                                P.op("tensor", fn, reads=[("kT", tb), "cm"], writes=[pr])
                                P.op("vector", lambda e, ps=ps, tb=tb: e.tensor_copy(out=ktm[:, tb * 4:(tb + 1) * 4, :], in_=ps[:].rearrange("p (j d) -> p j d", j=4)),
                                     reads=[pr], writes=[("ktm", tb)])
                    else:
                        for tb in range(NB):
                            ps, pr = self.psum()

                            def fn(e, ps=ps, tb=tb):
                                ins = None
                                for j in range(4):
                                    ins = e.transpose(out=ps[:, j * 128:(j + 1) * 128], in_=cf[:, (tb * 4 + j) * 128:(tb * 4 + j + 1) * 128], identity=ident)
                                return ins
                            P.op("tensor", fn, reads=["cf", "cm"], writes=[pr])
                            P.op("vector", lambda e, ps=ps, tb=tb: e.tensor_copy(out=vtm[:, tb * 4:(tb + 1) * 4, :], in_=ps[:].rearrange("p (j d) -> p j d", j=4)),
                                 reads=[pr], writes=[("vtm", tb)])
                P.op("vector", lambda e: e.memset(Sst[:], 0.0), reads=["S"], writes=["S"])
                PREP = ["dg", "ndg", "t1", "Dl", "L", "t2", "Du", "R0", "R1", "P0", "P1", "Y0", "Y1"]

                def lane_mat(lane, nm):
                    if lane == 0:
                        return mats[nm], nm
                    return lane1[nm], nm + "_l1"

                def make_ops(i, lane, par, banks):
                    prep, tail = [], []
                    bctr = [0]

                    def psb():
                        b = banks[bctr[0] % len(banks)]
                        bctr[0] += 1
                        return self.ps[b], ("ps", b)
                    tctr = [0]

                    def pst():
                        b = (6, 7)[tctr[0] % 2]
                        tctr[0] += 1
                        return self.ps[b], ("ps", b)

                    def MM(lst, out_ap, out_res, pairs, reads):
                        pairs = list(pairs)

                        def fn(e):
                            n = len(pairs)
                            ins = None
                            for q_, (l_, r_) in enumerate(pairs):
                                ins = e.matmul(out_ap, lhsT=l_, rhs=r_, start=(q_ == 0), stop=(q_ == n - 1))
                            return ins
                        lst.append(("tensor", fn, list(reads), [out_res]))
                    cs = slice(i * 128, (i + 1) * 128)
                    SC = [("gsc", i)]
                    colh = lambda tl: tl[:, i, h:h + 1]
                    M = {}
                    RK = {}
                    for nm in PREP:
                        M[nm], RK[nm] = lane_mat(lane, nm)
                    TTp = TTs[:, par, lane, :]
                    ATp = ATs[:, par, lane, :]
                    TTr = ("TT", par, lane)
                    ATr = ("AT", par, lane)
                    A = lambda eng, fn, reads, writes: prep.append((eng, fn, list(reads), list(writes)))
                    psK, prK = psb()
                    MM(prep, psK[:, 0:128], prK, [(kT[:, cs], kT[:, cs])], [("kT", i // 4)])
                    psQ, prQ = psb()
                    MM(prep, psQ[:, 0:128], prQ, [(kT[:, cs], qT[:, cs])], [("kT", i // 4), ("qT", i // 4)])
                    A("vector", lambda e, a=colh(gc): e.tensor_scalar(out=M["dg"][:], in0=ident, scalar1=a, scalar2=None, op0=ALU.mult), SC + ["cm", RK["dg"]], [RK["dg"]])
                    A("gpsimd", lambda e, a=colh(ngc): e.tensor_scalar(out=M["ndg"][:], in0=ident, scalar1=a, scalar2=None, op0=ALU.mult), SC + ["cm", RK["ndg"]], [RK["ndg"]])
                    psG, prG = psb()
                    MM(prep, psG[:, 0:128], prG, [(M["dg"][:], ones_f[:]), (ones_f[:], M["ndg"][:])], [RK["dg"], RK["ndg"], "ones_f"])
                    A("vector", lambda e, psG=psG: e.tensor_tensor(out=M["t1"][:], in0=psG[:, 0:128], in1=mlow, op=ALU.add), [prG, "cm", RK["t1"]], [RK["t1"]])
                    A("scalar", lambda e: e.activation(out=M["Dl"][:], in_=M["t1"][:], func=AF.Exp), [RK["t1"], RK["Dl"]], [RK["Dl"]])
                    A("vector", lambda e, psK=psK, a=colh(beta): e.scalar_tensor_tensor(out=M["L"][:], in0=psK[:, 0:128], scalar=a, in1=M["Dl"][:], op0=ALU.mult, op1=ALU.mult),
                      [prK, RK["Dl"], RK["L"]] + SC, [RK["L"]])
                    A("gpsimd", lambda e: e.tensor_tensor(out=M["L"][:], in0=M["L"][:], in1=strict, op=ALU.mult), [RK["L"], "cm"], [RK["L"]])
                    A("vector", lambda e, psG=psG: e.scalar_tensor_tensor(out=M["t2"][:], in0=psG[:, 0:128], scalar=-1.0, in1=mup, op0=ALU.mult, op1=ALU.add), [prG, "cm", RK["t2"]], [RK["t2"]])
                    A("scalar", lambda e: e.activation(out=M["Du"][:], in_=M["t2"][:], func=AF.Exp), [RK["t2"], RK["Du"]], [RK["Du"]])
                    A("vector", lambda e, psQ=psQ: e.tensor_tensor(out=ATp, in0=psQ[:, 0:128], in1=M["Du"][:], op=ALU.mult), [prQ, RK["Du"], ATr], [ATr])
                    psM, prM = psb()
                    prep.append(("tensor", lambda e, psM=psM: e.transpose(out=psM[:, 0:128], in_=M["L"][:], identity=ident), [RK["L"], "cm"], [prM]))
                    A("vector", lambda e, psM=psM: e.tensor_copy(out=M["R0"][:], in_=psM[:, 0:128]), [prM, RK["R0"]], [RK["R0"]])
                    A("vector", lambda e, psM=psM: e.scalar_tensor_tensor(out=M["Y0"][:], in0=psM[:, 0:128], scalar=-1.0, in1=ident, op0=ALU.mult, op1=ALU.add), [prM, "cm", RK["Y0"]], [RK["Y0"]])
                    Pn, Rn, Yn = "L", "R0", "Y0"
                    for lev in range(1, 7):
                        Pnew, Rnew, Ynew = "P%d" % (lev % 2), "R%d" % (lev % 2), "Y%d" % (lev % 2)
                        psP, prP = psb()
                        MM(prep, psP[:, 0:128], prP, [(M[Rn][:], M[Pn][:])], [RK[Rn], RK[Pn]])
                        if lev < 6:
                            psR, prR = psb()
                            MM(prep, psR[:, 0:128], prR, [(M[Pn][:], M[Rn][:])], [RK[Rn], RK[Pn]])
                        A("scalar", lambda e, psP=psP, Pnew=Pnew: e.copy(out=M[Pnew][:], in_=psP[:, 0:128]), [prP, RK[Pnew]], [RK[Pnew]])
                        if lev < 6:
                            A("vector", lambda e, psR=psR, Rnew=Rnew: e.tensor_copy(out=M[Rnew][:], in_=psR[:, 0:128]), [prR, RK[Rnew]], [RK[Rnew]])
                        psY, prY = psb()
                        MM(prep, psY[:, 0:128], prY, [(M[Pnew][:], M[Yn][:])], [RK[Pnew], RK[Yn]])
                        if lev < 6:
                            A("vector", lambda e, psY=psY, Yn=Yn, Ynew=Ynew: e.tensor_tensor(out=M[Ynew][:], in0=psY[:, 0:128], in1=M[Yn][:], op=ALU.add), [prY, RK[Yn], RK[Ynew]], [RK[Ynew]])
                        else:
                            A("vector", lambda e, psY=psY, Yn=Yn: e.tensor_tensor(out=TTp, in0=psY[:, 0:128], in1=M[Yn][:], op=ALU.add), [prY, RK[Yn], TTr], [TTr])
                        Pn, Rn, Yn = Pnew, Rnew, Ynew
                    m = mats
                    B = lambda eng, fn, reads, writes: tail.append((eng, fn, list(reads), list(writes)))
                    B("vector", lambda e, a=colh(beta): e.tensor_scalar(out=m["vb"][:], in0=vtm[:, i, :], scalar1=a, scalar2=None, op0=ALU.mult), [("vtm", i // 4), "vb"] + SC, ["vb"])
                    B("vector", lambda e, a=colh(bexp): e.tensor_scalar(out=m["kbg"][:], in0=ktm[:, i, :], scalar1=a, scalar2=None, op0=ALU.mult), [("ktm", i // 4), "kbg"] + SC, ["kbg"])
                    B("gpsimd", lambda e, a=colh(kendc): e.tensor_scalar(out=m["kend"][:], in0=ktm[:, i, :], scalar1=a, scalar2=None, op0=ALU.mult), [("ktm", i // 4), "kend"] + SC, ["kend"])
                    psW, prW = pst()
                    MM(tail, psW[:, 0:128], prW, [(m["kbg"][:], TTp)], ["kbg", TTr])
                    B("scalar", lambda e, psW=psW: e.activation(out=m["nwT"][:], in_=psW[:, 0:128], func=AF.Copy, scale=-1.0), [prW, "nwT"], ["nwT"])
                    psV, prV = pst()
                    MM(tail, psV[:, 0:128], prV, [(TTp, m["vb"][:]), (m["nwT"][:], Sst[:])], [TTr, "vb", "nwT", "S"])
                    B("vector", lambda e, psV=psV: e.tensor_copy(out=m["vnew"][:], in_=psV[:, 0:128]), [prV, "vnew"], ["vnew"])
                    psA, prA = pst()
                    MM(tail, psA[:, 0:128], prA, [(qT[:, cs], Sst[:])], [("qT", i // 4), "S"])
                    B("scalar", lambda e, psA=psA, a=colh(egc): e.activation(out=m["tmpo"][:], in_=psA[:, 0:128], func=AF.Copy, scale=a), [prA, "tmpo"] + SC, ["tmpo"])
                    psB, prB = pst()
                    MM(tail, psB[:, 0:128], prB, [(ATp, m["vnew"][:])], [ATr, "vnew"])
                    B("vector", lambda e, psB=psB: e.tensor_tensor(out=otm[:, i, :], in0=psB[:, 0:128], in1=m["tmpo"][:], op=ALU.add), [prB, "tmpo"], [("otm", i), "cf"])
                    psS, prS = pst()
                    MM(tail, psS[:, 0:128], prS, [(m["kend"][:], m["vnew"][:])], ["kend", "vnew"])
                    B("vector", lambda e, psS=psS, a=colh(eglast): e.scalar_tensor_tensor(out=Sst[:], in0=Sst[:], scalar=a, in1=psS[:, 0:128], op0=ALU.mult, op1=ALU.add),
                      [prS, "S"] + SC, ["S"])
                    return prep, tail

                def emit_merged(streams):
                    n = max(len(s_) for s_ in streams) if streams else 0
                    for k in range(n):
                        for s_ in streams:
                            if k < len(s_):
                                eng, fn, reads, writes = s_[k]
                                P.op(eng, fn, reads=reads, writes=writes)
                pending_tail = []
                for pr_ in range(NT // 2):
                    par = pr_ % 2
                    p0, t0 = make_ops(2 * pr_, 0, par, (0, 1, 2))
                    p1, t1 = make_ops(2 * pr_ + 1, 1, par, (3, 4, 5))
                    emit_merged([p0, p1, pending_tail])
                    pending_tail = t0 + t1
                emit_merged([pending_tail])

                wz, wzr = self.wload(self.wview(W, 0, 8, 3072 + h * 128, 128), 8, 128)
                for tb in range(NB):
                    psT, prT = self.psum()
                    for j in range(4):
                        t = tb * 4 + j
                        psz, przr = self.psum()
                        self.mm(psz[:, 0:128], przr, [(xb[:, k, t * 128:(t + 1) * 128], wz[:, k, :]) for k in range(KC)], reads=[wzr] + self.xb_res(tb))
                        P.op("scalar", lambda e, psz=psz: e.activation(out=mats["sz"][:], in_=psz[:, 0:128], func=AF.Silu), reads=[przr, "sz"], writes=["sz"])
                        sm = self.small[:, self.rot("small", 8), :]
                        sres = ("small", (self._rot_small - 1) % 8)
                        P.op("vector", lambda e, t=t, sm=sm: e.scalar_tensor_tensor(out=mats["on"][:], in0=otm[:, t, :], scalar=1.0, in1=otm[:, t, :], op0=ALU.mult, op1=ALU.mult, accum_out=sm[:, 0:1]),
                             reads=[("otm", t), "cf", "on", sres], writes=["on", sres])
                        P.op("scalar", lambda e, sm=sm: e.activation(out=sm[:, 1:2], in_=sm[:, 0:1], func=AF.Sqrt, bias=1e-6, scale=1.0 / 128.0), reads=[sres], writes=[sres])
                        P.op("vector", lambda e, sm=sm: e.reciprocal(out=sm[:, 2:3], in_=sm[:, 1:2]), reads=[sres], writes=[sres])
                        P.op("vector", lambda e, t=t, sm=sm: e.scalar_tensor_tensor(out=mats["on"][:], in0=otm[:, t, :], scalar=sm[:, 2:3], in1=hb[:, 16:144], op0=ALU.mult, op1=ALU.mult),
                             reads=[("otm", t), "cf", sres, "hb", "on"], writes=["on"])
                        P.op("vector", lambda e: e.tensor_tensor(out=mats["on"][:], in0=mats["on"][:], in1=mats["sz"][:], op=ALU.mult), reads=["on", "sz"], writes=["on"])
                        P.op("tensor", lambda e, psT=psT, j=j: e.transpose(out=psT[:, j * 128:(j + 1) * 128], in_=mats["on"][:], identity=ident), reads=["on", "cm"], writes=[prT])
                    P.op("scalar", lambda e, psT=psT, tb=tb: e.copy(out=ohT[:, tb * 512:(tb + 1) * 512], in_=psT[:]), reads=[prT], writes=[("ohT", tb)])
                self.out_proj_acc(ohT, lambda t: [("ohT", t // 4)], Wo, h, first=(h == 0))
            self.n_slots = 4
        P.barrier()


def host_layout(inputs, b):
    m = {}
    m["x"] = np.ascontiguousarray(inputs["x"][b])
    m["p"] = np.ascontiguousarray(inputs["p"][:, b])
    for n in INPUT_SHAPES:
        if n not in ("x", "p"):
            m[n] = np.ascontiguousarray(inputs[n], dtype=np.float32)
    bc = np.zeros((128, BC_W), np.float32)
    for n, (o, w) in BC_LAYOUT.items():
        if n == "moe_w_routerT":
            v = np.transpose(np.asarray(inputs["moe_w_router"]), (0, 2, 1)).reshape(-1)
        else:
            v = np.asarray(inputs[n]).reshape(-1)
        bc[:, o:o + w] = v[None, :]
    m["bc"] = bc
    col = np.zeros((128, COL_W), np.float32)

    def put(n, arr):
        o, w = COL_LAYOUT[n]
        col[:, o:o + w] = arr.reshape(128, w)
    put("lru_conv_w", np.asarray(inputs["lru_conv_w"])[0].reshape(4, 10, 128).transpose(2, 1, 0))
    for n in ("lru_conv_b", "lru_b_a", "lru_b_x", "lru_lambda"):
        put(n, np.asarray(inputs[n])[0].reshape(10, 128).T)
    put("cv_b_in", np.asarray(inputs["cv_b_in"])[0].reshape(16, 128).T)
    put("cv_dw_w", np.asarray(inputs["cv_dw_w"])[0].reshape(31, 8, 128).transpose(2, 1, 0))
    for n in ("cv_dw_b", "cv_ln_g", "cv_ln_b"):
        put(n, np.asarray(inputs[n])[0].reshape(8, 128).T)
    put("gdn_conv_w", np.asarray(inputs["gdn_conv_w"])[0].reshape(4, 24, 128).transpose(2, 1, 0))
    m["col"] = col
    m["cm"] = host_consts()
    return m


_NC_CACHE = {}


def run(inputs, n_layers=DEPTH, mixers=(0, 1, 2, 3), ffns=(0, 1, 0, 1), cores=8):
    key = (n_layers, tuple(mixers), tuple(ffns))
    if key not in _NC_CACHE:
        _NC_CACHE[key] = KB(n_layers, mixers, ffns).build()
    nc = _NC_CACHE[key]
    in_maps = [host_layout(inputs, b) for b in range(cores)]
    if os.environ.get("KB_TRACE"):
        res = run_bass_kernel_spmd(nc, in_maps, core_ids=list(range(cores)), trace=True)
        print("EXEC_NS", res.exec_time_ns, flush=True)
    else:
        res = run_bass_kernel_spmd(nc, in_maps, core_ids=list(range(cores)))
    return np.stack([r["y"] for r in res.results], axis=0)


def kernel(**inputs):
    inputs = {k: np.asarray(v) for k, v in inputs.items()}
    return run(inputs).astype(np.float32)
```

```python
import os
import numpy as np
from contextlib import ExitStack
import concourse.bass as bass
import concourse.mybir as mybir
from concourse.bass_utils import run_bass_kernel_spmd

F32 = mybir.dt.float32
BF16 = mybir.dt.bfloat16
AF = mybir.ActivationFunctionType
ALU = mybir.AluOpType

ENGS = ("tensor", "vector", "scalar", "gpsimd", "sync")
EPOCH = 16000
N_DMA_SEMS = 24
NO_SAME_ENG_WAIT = bool(int(os.environ.get("KB_NOSAME", "0")))

S = 2048
D = 1024
NT = 16
NB = 4
KC = 8
DEPTH = 4
ALPHA = float((2 * DEPTH) ** 0.25)
LN_EPS = 1e-5
NEG = -30000.0


class Op:
    __slots__ = ("eng", "fn", "reads", "writes", "is_dma", "idx", "sem", "val", "deps", "prev_val", "barrier")

    def __init__(self, eng, fn, reads, writes, is_dma):
        self.eng = eng
        self.fn = fn
        self.reads = reads
        self.writes = writes
        self.is_dma = is_dma
        self.deps = ()
        self.barrier = False


class Prog:
    def __init__(self, nc):
        self.nc = nc
        self.ops = []

    def op(self, eng, fn, reads=(), writes=()):
        o = Op(eng, fn, tuple(reads), tuple(writes), False)
        self.ops.append(o)
        return o

    def dma(self, eng, fn, reads=(), writes=()):
        o = Op(eng, fn, tuple(reads), tuple(writes), True)
        self.ops.append(o)
        return o

    def barrier(self):
        o = Op(None, None, (), (), False)
        o.barrier = True
        self.ops.append(o)

    def emit(self, stack):
        nc = self.nc
        ops = self.ops
        last_w = {}
        readers = {}
        eng_count = {e: 0 for e in ENGS}
        cur_sem = {}
        n_dma = {e: 0 for e in ENGS}
        per_eng = {e: [] for e in ENGS}
        for o in ops:
            if o.barrier:
                snap = dict(cur_sem)
                for e in ENGS:
                    per_eng[e].append(("bar", snap))
                last_w = {}
                readers = {}
                continue
            deps = set()
            for r in o.reads:
                w = last_w.get(r)
                if w is not None:
                    deps.add(w)
            for r in o.writes:
                w = last_w.get(r)
                if w is not None:
                    deps.add(w)
                for rd in readers.get(r, ()):
                    deps.add(rd)
            deps.discard(o)
            o.deps = deps
            for r in o.reads:
                readers.setdefault(r, []).append(o)
            for r in o.writes:
                last_w[r] = o
                readers[r] = []
            if o.is_dma:
                nd = n_dma[o.eng]
                o.sem = ("dma_" + o.eng, nd % N_DMA_SEMS)
                o.val = 16 * (nd // N_DMA_SEMS + 1)
                o.prev_val = o.val - 16
                n_dma[o.eng] = nd + 1
            else:
                eng_count[o.eng] += 1
                s = eng_count[o.eng]
                o.sem = (o.eng, (s - 1) // EPOCH)
                o.val = (s - 1) % EPOCH + 1
            cur_sem[o.sem] = o.val
            per_eng[o.eng].append(o)
        sems = {}
        for o in ops:
            if not o.barrier and o.sem not in sems:
                sems[o.sem] = stack.enter_context(nc.semaphore("s_%s_%d" % o.sem))
        final = dict(cur_sem)

        def make_body(ename):
            def body(e):
                seen = {}

                def wait(s, v):
                    if seen.get(s, 0) >= v:
                        return
                    seen[s] = v
                    e.wait_ge(sems[s], v)

                for o in per_eng[ename]:
                    if isinstance(o, tuple):
                        for s, v in o[1].items():
                            wait(s, v)
                        continue
                    need = {}
                    for d in o.deps:
                        if (not d.is_dma) and d.eng == "tensor" and ename == "tensor" and not o.is_dma:
                            continue
                        if NO_SAME_ENG_WAIT and (not d.is_dma) and (not o.is_dma) and d.eng == ename and ename in ("vector", "scalar"):
                            continue
                        need[d.sem] = max(need.get(d.sem, 0), d.val)
                    if o.is_dma and o.prev_val > 0:
                        need[o.sem] = max(need.get(o.sem, 0), o.prev_val)
                    for s, v in need.items():
                        wait(s, v)
                    ins = o.fn(e)
                    ins.then_inc(sems[o.sem], 16 if o.is_dma else 1)
                if ename == "sync":
                    for s, v in final.items():
                        wait(s, v)
            return body

        with nc.Block() as block:
            for ename in ENGS:
                getattr(block, ename)(make_body(ename))


INPUT_SHAPES = {
    "x": (S, D), "p": (4, S, 256),
    "ple_w": (4, 256, 1024), "ple_gate_w": (4, 1024, 1024),
    "fox_w_in": (1, 1024, 3088), "fox_w_out": (1, 1024, 1024),
    "lru_w_in": (1, 1024, 2560), "lru_w_a": (1, 10, 128, 128), "lru_w_x": (1, 10, 128, 128),
    "lru_w_out": (1, 1280, 1024),
    "cv_w_in": (1, 1024, 2048), "cv_w_out": (1, 1024, 1024),
    "gdn_w_in": (1, 1024, 4112), "gdn_w_out": (1, 1024, 1024),
    "ffn_w_gu": (2, 1024, 5632), "ffn_w_down": (2, 2816, 1024),
    "moe_w_gu": (2, 8, 1024, 7168), "moe_w_down": (2, 8, 3584, 1024),
}

BC_LAYOUT = {}
_off = 0
for _n, _w in [("ln_mix_g", 4096), ("ln_mix_b", 4096), ("ln_ffn_g", 4096), ("ln_ffn_b", 4096),
               ("fox_b_f", 16), ("cv_b_out", 1024), ("gdn_a_log", 8), ("gdn_dt_bias", 8),
               ("gdn_norm_g", 128), ("moe_b_router", 16), ("moe_w_routerT", 2 * 8 * 1024)]:
    BC_LAYOUT[_n] = (_off, _w)
    _off += _w
BC_W = _off
COL_LAYOUT = {}
_off = 0
for _n, _w in [("lru_conv_w", 40), ("lru_conv_b", 10), ("lru_b_a", 10), ("lru_b_x", 10), ("lru_lambda", 10),
               ("cv_b_in", 16), ("cv_dw_w", 8 * 31), ("cv_dw_b", 8), ("cv_ln_g", 8), ("cv_ln_b", 8),
               ("gdn_conv_w", 24 * 4)]:
    COL_LAYOUT[_n] = (_off, _w)
    _off += _w
COL_W = _off
CM_IDENT, CM_U, CM_MLOW, CM_MUP, CM_STRICT, CM_UBIG = 0, 128, 256, 384, 512, 640
CM_W = 640 + 2048
CAP = 640
NS = CAP // 128
CM_IOTA = CM_W
CM_SLOT = CM_W + CAP
CM_W = CM_W + CAP + 8


def host_consts():
    r = np.arange(128)[:, None]
    c = np.arange(128)[None, :]
    cm = np.zeros((128, CM_W), np.float32)
    cm[:, CM_IDENT:CM_IDENT + 128] = (r == c)
    cm[:, CM_U:CM_U + 128] = (r <= c)
    cm[:, CM_MLOW:CM_MLOW + 128] = np.where(c <= r, 0.0, NEG)
    cm[:, CM_MUP:CM_MUP + 128] = np.where(r <= c, 0.0, NEG)
    cm[:, CM_STRICT:CM_STRICT + 128] = np.where(c < r, 0.0, NEG)
    t = np.arange(512)[None, :]
    for jj in range(4):
        cm[:, CM_UBIG + jj * 512:CM_UBIG + (jj + 1) * 512] = ((jj * 128 + r) <= t)
    cm[:, CM_IOTA:CM_IOTA + CAP] = np.arange(CAP)[None, :]
    for s_ in range(8):
        cm[:, CM_SLOT + s_] = np.arange(128) + 128 * s_
    return cm


class KB:
    def __init__(self, n_layers, mixers=(0, 1, 2, 3), ffns=(0, 1, 0, 1)):
        self.n_layers = n_layers
        self.mixers = mixers
        self.ffns = ffns
        self.nc = bass.Bass("TRN2", target_bir_lowering=False)
        nc = self.nc
        self.dr = {}
        for n, shp in INPUT_SHAPES.items():
            self.dr[n] = nc.dram_tensor(n, list(shp), F32, kind="ExternalInput").ap()
        self.dr["bc"] = nc.dram_tensor("bc", [128, BC_W], F32, kind="ExternalInput").ap()
        self.dr["col"] = nc.dram_tensor("col", [128, COL_W], F32, kind="ExternalInput").ap()
        self.dr["cm"] = nc.dram_tensor("cm", [128, CM_W], F32, kind="ExternalInput").ap()
        self.y = nc.dram_tensor("y", [S, D], F32, kind="ExternalOutput").ap()
        self.P = Prog(nc)
        self.ps_rr = 0
        self.slot_rr = 0
        self.uid = 0

    def T(self, st, name, shape, dt=F32):
        self.uid += 1
        return st.enter_context(self.nc.sbuf_tensor("sb%d_%s" % (self.uid, name), list(shape), dt))

    def psum(self):
        i = self.ps_rr % self.n_rot
        self.ps_rr += 1
        return self.ps[i], ("ps", i)

    def rot(self, key, n):
        v = getattr(self, "_rot_" + key, 0)
        setattr(self, "_rot_" + key, v + 1)
        return v % n

    def wload(self, src3, nk, ncol):
        i = self.slot_rr % self.n_slots
        self.slot_rr += 1
        sl = self.slots[i]
        dst = sl[:, 0:nk * ncol].rearrange("p (k c) -> p k c", k=nk)
        res = ("slot", i)
        self.P.dma("gpsimd", lambda e: e.dma_start(out=dst, in_=src3), writes=[res])
        return dst, res

    def wload_narrow(self, st, name, W, c0, ncol):
        stg = self.T(st, name + "_stg", [128, KC, ncol])
        wb = self.T(st, name + "_bf", [128, KC, ncol], BF16)
        src = W[:, c0:c0 + ncol].rearrange("(k p) c -> p k c", p=128)
        self.P.dma("sync", lambda e: e.dma_start(out=stg[:], in_=src), writes=[name + "_stg"])
        self.P.op("vector", lambda e: e.tensor_copy(out=wb[:], in_=stg[:]), reads=[name + "_stg"], writes=[name + "_bf"])
        return wb, name + "_bf"

    def wview(self, W, r0, nk, c0, ncol):
        return W[r0:r0 + nk * 128, c0:c0 + ncol].rearrange("(k p) c -> p k c", p=128)

    def mm(self, out_ap, out_res, pairs, reads):
        pairs = list(pairs)

        def fn(e):
            n = len(pairs)
            ins = None
            for i, (l, r) in enumerate(pairs):
                ins = e.matmul(out_ap, lhsT=l, rhs=r, start=(i == 0), stop=(i == n - 1))
            return ins
        self.P.op("tensor", fn, reads=reads, writes=[out_res])

    def xb_res(self, tb):
        return [("xb", c, tb) for c in range(KC)]

    def build(self):
        nc, P = self.nc, self.P
        with ExitStack() as st:
            self.st = st
            self.ps = [st.enter_context(nc.psum_tensor("ps%d" % i, [128, 512], F32)) for i in range(8)]
            self.n_rot = 8
            self.xtm = self.T(st, "xtm", [128, NT, D])
            self.xb = self.T(st, "xb", [128, KC, S], BF16)
            self.n_slots = 4
            self.slots = [self.T(st, "slot%d" % i, [128, 4096], BF16) for i in range(self.n_slots)]
            self.cm = self.T(st, "cm", [128, 640])
            self.ones_f = self.T(st, "ones_f", [128, 128])
            self.ones_b = self.T(st, "ones_b", [128, 128], BF16)
            self.lnw = self.T(st, "lnw", [128, 2, D])
            self.small = self.T(st, "small", [128, 8, 32])
            self.ln_st6 = self.T(st, "ln_st6", [128, NT, 12])
            self.ln_mv = self.T(st, "ln_mv", [128, NT, 2])
            self.ln_sd = self.T(st, "ln_sd", [128, 2, NT])
            self.ident = self.cm[:, CM_IDENT:CM_IDENT + 128]
            xtm = self.xtm
            P.dma("sync", lambda e: e.dma_start(out=self.cm[:], in_=self.dr["cm"][:, 0:640]), writes=["cm"])
            P.op("vector", lambda e: e.memset(self.ones_f[:], 1.0), writes=["ones_f"])
            P.op("vector", lambda e: e.memset(self.ones_b[:], 1.0), writes=["ones_b"])
            xsrc = self.dr["x"].rearrange("(t p) d -> p t d", p=128)
            for t in range(NT):
                P.dma("sync", lambda e, t=t: e.dma_start(out=xtm[:, t, :], in_=xsrc[:, t, :]), writes=[("xtm", t)])
            self.rebuild_xb()
            for l in range(self.n_layers):
                self.layer(l)
            for t in range(NT):
                ysrc = self.y.rearrange("(t p) d -> p t d", p=128)
                P.dma("sync", lambda e, t=t: e.dma_start(out=ysrc[:, t, :], in_=xtm[:, t, :]), reads=[("xtm", t)], writes=[("y", t)])
            P.emit(st)
        return nc

    def rebuild_xb(self):
        P = self.P
        xtm, xb = self.xtm, self.xb
        for tb in range(NB):
            for c in range(KC):
                ps, pr = self.psum()

                def fn(e, ps=ps, tb=tb, c=c):
                    ins = None
                    for j in range(4):
                        ins = e.transpose(out=ps[:, j * 128:(j + 1) * 128], in_=xtm[:, tb * 4 + j, c * 128:(c + 1) * 128], identity=self.ident)
                    return ins
                P.op("tensor", fn, reads=[("xtm", tb * 4 + j) for j in range(4)] + ["cm"], writes=[pr])
                dst = xb[:, c, tb * 512:(tb + 1) * 512]
                if (tb * KC + c) % 2 == 0:
                    P.op("scalar", lambda e, ps=ps, dst=dst: e.copy(out=dst, in_=ps[:]), reads=[pr], writes=[("xb", c, tb)])
                else:
                    P.op("vector", lambda e, ps=ps, dst=dst: e.tensor_copy(out=dst, in_=ps[:]), reads=[pr], writes=[("xb", c, tb)])

    def layer_norm(self, which):
        P = self.P
        xtm = self.xtm
        g = self.lnw[:, 0, :]
        b = self.lnw[:, 1, :]
        bc = self.dr["bc"]
        st6, mv, sd = self.ln_st6, self.ln_mv, self.ln_sd
        for i, n in enumerate([["ln_mix_g", "ln_mix_b"], ["ln_ffn_g", "ln_ffn_b"]][which]):
            o0 = BC_LAYOUT[n][0] + self.cur_layer * D
            P.dma("sync", lambda e, i=i, o0=o0: e.dma_start(out=self.lnw[:, i, :], in_=bc[:, o0:o0 + D]), reads=["lnw"], writes=["lnw"])
        for t in range(NT):
            xt = xtm[:, t, :]
            P.op("vector", lambda e, t=t, xt=xt: e.bn_stats(out=st6[:, t, 0:6], in_=xt[:, 0:512]), reads=[("xtm", t), ("lnst", t)], writes=[("lnst", t)])
            P.op("vector", lambda e, t=t, xt=xt: e.bn_stats(out=st6[:, t, 6:12], in_=xt[:, 512:1024]), reads=[("xtm", t), ("lnst", t)], writes=[("lnst", t)])
            P.op("vector", lambda e, t=t: e.bn_aggr(out=mv[:, t, :], in_=st6[:, t, :]), reads=[("lnst", t), "lnmv"], writes=["lnmv"])
        P.op("scalar", lambda e: e.activation(out=sd[:, 0, :], in_=mv[:, :, 1], func=AF.Sqrt, bias=LN_EPS, scale=1.0), reads=["lnmv", "lnsd"], writes=["lnsd"])
        P.op("vector", lambda e: e.reciprocal(out=sd[:, 1, :], in_=sd[:, 0, :]), reads=["lnsd"], writes=["lnsd"])
        for t in range(NT):
            xt = xtm[:, t, :]
            P.op("vector", lambda e, t=t, xt=xt: e.scalar_tensor_tensor(out=xt, in0=xt, scalar=mv[:, t, 0:1], in1=g, op0=ALU.subtract, op1=ALU.mult),
                 reads=[("xtm", t), "lnmv", "lnw"], writes=[("xtm", t)])
            P.op("scalar", lambda e, t=t, xt=xt: e.activation(out=xt, in_=xt, func=AF.Copy, scale=sd[:, 1, t:t + 1]), reads=[("xtm", t), "lnsd"], writes=[("xtm", t)])
            P.op("gpsimd", lambda e, xt=xt: e.tensor_tensor(out=xt, in0=xt, in1=b, op=ALU.add), reads=[("xtm", t), "lnw"], writes=[("xtm", t)])

    def out_proj_acc(self, yT, yres, W, cc, first=False, nk=1):
        P = self.P
        xtm = self.xtm
        for f in range(2):
            wv, wr = self.wload(self.wview(W, cc * 128, nk, f * 512, 512), nk, 512)
            for t in range(NT):
                ps, pr = self.psum()
                if nk == 1:
                    pairs = [(yT[:, t * 128:(t + 1) * 128], wv[:, 0, :])]
                else:
                    pairs = [(yT[:, k, t * 128:(t + 1) * 128], wv[:, k, :]) for k in range(nk)]
                self.mm(ps[:, :], pr, pairs, reads=[wr] + list(yres(t)))
                xs = xtm[:, t, f * 512:(f + 1) * 512]
                if first:
                    P.op("vector", lambda e, xs=xs, ps=ps: e.scalar_tensor_tensor(out=xs, in0=xs, scalar=ALPHA, in1=ps[:], op0=ALU.mult, op1=ALU.add),
                         reads=[pr, ("xtm", t)], writes=[("xtm", t)])
                else:
                    P.op("vector", lambda e, xs=xs, ps=ps: e.tensor_tensor(out=xs, in0=xs, in1=ps[:], op=ALU.add),
                         reads=[pr, ("xtm", t)], writes=[("xtm", t)])

    def out_proj_full(self, yT, nk, yres, W):
        P = self.P
        xtm = self.xtm
        for f in range(2):
            wv, wr = self.wload(self.wview(W, 0, nk, f * 512, 512), nk, 512)
            for t in range(NT):
                ps, pr = self.psum()
                self.mm(ps[:, :], pr, [(yT[:, k, t * 128:(t + 1) * 128], wv[:, k, :]) for k in range(nk)], reads=[wr] + list(yres(t)))
                xs = xtm[:, t, f * 512:(f + 1) * 512]
                P.op("vector", lambda e, xs=xs, ps=ps: e.scalar_tensor_tensor(out=xs, in0=xs, scalar=ALPHA, in1=ps[:], op0=ALU.mult, op1=ALU.add),
                     reads=[pr, ("xtm", t)], writes=[("xtm", t)])

    def layer(self, l):
        P = self.P
        self.cur_layer = l
        m = self.mixers[l]
        if m == 0:
            self.fox()
        elif m == 1:
            self.lru()
        elif m == 2:
            self.conformer()
        elif m == 3:
            self.gdn()
        if m >= 0:
            self.layer_norm(0)
            self.rebuild_xb()
        P.barrier()
        fk = self.ffns[l]
        if fk == 0:
            with ExitStack() as st:
                self.ffn_alloc(st)
                self.ffn(self.dr["ffn_w_gu"][l // 2], self.dr["ffn_w_down"][l // 2], 2816, None)
            self.layer_norm(1)
            self.rebuild_xb()
        elif fk == 2:
            self.scale_x()
            self.layer_norm(1)
            self.rebuild_xb()
        elif fk == 1:
            self.moe(l // 2)
            self.layer_norm(1)
            self.rebuild_xb()
        P.barrier()
        self.ple(l)
        if l != self.n_layers - 1:
            self.rebuild_xb()
        P.barrier()

    def scale_x(self):
        for t in range(NT):
            xt = self.xtm[:, t, :]
            self.P.op("gpsimd", lambda e, xt=xt: e.tensor_scalar(out=xt, in0=xt, scalar1=ALPHA, scalar2=None, op0=ALU.mult),
                      reads=[("xtm", t)], writes=[("xtm", t)])

    def ffn_alloc(self, st):
        self.hT = [self.T(st, "hT%d" % i, [128, 4, S], BF16) for i in range(2)]
        self.sg = [self.T(st, "sg%d" % i, [128, 512]) for i in range(3)]

    def ffn(self, Wgu, Wd, F, gate):
        P = self.P
        xb, xtm = self.xb, self.xtm
        ng = (F + 511) // 512
        for g in range(ng):
            hc = min(512, F - g * 512)
            nj = hc // 128
            wg, wgr = self.wload(self.wview(Wgu, 0, 8, g * 512, hc), 8, hc)
            wu, wur = self.wload(self.wview(Wgu, 0, 8, F + g * 512, hc), 8, hc)
            wd, wdr = self.wload(self.wview(Wd, g * 512, nj, 0, 1024), nj, 1024)
            hi = self.rot("hT", 2)
            hT = self.hT[hi]
            for j in range(nj):
                for n in range(NB):
                    pg, pgr = self.psum()
                    self.mm(pg[:, :], pgr, [(wg[:, k, j * 128:(j + 1) * 128], xb[:, k, n * 512:(n + 1) * 512]) for k in range(KC)],
                            reads=[wgr] + self.xb_res(n))
                    pu, pur = self.psum()
                    self.mm(pu[:, :], pur, [(wu[:, k, j * 128:(j + 1) * 128], xb[:, k, n * 512:(n + 1) * 512]) for k in range(KC)],
                            reads=[wur] + self.xb_res(n))
                    si = self.rot("sg", 3)
                    sg = self.sg[si]
                    P.op("scalar", lambda e, sg=sg, pg=pg: e.activation(out=sg[:], in_=pg[:], func=AF.Silu), reads=[pgr], writes=[("sg", si)])
                    hd = hT[:, j, n * 512:(n + 1) * 512]
                    P.op("vector", lambda e, hd=hd, sg=sg, pu=pu: e.tensor_tensor(out=hd, in0=sg[:], in1=pu[:], op=ALU.mult),
                         reads=[("sg", si), pur], writes=[("hT", hi, j, n)])
            for t in range(NT):
                for f in range(2):
                    po, por = self.psum()
                    self.mm(po[:, :], por, [(hT[:, j, t * 128:(t + 1) * 128], wd[:, j, f * 512:(f + 1) * 512]) for j in range(nj)],
                            reads=[wdr] + [("hT", hi, j, t // 4) for j in range(nj)])
                    xs = xtm[:, t, f * 512:(f + 1) * 512]
                    if gate is None and g == 0:
                        P.op("vector", lambda e, xs=xs, po=po: e.scalar_tensor_tensor(out=xs, in0=xs, scalar=ALPHA, in1=po[:], op0=ALU.mult, op1=ALU.add),
                             reads=[por, ("xtm", t)], writes=[("xtm", t)])
                    elif gate is None:
                        P.op("vector", lambda e, xs=xs, po=po: e.tensor_tensor(out=xs, in0=xs, in1=po[:], op=ALU.add),
                             reads=[por, ("xtm", t)], writes=[("xtm", t)])
                    else:
                        ga, gr = gate(t)
                        P.op("vector", lambda e, xs=xs, po=po, ga=ga: e.scalar_tensor_tensor(out=xs, in0=po[:], scalar=ga, in1=xs, op0=ALU.mult, op1=ALU.add),
                             reads=[por, ("xtm", t), gr], writes=[("xtm", t)])

    def moe(self, li):
        P = self.P
        xtm = self.xtm
        bc = self.dr["bc"]
        U = self.cm[:, CM_U:CM_U + 128]
        with ExitStack() as st0:
            gates = self.T(st0, "gates", [128, NT, 8])
            maskt = self.T(st0, "maskt", [128, NT, 8])
            posm = self.T(st0, "posm", [128, NT, 8])
            with ExitStack() as st:
                wr = self.T(st, "wrT", [128, 8, D])
                junk = self.T(st, "junk", [128, D])
                lg = self.T(st, "lg", [128, NT, 8])
                rs = self.T(st, "rs", [128, NT, 32])
                br = self.T(st, "br", [128, 8])
                o0 = BC_LAYOUT["moe_w_routerT"][0] + li * 8 * D
                P.dma("sync", lambda e: e.dma_start(out=wr[:], in_=bc[:, o0:o0 + 8 * D].rearrange("p (e d) -> p e d", e=8)), writes=["wrT"])
                o1 = BC_LAYOUT["moe_b_router"][0] + li * 8
                P.dma("sync", lambda e: e.dma_start(out=br[:], in_=bc[:, o1:o1 + 8]), writes=["br"])
                for t in range(NT):
                    for ex in range(8):
                        P.op("vector", lambda e, t=t, ex=ex: e.scalar_tensor_tensor(out=junk[:], in0=xtm[:, t, :], scalar=1.0, in1=wr[:, ex, :], op0=ALU.mult, op1=ALU.mult,
                                                                                   accum_out=lg[:, t, ex:ex + 1]),
                             reads=[("xtm", t), "wrT"], writes=["junk", ("lg", t)])
                    r = rs[:, t, :]
                    lt = lg[:, t, :]
                    R = [("lg", t)]
                    P.op("vector", lambda e, lt=lt: e.tensor_tensor(out=lt, in0=lt, in1=br[:], op=ALU.add), reads=R + ["br"], writes=R)
                    P.op("vector", lambda e, lt=lt, r=r: e.max(out=r[:, 0:8], in_=lt), reads=R, writes=R)
                    P.op("vector", lambda e, lt=lt, t=t, r=r: e.tensor_scalar(out=maskt[:, t, :], in0=lt, scalar1=r[:, 1:2], scalar2=None, op0=ALU.is_ge), reads=R, writes=R + [("maskt", t)])
                    P.op("vector", lambda e, r=r: e.tensor_scalar(out=r[:, 16:17], in0=r[:, 0:1], scalar1=-1.0, scalar2=None, op0=ALU.mult), reads=R, writes=R)
                    P.op("scalar", lambda e, lt=lt, r=r: e.activation(out=r[:, 24:32], in_=lt, func=AF.Exp, bias=r[:, 16:17], scale=1.0), reads=R, writes=R)
                    P.op("vector", lambda e, r=r, t=t: e.tensor_tensor(out=r[:, 24:32], in0=r[:, 24:32], in1=maskt[:, t, :], op=ALU.mult), reads=R + [("maskt", t)], writes=R)
                    P.op("vector", lambda e, r=r: e.reduce_sum(out=r[:, 17:18], in_=r[:, 24:32], axis=mybir.AxisListType.X), reads=R, writes=R)
                    P.op("vector", lambda e, r=r: e.reciprocal(out=r[:, 18:19], in_=r[:, 17:18]), reads=R, writes=R)
                    P.op("vector", lambda e, r=r, t=t: e.tensor_scalar(out=gates[:, t, :], in0=r[:, 24:32], scalar1=r[:, 18:19], scalar2=None, op0=ALU.mult),
                         reads=R, writes=[("gates", t)])
                for t in range(NT):
                    ps, pr = self.psum()
                    pairs = [(self.ones_f[:], maskt[:, j, :]) for j in range(t)] + [(U, maskt[:, t, :])]
                    self.mm(ps[:, 0:8], pr, pairs, reads=["ones_f", "cm"] + [("maskt", j) for j in range(t + 1)])
                    P.op("vector", lambda e, ps=ps, t=t: e.tensor_tensor(out=posm[:, t, :], in0=ps[:, 0:8], in1=maskt[:, t, :], op=ALU.mult), reads=[pr, ("maskt", t)], writes=[("posm", t)])
                    P.op("vector", lambda e, t=t: e.tensor_scalar(out=posm[:, t, :], in0=posm[:, t, :], scalar1=-1.0, scalar2=None, op0=ALU.add), reads=[("posm", t)], writes=[("posm", t)])
            P.barrier()
            xtb = self.xb[:, :, :].rearrange("p c s -> p (c s)").rearrange("p (t d) -> p t d", t=NT)
            for t in range(NT):
                if t % 2 == 0:
                    P.op("scalar", lambda e, t=t: e.copy(out=xtb[:, t, :], in_=xtm[:, t, :]), reads=[("xtm", t)], writes=[("xtb", t)])
                else:
                    P.op("vector", lambda e, t=t: e.tensor_copy(out=xtb[:, t, :], in_=xtm[:, t, :]), reads=[("xtm", t)], writes=[("xtb", t)])
            self.scale_x()
            with ExitStack() as st:
                selbuf = self.T(st, "selbuf", [128, NT * CAP], BF16)
                Sel = selbuf[:, :].rearrange("p (t c) -> p t c", t=NT)
                SelT = selbuf[:, :].rearrange("p (s t) -> p s t", s=NS)
                XgT = self.T(st, "XgT", [128, KC, CAP], BF16)
                Yb = XgT[:, :, :].rearrange("p c s -> p (c s)").rearrange("p (s d) -> p s d", s=NS)
                hT = self.T(st, "hTr", [128, 4, CAP], BF16)
                Yacc = self.T(st, "Yacc", [128, NS, D])
                sgl = [self.T(st, "sgr%d" % i, [128, 512]) for i in range(2)]
                iota = self.T(st, "iota", [128, CAP])
                slotid = self.T(st, "slotid", [128, 8])
                dgl = [self.T(st, "dgr%d" % i, [128, 128]) for i in range(2)]
                P.dma("sync", lambda e: e.dma_start(out=iota[:], in_=self.dr["cm"][:, CM_IOTA:CM_IOTA + CAP]), writes=["iota"])
                P.dma("sync", lambda e: e.dma_start(out=slotid[:], in_=self.dr["cm"][:, CM_SLOT:CM_SLOT + 8]), writes=["slotid"])
                blocks = [(0, 512), (512, CAP - 512)]
                ecnt = 0
                for ex in range(8):
                    Wgu = self.dr["moe_w_gu"][li, ex]
                    Wd = self.dr["moe_w_down"][li, ex]
                    F = 3584
                    for t in range(NT):
                        P.op("vector", lambda e, t=t, ex=ex: e.tensor_scalar(out=Sel[:, t, :], in0=iota[:], scalar1=posm[:, t, ex:ex + 1], scalar2=None, op0=ALU.is_equal),
                             reads=["iota", ("posm", t), "selbuf"], writes=["selbuf"])
                    for c in range(KC):
                        for (b0, bn) in blocks:
                            ps, pr = self.psum()
                            self.mm(ps[:, 0:bn], pr, [(xtb[:, t, c * 128:(c + 1) * 128], Sel[:, t, b0:b0 + bn]) for t in range(NT)],
                                    reads=["selbuf"] + [("xtb", t) for t in range(NT)])
                            ecnt += 1
                            if ecnt % 2 == 0:
                                P.op("scalar", lambda e, ps=ps, c=c, b0=b0, bn=bn: e.copy(out=XgT[:, c, b0:b0 + bn], in_=ps[:, 0:bn]), reads=[pr, "XgT"], writes=["XgT"])
                            else:
                                P.op("vector", lambda e, ps=ps, c=c, b0=b0, bn=bn: e.tensor_copy(out=XgT[:, c, b0:b0 + bn], in_=ps[:, 0:bn]), reads=[pr, "XgT"], writes=["XgT"])
                    ng = F // 512
                    for g in range(ng):
                        wg, wgr = self.wload(self.wview(Wgu, 0, 8, g * 512, 512), 8, 512)
                        wu, wur = self.wload(self.wview(Wgu, 0, 8, F + g * 512, 512), 8, 512)
                        wd, wdr = self.wload(self.wview(Wd, g * 512, 4, 0, 1024), 4, 1024)
                        for j in range(4):
                            for (b0, bn) in blocks:
                                pg, pgr = self.psum()
                                self.mm(pg[:, 0:bn], pgr, [(wg[:, k, j * 128:(j + 1) * 128], XgT[:, k, b0:b0 + bn]) for k in range(KC)], reads=[wgr, "XgT"])
                                pu, pur = self.psum()
                                self.mm(pu[:, 0:bn], pur, [(wu[:, k, j * 128:(j + 1) * 128], XgT[:, k, b0:b0 + bn]) for k in range(KC)], reads=[wur, "XgT"])
                                si = self.rot("sgr", 2)
                                sg = sgl[si]
                                P.op("scalar", lambda e, sg=sg, pg=pg, bn=bn: e.activation(out=sg[:, 0:bn], in_=pg[:, 0:bn], func=AF.Silu), reads=[pgr], writes=[("sgr", si)])
                                P.op("vector", lambda e, sg=sg, pu=pu, j=j, b0=b0, bn=bn: e.tensor_tensor(out=hT[:, j, b0:b0 + bn], in0=sg[:, 0:bn], in1=pu[:, 0:bn], op=ALU.mult),
                                     reads=[("sgr", si), pur], writes=[("hTr", j)])
                        for s_ in range(NS):
                            for f in range(2):
                                po, por = self.psum()
                                self.mm(po[:, :], por, [(hT[:, j, s_ * 128:(s_ + 1) * 128], wd[:, j, f * 512:(f + 1) * 512]) for j in range(4)],
                                        reads=[wdr] + [("hTr", j) for j in range(4)])
                                ys = Yacc[:, s_, f * 512:(f + 1) * 512]
                                if g == 0:
                                    P.op("vector", lambda e, ys=ys, po=po: e.tensor_copy(out=ys, in_=po[:]), reads=[por, ("Yacc", s_)], writes=[("Yacc", s_)])
                                else:
                                    P.op("vector", lambda e, ys=ys, po=po: e.tensor_tensor(out=ys, in0=ys, in1=po[:], op=ALU.add), reads=[por, ("Yacc", s_)], writes=[("Yacc", s_)])
                    for s_ in range(NS):
                        P.op("scalar", lambda e, s_=s_: e.copy(out=Yb[:, s_, :], in_=Yacc[:, s_, :]), reads=[("Yacc", s_), "XgT"], writes=["XgT"])
                    for tb in range(NB):
                        ps, pr = self.psum()
                        for j in range(4):
                            t = tb * 4 + j
                            di = self.rot("dgr", 2)
                            dg = dgl[di]
                            P.op("gpsimd", lambda e, dg=dg, t=t, ex=ex: e.tensor_scalar(out=dg[:], in0=self.ident, scalar1=posm[:, t, ex:ex + 1], scalar2=None, op0=ALU.mult),
                                 reads=["cm", ("posm", t), ("dgr", di)], writes=[("dgr", di)])
                            P.op("tensor", lambda e, ps=ps, dg=dg, j=j: e.matmul(ps[:, j * 128:(j + 1) * 128], lhsT=self.ones_f[:], rhs=dg[:], start=True, stop=True),
                                 reads=["ones_f", ("dgr", di)], writes=[pr])
                        for s_ in range(NS):
                            P.op("vector", lambda e, ps=ps, s_=s_, tb=tb: e.tensor_scalar(out=SelT[:, s_, tb * 512:(tb + 1) * 512], in0=ps[:, :], scalar1=slotid[:, s_:s_ + 1], scalar2=None, op0=ALU.is_equal),
                                 reads=[pr, "slotid", "selbuf"], writes=["selbuf"])
                    for t in range(NT):
                        for f in range(2):
                            po, por = self.psum()
                            self.mm(po[:, :], por, [(SelT[:, s_, t * 128:(t + 1) * 128], Yb[:, s_, f * 512:(f + 1) * 512]) for s_ in range(NS)], reads=["selbuf", "XgT"])
                            xs = xtm[:, t, f * 512:(f + 1) * 512]
                            P.op("vector", lambda e, xs=xs, po=po, t=t, ex=ex: e.scalar_tensor_tensor(out=xs, in0=po[:], scalar=gates[:, t, ex:ex + 1], in1=xs, op0=ALU.mult, op1=ALU.add),
                                 reads=[por, ("xtm", t), ("gates", t)], writes=[("xtm", t)])
            P.barrier()

    def ple(self, l):
        P = self.P
        xb, xtm = self.xb, self.xtm
        with ExitStack() as st:
            ptm = self.T(st, "ptm", [128, NT, 256])
            pT = self.T(st, "pT", [128, 2, S], BF16)
            tmp = [self.T(st, "pletmp%d" % i, [128, 512]) for i in range(3)]
            psrc = self.dr["p"][l].rearrange("(t p) f -> p t f", p=128)
            P.dma("sync", lambda e: e.dma_start(out=ptm[:], in_=psrc), writes=["ptm"])
            for tb in range(NB):
                for c in range(2):
                    ps, pr = self.psum()

                    def fn(e, ps=ps, tb=tb, c=c):
                        ins = None
                        for j in range(4):
                            ins = e.transpose(out=ps[:, j * 128:(j + 1) * 128], in_=ptm[:, tb * 4 + j, c * 128:(c + 1) * 128], identity=self.ident)
                        return ins
                    P.op("tensor", fn, reads=["ptm", "cm"], writes=[pr])
                    dst = pT[:, c, tb * 512:(tb + 1) * 512]
                    P.op("scalar", lambda e, ps=ps, dst=dst: e.copy(out=dst, in_=ps[:]), reads=[pr], writes=[("pT", c, tb)])
            wp, wpr = self.wload(self.wview(self.dr["ple_w"][l], 0, 2, 0, 1024), 2, 1024)
            for f in range(2):
                wg, wgr = self.wload(self.wview(self.dr["ple_gate_w"][l], 0, 8, f * 512, 512), 8, 512)
                for t in range(NT):
                    pg, pgr = self.psum()
                    self.mm(pg[:, :], pgr, [(xb[:, k, t * 128:(t + 1) * 128], wg[:, k, :]) for k in range(KC)], reads=[wgr] + self.xb_res(t // 4))
                    pp, ppr = self.psum()
                    self.mm(pp[:, :], ppr, [(pT[:, c, t * 128:(t + 1) * 128], wp[:, c, f * 512:(f + 1) * 512]) for c in range(2)],
                            reads=[wpr, ("pT", 0, t // 4), ("pT", 1, t // 4)])
                    ti = self.rot("pletmp", 3)
                    tm = tmp[ti]
                    P.op("scalar", lambda e, tm=tm, pg=pg: e.activation(out=tm[:], in_=pg[:], func=AF.Sigmoid), reads=[pgr], writes=[("pletmp", ti)])
                    P.op("vector", lambda e, tm=tm, pp=pp: e.tensor_tensor(out=tm[:], in0=tm[:], in1=pp[:], op=ALU.mult), reads=[("pletmp", ti), ppr], writes=[("pletmp", ti)])
                    xs = xtm[:, t, f * 512:(f + 1) * 512]
                    P.op("vector", lambda e, xs=xs, tm=tm: e.tensor_tensor(out=xs, in0=xs, in1=tm[:], op=ALU.add), reads=[("pletmp", ti), ("xtm", t)], writes=[("xtm", t)])

    def fox(self):
        P = self.P
        xb, xtm = self.xb, self.xtm
        W = self.dr["fox_w_in"][0]
        bc = self.dr["bc"]
        self.n_rot = 4
        with ExitStack() as st:
            oT = self.T(st, "oT", [128, 2, S], BF16)
            self.ubig = self.T(st, "ubig", [128, 4, 512], BF16)
            P.dma("gpsimd", lambda e: e.dma_start(out=self.ubig[:], in_=self.dr["cm"][:, CM_UBIG:CM_UBIG + 2048].rearrange("p (j t) -> p j t", j=4)), writes=["ubig"])
            vpad = self.T(st, "vpad", [128, NT, 2, 128], BF16)
            qT = self.T(st, "qT", [128, 2, S], BF16)
            kT = self.T(st, "kT", [128, S], BF16)
            P.op("vector", lambda e: e.memset(qT[:], 0.0), writes=["qTz"])
            pT = [self.T(st, "pexp%d" % i, [128, 512], BF16) for i in range(4)]
            rec = [self.T(st, "rec%d" % i, [128, 512]) for i in range(2)]
            lf = self.T(st, "lf", [128, NT, 16])
            negc = self.T(st, "negc", [128, NT, 16])
            kap = self.T(st, "kap", [128, NB, 16])
            biasT = self.T(st, "biasT", [128, NB, NT, 16])
            bfb = self.T(st, "bfb", [128, 16])
            zt = [self.T(st, "zt%d" % i, [128, 16]) for i in range(2)]
            oneslr = self.T(st, "oneslr", [128, 2, 128], BF16)
            o0 = BC_LAYOUT["fox_b_f"][0]
            P.dma("sync", lambda e: e.dma_start(out=bfb[:], in_=bc[:, o0:o0 + 16]), writes=["bfb"])
            P.op("vector", lambda e: e.memset(vpad[:], 0.0), writes=["vpad"])
            P.op("vector", lambda e: e.memset(oneslr[:], 0.0), writes=["oneslr"])
            P.op("vector", lambda e: e.memset(oneslr[:, 0, 0:64], 1.0), reads=["oneslr"], writes=["oneslr"])
            P.op("vector", lambda e: e.memset(oneslr[:, 1, 64:128], 1.0), reads=["oneslr"], writes=["oneslr"])
            wf, wfr = self.wload_narrow(st, "foxwf", W, 3072, 16)
            for t in range(NT):
                ps, pr = self.psum()
                self.mm(ps[:, 0:16], pr, [(xb[:, k, t * 128:(t + 1) * 128], wf[:, k, :]) for k in range(KC)], reads=[wfr] + self.xb_res(t // 4))
                z = zt[t % 2]
                zr = ("zt", t % 2)
                P.op("vector", lambda e, z=z, ps=ps: e.tensor_tensor(out=z[:], in0=ps[:, 0:16], in1=bfb[:], op=ALU.add), reads=[pr, "bfb"], writes=[zr])
                P.op("scalar", lambda e, z=z: e.activation(out=z[:], in_=z[:], func=AF.Exp, scale=-1.0), reads=[zr], writes=[zr])
                P.op("scalar", lambda e, z=z, t=t: e.activation(out=lf[:, t, :], in_=z[:], func=AF.Ln, bias=1.0, scale=1.0), reads=[zr], writes=[("lf", t)])
            STOP = int(os.environ.get("KB_STOP", "99"))
            if STOP <= 1:
                self.n_rot = 8
                P.barrier()
                return
            U = self.cm[:, CM_U:CM_U + 128]
            for t in range(NT):
                ps, pr = self.psum()
                pairs = [(self.ones_f[:], lf[:, j, :]) for j in range(t)] + [(U, lf[:, t, :])]
                self.mm(ps[:, 0:16], pr, pairs, reads=["ones_f", "cm"] + [("lf", j) for j in range(t + 1)])
                P.op("vector", lambda e, ps=ps, t=t: e.tensor_copy(out=negc[:, t, :], in_=ps[:, 0:16]), reads=[pr], writes=[("negc", t)])
            for n in range(NB):
                ps, pr = self.psum()
                pairs = [(self.ones_f[:], lf[:, j, :]) for j in range(4 * n + 2)]
                self.mm(ps[:, 0:16], pr, pairs, reads=["ones_f"] + [("lf", j) for j in range(4 * n + 2)])
                P.op("vector", lambda e, ps=ps, n=n: e.tensor_copy(out=kap[:, n, :], in_=ps[:, 0:16]), reads=[pr], writes=[("kap", n)])
                for j in range(4 * n + 4):
                    P.op("vector", lambda e, n=n, j=j: e.tensor_tensor(out=biasT[:, n, j, :], in0=negc[:, j, :], in1=kap[:, n, :], op=ALU.subtract),
                         reads=[("negc", j), ("kap", n)], writes=[("biasT", n, j)])
            if STOP <= 2:
                self.n_rot = 8
                P.barrier()
                return
            for c in range(KC):
                if STOP <= 5 and c >= 1:
                    break
                wq, wqr = self.wload(self.wview(W, 0, 8, c * 128, 128), 8, 128)
                wk, wkr = self.wload(self.wview(W, 0, 8, 1024 + c * 128, 128), 8, 128)
                wv, wvr = self.wload(self.wview(W, 0, 8, 2048 + c * 128, 128), 8, 128)
                for n in range(NB):
                    ps, pr = self.psum()
                    self.mm(ps[:, :], pr, [(wq[:, k, :], xb[:, k, n * 512:(n + 1) * 512]) for k in range(KC)], reads=[wqr] + self.xb_res(n))
                    P.op("vector", lambda e, ps=ps, n=n: e.tensor_scalar(out=qT[0:64, 0, n * 512:(n + 1) * 512], in0=ps[0:64, :], scalar1=0.125, scalar2=None, op0=ALU.mult), reads=[pr, "qTz"], writes=[("qT", n, 0)])
                    P.op("vector", lambda e, ps=ps, n=n: e.tensor_scalar(out=qT[64:128, 1, n * 512:(n + 1) * 512], in0=ps[64:128, :], scalar1=0.125, scalar2=None, op0=ALU.mult), reads=[pr, "qTz"], writes=[("qT", n, 1)])
                    ps, pr = self.psum()
                    self.mm(ps[:, :], pr, [(wk[:, k, :], xb[:, k, n * 512:(n + 1) * 512]) for k in range(KC)], reads=[wkr] + self.xb_res(n))
                    P.op("vector", lambda e, ps=ps, n=n: e.tensor_copy(out=kT[:, n * 512:(n + 1) * 512], in_=ps[:]), reads=[pr], writes=[("kT", n)])
                for t in range(NT):
                    ps, pr = self.psum()
                    self.mm(ps[:, 0:128], pr, [(xb[:, k, t * 128:(t + 1) * 128], wv[:, k, :]) for k in range(KC)], reads=[wvr] + self.xb_res(t // 4))
                    P.op("vector", lambda e, ps=ps, t=t: e.tensor_copy(out=vpad[:, t, 0, 0:64], in_=ps[:, 0:64]), reads=[pr, "vpad"], writes=[("vpad", t, 0)])
                    P.op("vector", lambda e, ps=ps, t=t: e.tensor_copy(out=vpad[:, t, 1, 64:128], in_=ps[:, 64:128]), reads=[pr, "vpad"], writes=[("vpad", t, 1)])
                if STOP <= 3:
                    break
                for n in range(NB):
                    if STOP <= 4 and n >= 1:
                        break
                    pi = self.rot("foxacc", 2)
                    num, numr = self.ps[4 + 2 * pi], ("ps", 4 + 2 * pi)
                    den, denr = self.ps[5 + 2 * pi], ("ps", 5 + 2 * pi)
                    nkt = 4 * n + 4
                    total = 2 * nkt
                    steps = [(hh, j) for hh in range(2) for j in range(nkt)]

                    def emit_S(k, n=n, c=c):
                        hh, j = steps[k]
                        sp, spr = self.psum()
                        self.mm(sp[:, :], spr, [(kT[:, j * 128:(j + 1) * 128], qT[:, hh, n * 512:(n + 1) * 512])], reads=[("kT", j // 4), ("qT", n, hh), "qTz"])
                        ei = self.rot("pexp", 4)
                        pe = pT[ei]
                        per = ("pexp", ei)
                        bcol = biasT[:, n, j, 2 * c + hh:2 * c + hh + 1]
                        P.op("scalar", lambda e, pe=pe, sp=sp, bcol=bcol: e.activation(out=pe[:], in_=sp[:], func=AF.Exp, bias=bcol, scale=1.0),
                             reads=[spr, ("biasT", n, j)], writes=[per])
                        if j >= 4 * n:
                            P.op("vector", lambda e, pe=pe, jj=j - 4 * n: e.tensor_tensor(out=pe[:], in0=pe[:], in1=self.ubig[:, jj, :], op=ALU.mult),
                                 reads=[per, "ubig"], writes=[per])
                        return pe, per
                    LA = 2
                    q_ = [emit_S(k) for k in range(min(LA, total))]
                    for idx in range(total):
                        if idx + LA < total:
                            q_.append(emit_S(idx + LA))
                        pe, per = q_[idx]
                        hh, j = steps[idx]
                        first, last = (idx == 0), (idx == total - 1)

                        def fn(e, num=num, den=den, pe=pe, j=j, hh=hh, first=first, last=last):
                            e.matmul(num[:, :], lhsT=vpad[:, j, hh, :], rhs=pe[:], start=first, stop=last)
                            return e.matmul(den[:, :], lhsT=oneslr[:, hh, :], rhs=pe[:], start=first, stop=last)
                        P.op("tensor", fn, reads=[per, ("vpad", j, hh), "vpad", "oneslr"], writes=[numr, denr])
                    rc = rec[pi]
                    P.op("vector", lambda e, rc=rc, den=den: e.reciprocal(out=rc[:], in_=den[:]), reads=[denr], writes=[("rec", pi)])
                    P.op("vector", lambda e, rc=rc, num=num, c=c, n=n: e.tensor_tensor(out=oT[:, c % 2, n * 512:(n + 1) * 512], in0=num[:], in1=rc[:], op=ALU.mult),
                         reads=[numr, ("rec", pi)], writes=[("oT", c % 2, n)])
                if c % 2 == 1:
                    self.out_proj_acc(oT, lambda t: [("oT", 0, t // 4), ("oT", 1, t // 4)], self.dr["fox_w_out"][0], c - 1, first=(c == 1), nk=2)
            self.n_rot = 8
        P.barrier()

    def lru(self):
        P = self.P
        xb = self.xb
        W = self.dr["lru_w_in"][0]
        Wo = self.dr["lru_w_out"][0]
        col = self.dr["col"]
        B4 = lambda nm: [(nm, n) for n in range(NB)]
        with ExitStack() as st:
            cv = self.T(st, "lrucol", [128, 80])
            sp8 = self.T(st, "sp8", [128, 10])
            recp = self.T(st, "recp", [128, 3 + S])
            u = self.T(st, "lru_u", [128, S])
            ub = self.T(st, "lru_ub", [128, S], BF16)
            ra = self.T(st, "lru_ra", [128, S])
            ib = self.T(st, "lru_ib", [128, S])
            y2 = self.T(st, "lru_y", [128, 2, S], BF16)
            wab = self.T(st, "lru_wab", [128, 2, 128], BF16)
            o0 = COL_LAYOUT["lru_conv_w"][0]
            P.dma("sync", lambda e: e.dma_start(out=cv[:], in_=col[:, o0:o0 + 80]), writes=["lrucol"])
            P.op("scalar", lambda e: e.activation(out=sp8[:], in_=cv[:, 70:80], func=AF.Exp, scale=-1.0), reads=["lrucol"], writes=["sp8"])
            P.op("scalar", lambda e: e.activation(out=sp8[:], in_=sp8[:], func=AF.Ln, bias=1.0, scale=1.0), reads=["sp8"], writes=["sp8"])
            P.op("vector", lambda e: e.tensor_scalar(out=sp8[:], in0=sp8[:], scalar1=-8.0, scalar2=None, op0=ALU.mult), reads=["sp8"], writes=["sp8"])
            P.op("vector", lambda e: e.memset(recp[:, 0:3], 0.0), writes=["recpad"])
            for cc in range(10):
                y = y2[:, cc % 2, :]
                yk = "y%d" % (cc % 2)
                wg, wgr = self.wload(self.wview(W, 0, 8, cc * 128, 128), 8, 128)
                wr_, wrr = self.wload(self.wview(W, 0, 8, 1280 + cc * 128, 128), 8, 128)
                P.dma("gpsimd", lambda e, cc=cc: e.dma_start(out=wab[:, 0, :], in_=self.dr["lru_w_a"][0, cc]), writes=[("wab", 0)])
                P.dma("gpsimd", lambda e, cc=cc: e.dma_start(out=wab[:, 1, :], in_=self.dr["lru_w_x"][0, cc]), writes=[("wab", 1)])
                for n in range(NB):
                    bs = slice(n * 512, (n + 1) * 512)
                    ps, pr = self.psum()
                    self.mm(ps[:, :], pr, [(wg[:, k, :], xb[:, k, bs]) for k in range(KC)], reads=[wgr] + self.xb_res(n))
                    P.op("scalar", lambda e, ps=ps, bs=bs, y=y: e.activation(out=y[:, bs], in_=ps[:], func=AF.Gelu_apprx_tanh), reads=[pr], writes=[(yk, n)])
                    ps, pr = self.psum()
                    self.mm(ps[:, :], pr, [(wr_[:, k, :], xb[:, k, bs]) for k in range(KC)], reads=[wrr] + self.xb_res(n))
                    P.op("vector", lambda e, ps=ps, n=n: e.tensor_copy(out=recp[:, 3 + n * 512:3 + (n + 1) * 512], in_=ps[:]), reads=[pr], writes=[("recp", n)])
                cw = lambda k: cv[:, cc * 4 + k:cc * 4 + k + 1]
                cb = cv[:, 40 + cc:41 + cc]
                P.op("vector", lambda e, c0=cw(0), cb=cb: e.tensor_scalar(out=u[:], in0=recp[:, 0:S], scalar1=c0, scalar2=cb, op0=ALU.mult, op1=ALU.add),
                     reads=B4("recp") + ["recpad", "lrucol"], writes=["u"])
                for k in range(1, 4):
                    P.op("vector", lambda e, k=k, ck=cw(k): e.scalar_tensor_tensor(out=u[:], in0=recp[:, k:k + S], scalar=ck, in1=u[:], op0=ALU.mult, op1=ALU.add),
                         reads=B4("recp") + ["recpad", "lrucol", "u"], writes=["u"])
                P.op("scalar", lambda e: e.copy(out=ub[:], in_=u[:]), reads=["u"], writes=["ub"])
                for n in range(NB):
                    bs = slice(n * 512, (n + 1) * 512)
                    ps, pr = self.psum()
                    self.mm(ps[:, :], pr, [(wab[:, 0, :], ub[:, bs])], reads=[("wab", 0), "ub"])
                    P.op("scalar", lambda e, ps=ps, bs=bs, b=cv[:, 50 + cc:51 + cc]: e.activation(out=ra[:, bs], in_=ps[:], func=AF.Sigmoid, bias=b, scale=1.0),
                         reads=[pr, "lrucol"], writes=[("ra", n)])
                    ps, pr = self.psum()
                    self.mm(ps[:, :], pr, [(wab[:, 1, :], ub[:, bs])], reads=[("wab", 1), "ub"])
                    P.op("scalar", lambda e, ps=ps, bs=bs, b=cv[:, 60 + cc:61 + cc]: e.activation(out=ib[:, bs], in_=ps[:], func=AF.Sigmoid, bias=b, scale=1.0),
                         reads=[pr, "lrucol"], writes=[("ib", n)])
                P.op("scalar", lambda e, sc=sp8[:, cc:cc + 1]: e.activation(out=ra[:], in_=ra[:], func=AF.Exp, scale=sc), reads=B4("ra") + ["sp8"], writes=B4("ra"))
                P.op("vector", lambda e: e.tensor_tensor(out=ib[:], in0=ib[:], in1=u[:], op=ALU.mult), reads=B4("ib") + ["u"], writes=B4("ib"))
                P.op("vector", lambda e: e.tensor_tensor(out=u[:], in0=ra[:], in1=ra[:], op=ALU.mult), reads=B4("ra") + ["u"], writes=["u"])
                P.op("scalar", lambda e: e.activation(out=u[:], in_=u[:], func=AF.Sqrt, bias=1.0, scale=-1.0), reads=["u"], writes=["u"])
                P.op("vector", lambda e: e.tensor_tensor(out=ib[:], in0=ib[:], in1=u[:], op=ALU.mult), reads=B4("ib") + ["u"], writes=B4("ib"))
                P.op("vector", lambda e: e.tensor_tensor_scan(out=recp[:, 3:3 + S], data0=ra[:], data1=ib[:], initial=0.0, op0=ALU.mult, op1=ALU.add),
                     reads=B4("ra") + B4("ib") + B4("recp"), writes=B4("recp"))
                P.op("vector", lambda e, y=y: e.tensor_tensor(out=y, in0=y, in1=recp[:, 3:3 + S], op=ALU.mult), reads=B4(yk) + B4("recp"), writes=B4(yk))
                if cc % 2 == 1:
                    self.out_proj_acc(y2, lambda t: [("y0", t // 4), ("y1", t // 4)], Wo, cc - 1, first=(cc == 1), nk=2)
        P.barrier()

    def conformer(self):
        P = self.P
        xb, xtm = self.xb, self.xtm
        W = self.dr["cv_w_in"][0]
        Wo = self.dr["cv_w_out"][0]
        col = self.dr["col"]
        bc = self.dr["bc"]
        B4 = lambda nm: [(nm, n) for n in range(NB)]
        with ExitStack() as st:
            cvc = self.T(st, "cvcol", [128, 288])
            hpad = self.T(st, "hpad", [128, 30 + S], BF16)
            dgk = self.T(st, "dgk", [128, 31, 128], BF16)
            sgt = [self.T(st, "cvsg%d" % i, [128, 512]) for i in range(2)]
            sq = [self.T(st, "cvsq%d" % i, [128, 512], BF16) for i in range(2)]
            convb = self.T(st, "convb", [128, KC, S], BF16)
            mean = self.T(st, "cvmean", [128, 512])
            rstd = self.T(st, "cvrstd", [128, 512])
            bo = self.T(st, "cvbo", [128, D])
            o0 = COL_LAYOUT["cv_b_in"][0]
            P.dma("sync", lambda e: e.dma_start(out=cvc[:], in_=col[:, o0:o0 + 288]), writes=["cvcol"])
            o1 = BC_LAYOUT["cv_b_out"][0]
            P.dma("sync", lambda e: e.dma_start(out=bo[:], in_=bc[:, o1:o1 + D]), writes=["cvbo"])
            P.op("vector", lambda e: e.memset(hpad[:, 0:30], 0.0), writes=["hpadz"])
            for cc in range(KC):
                wv, wvr = self.wload(self.wview(W, 0, 8, cc * 128, 128), 8, 128)
                wg, wgr = self.wload(self.wview(W, 0, 8, 1024 + cc * 128, 128), 8, 128)
                for n in range(NB):
                    bs = slice(n * 512, (n + 1) * 512)
                    pv, pvr = self.psum()
                    self.mm(pv[:, :], pvr, [(wv[:, k, :], xb[:, k, bs]) for k in range(KC)], reads=[wvr] + self.xb_res(n))
                    pg, pgr = self.psum()
                    self.mm(pg[:, :], pgr, [(wg[:, k, :], xb[:, k, bs]) for k in range(KC)], reads=[wgr] + self.xb_res(n))
                    si = self.rot("cvsg", 2)
                    sg = sgt[si]
                    P.op("scalar", lambda e, sg=sg, pg=pg, b=cvc[:, 8 + cc:9 + cc]: e.activation(out=sg[:], in_=pg[:], func=AF.Sigmoid, bias=b, scale=1.0),
                         reads=[pgr, "cvcol"], writes=[("cvsg", si)])
                    P.op("vector", lambda e, sg=sg, pv=pv, n=n, b=cvc[:, cc:cc + 1]: e.scalar_tensor_tensor(out=hpad[:, 30 + n * 512:30 + (n + 1) * 512], in0=pv[:], scalar=b, in1=sg[:], op0=ALU.add, op1=ALU.mult),
                         reads=[pvr, ("cvsg", si), "cvcol"], writes=[("hpad", n)])
                dw = lambda k: cvc[:, 16 + cc * 31 + k:16 + cc * 31 + k + 1]
                db = cvc[:, 264 + cc:265 + cc]
                for k in range(31):
                    eng = "vector" if k % 2 == 0 else "gpsimd"
                    P.op(eng, lambda e, k=k, dk=dw(k): e.tensor_scalar(out=dgk[:, k, :], in0=self.ident, scalar1=dk, scalar2=None, op0=ALU.mult),
                         reads=["cm", "cvcol", ("dgk", k)], writes=[("dgk", k)])
                for n in range(NB):
                    ps, pr = self.psum()
                    self.mm(ps[:, :], pr, [(dgk[:, k, :], hpad[:, k + n * 512:k + (n + 1) * 512]) for k in range(31)],
                            reads=[("dgk", k) for k in range(31)] + B4("hpad") + ["hpadz"])
                    P.op("vector", lambda e, ps=ps, n=n, cc=cc, db=db: e.tensor_scalar(out=convb[:, cc, n * 512:(n + 1) * 512], in0=ps[:], scalar1=db, scalar2=None, op0=ALU.add),
                         reads=[pr, "cvcol"], writes=[("convb", cc, n)])
            for n in range(NB):
                bs = slice(n * 512, (n + 1) * 512)
                psum_, psr = self.psum()
                self.mm(psum_[:, :], psr, [(self.ones_b[:], convb[:, cc, bs]) for cc in range(KC)], reads=["ones_b"] + [("convb", cc, n) for cc in range(KC)])
                psq, pqr = self.psum()
                for cc in range(KC):
                    qi = self.rot("cvsq", 2)
                    P.op("scalar", lambda e, qi=qi, cc=cc, bs=bs: e.activation(out=sq[qi][:], in_=convb[:, cc, bs], func=AF.Square), reads=[("convb", cc, n)], writes=[("cvsq", qi)])
                    P.op("tensor", lambda e, qi=qi, cc=cc, psq=psq: e.matmul(psq[:, :], lhsT=self.ones_b[:], rhs=sq[qi][:], start=(cc == 0), stop=(cc == KC - 1)),
                         reads=["ones_b", ("cvsq", qi)], writes=[pqr])
                P.op("scalar", lambda e, psum_=psum_: e.activation(out=mean[:], in_=psum_[:], func=AF.Copy, scale=1.0 / D), reads=[psr], writes=["cvmean"])
                P.op("vector", lambda e: e.tensor_tensor(out=rstd[:], in0=mean[:], in1=mean[:], op=ALU.mult), reads=["cvmean"], writes=["cvrstd"])
                P.op("vector", lambda e, psq=psq: e.scalar_tensor_tensor(out=rstd[:], in0=psq[:], scalar=1.0 / D, in1=rstd[:], op0=ALU.mult, op1=ALU.subtract),
                     reads=[pqr, "cvrstd"], writes=["cvrstd"])
                P.op("scalar", lambda e: e.activation(out=rstd[:], in_=rstd[:], func=AF.Sqrt, bias=LN_EPS, scale=1.0), reads=["cvrstd"], writes=["cvrstd"])
                P.op("vector", lambda e: e.reciprocal(out=rstd[:], in_=rstd[:]), reads=["cvrstd"], writes=["cvrstd"])
                for cc in range(KC):
                    si = self.rot("cvsg", 2)
                    tm = sgt[si]
                    P.op("vector", lambda e, tm=tm, cc=cc, bs=bs: e.tensor_tensor(out=tm[:], in0=convb[:, cc, bs], in1=mean[:], op=ALU.subtract),
                         reads=[("convb", cc, n), "cvmean"], writes=[("cvsg", si)])
                    P.op("vector", lambda e, tm=tm: e.tensor_tensor(out=tm[:], in0=tm[:], in1=rstd[:], op=ALU.mult), reads=[("cvsg", si), "cvrstd"], writes=[("cvsg", si)])
                    P.op("scalar", lambda e, tm=tm, cc=cc, bs=bs: e.activation(out=convb[:, cc, bs], in_=tm[:], func=AF.Silu, bias=cvc[:, 280 + cc:281 + cc], scale=cvc[:, 272 + cc:273 + cc]),
                         reads=[("cvsg", si), "cvcol"], writes=[("convb", cc, n)])
            self.out_proj_full(convb, KC, lambda t: [("convb", cc, t // 4) for cc in range(KC)], Wo)
            for t in range(NT):
                xt = xtm[:, t, :]
                P.op("gpsimd", lambda e, xt=xt: e.tensor_tensor(out=xt, in0=xt, in1=bo[:], op=ALU.add), reads=[("xtm", t), "cvbo"], writes=[("xtm", t)])
        P.barrier()

    def gdn(self):
        P = self.P
        xb, xtm = self.xb, self.xtm
        W = self.dr["gdn_w_in"][0]
        Wo = self.dr["gdn_w_out"][0]
        col = self.dr["col"]
        bc = self.dr["bc"]
        cm = self.cm
        ident = self.ident
        U = cm[:, CM_U:CM_U + 128]
        mlow = cm[:, CM_MLOW:CM_MLOW + 128]
        mup = cm[:, CM_MUP:CM_MUP + 128]
        strict = cm[:, CM_STRICT:CM_STRICT + 128]
        ones_f = self.ones_f
        B4 = lambda nm: [(nm, n) for n in range(NB)]
        with ExitStack() as st:
            T = lambda nm, shp, dt=F32: self.T(st, "g_" + nm, shp, dt)
            gcol = T("col", [128, 96])
            hb = T("hb", [128, 144])
            beta = T("beta", [128, NT, 8])
            gt = T("gt", [128, NT, 8])
            gc = T("gc", [128, NT, 8])
            ngc = T("ngc", [128, NT, 8])
            egc = T("egc", [128, NT, 8])
            bexp = T("bexp", [128, NT, 8])
            glast = T("glast", [128, NT, 8])
            kendc = T("kendc", [128, NT, 8])
            eglast = T("eglast", [128, NT, 8])
            nea = T("nea", [128, 8])
            cpad = T("cpad", [128, 3 + S])
            cf = T("cf", [128, S])
            otm = cf[:, :].rearrange("p (t d) -> p t d", t=NT)
            qT = T("qT", [128, S])
            kT = T("kT", [128, S])
            vtm = T("vtm", [128, NT, 128], BF16)
            ktm = T("ktm", [128, NT, 128], BF16)
            ohT = T("ohT", [128, S], BF16)
            ssr = T("ssr", [128, 512])
            sqf = T("sqf", [128, 512])
            Sst = T("S", [128, 128])
            mats = {}
            for nm in ["dg", "ndg", "t1", "Dl", "L", "t2", "Du", "R0", "R1", "P0", "P1", "Y0", "Y1",
                       "vb", "kbg", "kend", "nwT", "vnew", "tmpo", "sz", "on"]:
                mats[nm] = T("m_" + nm, [128, 128])
            self.n_slots = 2
            self.slot_rr = 0
            l1a = self.slots[2][:, :].bitcast(F32)
            l1b = self.slots[3][:, :].bitcast(F32)
            lane1 = {}
            for q_, nm in enumerate(["dg", "ndg", "t1", "Dl", "L", "t2", "Du", "R0", "R1", "P0", "P1", "Y0", "Y1"]):
                src_ = l1a if q_ < 8 else l1b
                q2 = q_ % 8
                lane1[nm] = src_[:, q2 * 128:(q2 + 1) * 128]
            TTs = l1b[:, 5 * 128:9 * 128].rearrange("p (a b d) -> p a b d", a=2, b=2)
            ATs = l1b[:, 9 * 128:13 * 128].rearrange("p (a b d) -> p a b d", a=2, b=2)
            o0 = COL_LAYOUT["gdn_conv_w"][0]
            P.dma("sync", lambda e: e.dma_start(out=gcol[:], in_=col[:, o0:o0 + 96]), writes=["gcol"])
            o1 = BC_LAYOUT["gdn_a_log"][0]
            P.dma("sync", lambda e: e.dma_start(out=hb[:], in_=bc[:, o1:o1 + 144]), writes=["hb"])
            P.op("scalar", lambda e: e.activation(out=nea[:], in_=hb[:, 0:8], func=AF.Exp), reads=["hb"], writes=["nea"])
            P.op("vector", lambda e: e.tensor_scalar(out=nea[:], in0=nea[:], scalar1=-1.0, scalar2=None, op0=ALU.mult), reads=["nea"], writes=["nea"])
            P.op("vector", lambda e: e.memset(cpad[:, 0:3], 0.0), writes=["cpadz"])
            wba, wbar = self.wload_narrow(st, "gdnwba", W, 4096, 16)
            for t in range(NT):
                ps, pr = self.psum()
                self.mm(ps[:, 0:16], pr, [(xb[:, k, t * 128:(t + 1) * 128], wba[:, k, :]) for k in range(KC)], reads=[wbar] + self.xb_res(t // 4))
                R = [("gsc", t)]
                P.op("vector", lambda e, ps=ps, t=t: e.tensor_copy(out=beta[:, t, :], in_=ps[:, 0:8]), reads=[pr], writes=R)
                P.op("vector", lambda e, ps=ps, t=t: e.tensor_tensor(out=gt[:, t, :], in0=ps[:, 8:16], in1=hb[:, 8:16], op=ALU.add), reads=[pr, "hb"] + R, writes=R)
                P.op("scalar", lambda e, t=t: e.activation(out=beta[:, t, :], in_=beta[:, t, :], func=AF.Sigmoid), reads=R, writes=R)
                P.op("scalar", lambda e, t=t: e.activation(out=gt[:, t, :], in_=gt[:, t, :], func=AF.Exp), reads=R, writes=R)
                P.op("scalar", lambda e, t=t: e.activation(out=gt[:, t, :], in_=gt[:, t, :], func=AF.Ln, bias=1.0, scale=1.0), reads=R, writes=R)
                P.op("vector", lambda e, t=t: e.tensor_tensor(out=gt[:, t, :], in0=gt[:, t, :], in1=nea[:], op=ALU.mult), reads=R + ["nea"], writes=R)
                ps, pr = self.psum()
                self.mm(ps[:, 0:8], pr, [(U, gt[:, t, :])], reads=R + ["cm"])
                ps2, pr2 = self.psum()
                self.mm(ps2[:, 0:8], pr2, [(ones_f[:], gt[:, t, :])], reads=R + ["ones_f"])
                P.op("vector", lambda e, ps=ps, t=t: e.tensor_copy(out=gc[:, t, :], in_=ps[:, 0:8]), reads=[pr] + R, writes=R)
                P.op("vector", lambda e, ps2=ps2, t=t: e.tensor_copy(out=glast[:, t, :], in_=ps2[:, 0:8]), reads=[pr2] + R, writes=R)
                P.op("vector", lambda e, t=t: e.tensor_scalar(out=ngc[:, t, :], in0=gc[:, t, :], scalar1=-1.0, scalar2=None, op0=ALU.mult), reads=R, writes=R)
                P.op("scalar", lambda e, t=t: e.activation(out=egc[:, t, :], in_=gc[:, t, :], func=AF.Exp), reads=R, writes=R)
                P.op("vector", lambda e, t=t: e.tensor_tensor(out=bexp[:, t, :], in0=egc[:, t, :], in1=beta[:, t, :], op=ALU.mult), reads=R, writes=R)
                P.op("vector", lambda e, t=t: e.tensor_tensor(out=kendc[:, t, :], in0=glast[:, t, :], in1=gc[:, t, :], op=ALU.subtract), reads=R, writes=R)
                P.op("scalar", lambda e, t=t: e.activation(out=kendc[:, t, :], in_=kendc[:, t, :], func=AF.Exp), reads=R, writes=R)
                P.op("scalar", lambda e, t=t: e.activation(out=eglast[:, t, :], in_=glast[:, t, :], func=AF.Exp), reads=R, writes=R)
            for h in range(8):
                for which in range(3):
                    wv, wvr = self.wload(self.wview(W, 0, 8, which * 1024 + h * 128, 128), 8, 128)
                    for n in range(NB):
                        bs = slice(n * 512, (n + 1) * 512)
                        ps, pr = self.psum()
                        self.mm(ps[:, :], pr, [(wv[:, k, :], xb[:, k, bs]) for k in range(KC)], reads=[wvr] + self.xb_res(n))
                        P.op("scalar", lambda e, ps=ps, n=n: e.copy(out=cpad[:, 3 + n * 512:3 + (n + 1) * 512], in_=ps[:]), reads=[pr], writes=[("cpad", n)])
                    cidx = which * 8 + h
                    cw = lambda k: gcol[:, cidx * 4 + k:cidx * 4 + k + 1]
                    P.op("vector", lambda e, c0=cw(0): e.tensor_scalar(out=cf[:], in0=cpad[:, 0:S], scalar1=c0, scalar2=None, op0=ALU.mult),
                         reads=B4("cpad") + ["cpadz", "gcol"], writes=["cf"])
                    for k in range(1, 4):
                        P.op("vector", lambda e, k=k, ck=cw(k): e.scalar_tensor_tensor(out=cf[:], in0=cpad[:, k:k + S], scalar=ck, in1=cf[:], op0=ALU.mult, op1=ALU.add),
                             reads=B4("cpad") + ["cpadz", "gcol", "cf"], writes=["cf"])
                    P.op("scalar", lambda e: e.activation(out=cf[:], in_=cf[:], func=AF.Silu), reads=["cf"], writes=["cf"])
                    if which < 2:
                        dstT = qT if which == 0 else kT
                        dres = "qT" if which == 0 else "kT"
                        for n in range(NB):
                            bs = slice(n * 512, (n + 1) * 512)
                            P.op("vector", lambda e, bs=bs: e.tensor_tensor(out=sqf[:], in0=cf[:, bs], in1=cf[:, bs], op=ALU.mult), reads=["cf"], writes=["sqf"])
                            ps, pr = self.psum()
                            self.mm(ps[:, :], pr, [(ones_f[:], sqf[:])], reads=["ones_f", "sqf"])
                            P.op("scalar", lambda e, ps=ps: e.activation(out=ssr[:], in_=ps[:], func=AF.Sqrt, bias=1e-6, scale=1.0), reads=[pr], writes=["ssr"])
                            P.op("vector", lambda e: e.reciprocal(out=ssr[:], in_=ssr[:]), reads=["ssr"], writes=["ssr"])
                            sc = (128.0 ** -0.5) if which == 0 else 1.0
                            P.op("vector", lambda e, bs=bs, dstT=dstT, sc=sc: e.scalar_tensor_tensor(out=dstT[:, bs], in0=cf[:, bs], scalar=sc, in1=ssr[:], op0=ALU.mult, op1=ALU.mult),
                                 reads=["cf", "ssr"], writes=[(dres, n)])
                        if which == 1:
                            for tb in range(NB):
                                ps, pr = self.psum()

                                def fn(e, ps=ps, tb=tb):
                                    ins = None
                                    for j in range(4):
                                        ins = e.transpose(out=ps[:, j * 128:(j + 1) * 128], in_=kT[:, (tb * 4 + j) * 128:(tb * 4 + j + 1) * 128], identity=ident)
                                    return ins
                                P.op("tensor", fn, reads=[("kT", tb), "cm"], writes=[pr])
                                P.op("vector", lambda e, ps=ps, tb=tb: e.tensor_copy(out=ktm[:, tb * 4:(tb + 1) * 4, :], in_=ps[:].rearrange("p (j d) -> p j d", j=4)),
                                     reads=[pr], writes=[("ktm", tb)])
                    else:
                        for tb in range(NB):
                            ps, pr = self.psum()

                            def fn(e, ps=ps, tb=tb):
                                ins = None
                                for j in range(4):
                                    ins = e.transpose(out=ps[:, j * 128:(j + 1) * 128], in_=cf[:, (tb * 4 + j) * 128:(tb * 4 + j + 1) * 128], identity=ident)
                                return ins
                            P.op("tensor", fn, reads=["cf", "cm"], writes=[pr])
                            P.op("vector", lambda e, ps=ps, tb=tb: e.tensor_copy(out=vtm[:, tb * 4:(tb + 1) * 4, :], in_=ps[:].rearrange("p (j d) -> p j d", j=4)),
                                 reads=[pr], writes=[("vtm", tb)])
                P.op("vector", lambda e: e.memset(Sst[:], 0.0), reads=["S"], writes=["S"])
                PREP = ["dg", "ndg", "t1", "Dl", "L", "t2", "Du", "R0", "R1", "P0", "P1", "Y0", "Y1"]

                def lane_mat(lane, nm):
                    if lane == 0:
                        return mats[nm], nm
                    return lane1[nm], nm + "_l1"

                def make_ops(i, lane, par, banks):
                    prep, tail = [], []
                    bctr = [0]

                    def psb():
                        b = banks[bctr[0] % len(banks)]
                        bctr[0] += 1
                        return self.ps[b], ("ps", b)
                    tctr = [0]

                    def pst():
                        b = (6, 7)[tctr[0] % 2]
                        tctr[0] += 1
                        return self.ps[b], ("ps", b)

                    def MM(lst, out_ap, out_res, pairs, reads):
                        pairs = list(pairs)

                        def fn(e):
                            n = len(pairs)
                            ins = None
                            for q_, (l_, r_) in enumerate(pairs):
                                ins = e.matmul(out_ap, lhsT=l_, rhs=r_, start=(q_ == 0), stop=(q_ == n - 1))
                            return ins
                        lst.append(("tensor", fn, list(reads), [out_res]))
                    cs = slice(i * 128, (i + 1) * 128)
                    SC = [("gsc", i)]
                    colh = lambda tl: tl[:, i, h:h + 1]
                    M = {}
                    RK = {}
                    for nm in PREP:
                        M[nm], RK[nm] = lane_mat(lane, nm)
                    TTp = TTs[:, par, lane, :]
                    ATp = ATs[:, par, lane, :]
                    TTr = ("TT", par, lane)
                    ATr = ("AT", par, lane)
                    A = lambda eng, fn, reads, writes: prep.append((eng, fn, list(reads), list(writes)))
                    psK, prK = psb()
                    MM(prep, psK[:, 0:128], prK, [(kT[:, cs], kT[:, cs])], [("kT", i // 4)])
                    psQ, prQ = psb()
                    MM(prep, psQ[:, 0:128], prQ, [(kT[:, cs], qT[:, cs])], [("kT", i // 4), ("qT", i // 4)])
                    A("vector", lambda e, a=colh(gc): e.tensor_scalar(out=M["dg"][:], in0=ident, scalar1=a, scalar2=None, op0=ALU.mult), SC + ["cm", RK["dg"]], [RK["dg"]])
                    A("scalar", lambda e, a=colh(ngc): e.activation(out=M["ndg"][:], in_=ident, func=AF.Copy, scale=a), SC + ["cm", RK["ndg"]], [RK["ndg"]])
                    psG, prG = psb()
                    MM(prep, psG[:, 0:128], prG, [(M["dg"][:], ones_f[:]), (ones_f[:], M["ndg"][:])], [RK["dg"], RK["ndg"], "ones_f"])
                    A("vector", lambda e, psG=psG: e.tensor_tensor(out=M["t1"][:], in0=psG[:, 0:128], in1=strict, op=ALU.add), [prG, "cm", RK["t1"]], [RK["t1"]])
                    A("scalar", lambda e: e.activation(out=M["Dl"][:], in_=M["t1"][:], func=AF.Exp), [RK["t1"], RK["Dl"]], [RK["Dl"]])
                    A("vector", lambda e, psK=psK, a=colh(beta): e.scalar_tensor_tensor(out=M["L"][:], in0=psK[:, 0:128], scalar=a, in1=M["Dl"][:], op0=ALU.mult, op1=ALU.mult),
                      [prK, RK["Dl"], RK["L"]] + SC, [RK["L"]])
                    A("vector", lambda e, psG=psG: e.scalar_tensor_tensor(out=M["t2"][:], in0=psG[:, 0:128], scalar=-1.0, in1=mup, op0=ALU.mult, op1=ALU.add), [prG, "cm", RK["t2"]], [RK["t2"]])
                    A("scalar", lambda e: e.activation(out=M["Du"][:], in_=M["t2"][:], func=AF.Exp), [RK["t2"], RK["Du"]], [RK["Du"]])
                    A("vector", lambda e, psQ=psQ: e.tensor_tensor(out=ATp, in0=psQ[:, 0:128], in1=M["Du"][:], op=ALU.mult), [prQ, RK["Du"], ATr], [ATr])
                    psM, prM = psb()
                    prep.append(("tensor", lambda e, psM=psM: e.transpose(out=psM[:, 0:128], in_=M["L"][:], identity=ident), [RK["L"], "cm"], [prM]))
                    A("vector", lambda e, psM=psM: e.tensor_copy(out=M["R0"][:], in_=psM[:, 0:128]), [prM, RK["R0"]], [RK["R0"]])
                    A("vector", lambda e, psM=psM: e.scalar_tensor_tensor(out=M["Y0"][:], in0=psM[:, 0:128], scalar=-1.0, in1=ident, op0=ALU.mult, op1=ALU.add), [prM, "cm", RK["Y0"]], [RK["Y0"]])
                    Pn, Rn, Yn = "L", "R0", "Y0"
                    for lev in range(1, 7):
                        Pnew, Rnew, Ynew = "P%d" % (lev % 2), "R%d" % (lev % 2), "Y%d" % (lev % 2)
                        psP, prP = psb()
                        MM(prep, psP[:, 0:128], prP, [(M[Rn][:], M[Pn][:])], [RK[Rn], RK[Pn]])
                        if lev < 6:
                            psR, prR = psb()
                            MM(prep, psR[:, 0:128], prR, [(M[Pn][:], M[Rn][:])], [RK[Rn], RK[Pn]])
                        A("scalar", lambda e, psP=psP, Pnew=Pnew: e.copy(out=M[Pnew][:], in_=psP[:, 0:128]), [prP, RK[Pnew]], [RK[Pnew]])
                        if lev < 6:
                            A("vector", lambda e, psR=psR, Rnew=Rnew: e.tensor_copy(out=M[Rnew][:], in_=psR[:, 0:128]), [prR, RK[Rnew]], [RK[Rnew]])
                        psY, prY = psb()
                        MM(prep, psY[:, 0:128], prY, [(M[Pnew][:], M[Yn][:])], [RK[Pnew], RK[Yn]])
                        if lev < 6:
                            A("vector", lambda e, psY=psY, Yn=Yn, Ynew=Ynew: e.tensor_tensor(out=M[Ynew][:], in0=psY[:, 0:128], in1=M[Yn][:], op=ALU.add), [prY, RK[Yn], RK[Ynew]], [RK[Ynew]])
                        else:
                            A("vector", lambda e, psY=psY, Yn=Yn: e.tensor_tensor(out=TTp, in0=psY[:, 0:128], in1=M[Yn][:], op=ALU.add), [prY, RK[Yn], TTr], [TTr])
                        Pn, Rn, Yn = Pnew, Rnew, Ynew
                    m = mats
                    B = lambda eng, fn, reads, writes: tail.append((eng, fn, list(reads), list(writes)))
                    B("vector", lambda e, a=colh(beta): e.tensor_scalar(out=m["vb"][:], in0=vtm[:, i, :], scalar1=a, scalar2=None, op0=ALU.mult), [("vtm", i // 4), "vb"] + SC, ["vb"])
                    B("vector", lambda e, a=colh(bexp): e.tensor_scalar(out=m["kbg"][:], in0=ktm[:, i, :], scalar1=a, scalar2=None, op0=ALU.mult), [("ktm", i // 4), "kbg"] + SC, ["kbg"])
                    B("gpsimd", lambda e, a=colh(kendc): e.tensor_scalar(out=m["kend"][:], in0=ktm[:, i, :], scalar1=a, scalar2=None, op0=ALU.mult), [("ktm", i // 4), "kend"] + SC, ["kend"])
                    psW, prW = pst()
                    MM(tail, psW[:, 0:128], prW, [(m["kbg"][:], TTp)], ["kbg", TTr])
                    B("scalar", lambda e, psW=psW: e.activation(out=m["nwT"][:], in_=psW[:, 0:128], func=AF.Copy, scale=-1.0), [prW, "nwT"], ["nwT"])
                    psV, prV = pst()
                    MM(tail, psV[:, 0:128], prV, [(TTp, m["vb"][:]), (m["nwT"][:], Sst[:])], [TTr, "vb", "nwT", "S"])
                    B("vector", lambda e, psV=psV: e.tensor_copy(out=m["vnew"][:], in_=psV[:, 0:128]), [prV, "vnew"], ["vnew"])
                    psA, prA = pst()
                    MM(tail, psA[:, 0:128], prA, [(qT[:, cs], Sst[:])], [("qT", i // 4), "S"])
                    B("scalar", lambda e, psA=psA, a=colh(egc): e.activation(out=m["tmpo"][:], in_=psA[:, 0:128], func=AF.Copy, scale=a), [prA, "tmpo"] + SC, ["tmpo"])
                    psB, prB = pst()
                    MM(tail, psB[:, 0:128], prB, [(ATp, m["vnew"][:])], [ATr, "vnew"])
                    B("vector", lambda e, psB=psB: e.tensor_tensor(out=otm[:, i, :], in0=psB[:, 0:128], in1=m["tmpo"][:], op=ALU.add), [prB, "tmpo"], [("otm", i), "cf"])
                    psS, prS = pst()
                    MM(tail, psS[:, 0:128], prS, [(m["kend"][:], m["vnew"][:])], ["kend", "vnew"])
                    B("vector", lambda e, psS=psS, a=colh(eglast): e.scalar_tensor_tensor(out=Sst[:], in0=Sst[:], scalar=a, in1=psS[:, 0:128], op0=ALU.mult, op1=ALU.add),
                      [prS, "S"] + SC, ["S"])
                    return prep, tail

                def emit_merged(streams):
                    n = max(len(s_) for s_ in streams) if streams else 0
                    for k in range(n):
                        for s_ in streams:
                            if k < len(s_):
                                eng, fn, reads, writes = s_[k]
                                P.op(eng, fn, reads=reads, writes=writes)
                pending_tail = []
                for pr_ in range(NT // 2):
                    par = pr_ % 2
                    p0, t0 = make_ops(2 * pr_, 0, par, (0, 1, 2))
                    p1, t1 = make_ops(2 * pr_ + 1, 1, par, (3, 4, 5))
                    emit_merged([p0, p1, pending_tail])
                    pending_tail = t0 + t1
                emit_merged([pending_tail])

                wz, wzr = self.wload(self.wview(W, 0, 8, 3072 + h * 128, 128), 8, 128)
                for tb in range(NB):
                    psT, prT = self.psum()
                    for j in range(4):
                        t = tb * 4 + j
                        psz, przr = self.psum()
                        self.mm(psz[:, 0:128], przr, [(xb[:, k, t * 128:(t + 1) * 128], wz[:, k, :]) for k in range(KC)], reads=[wzr] + self.xb_res(tb))
                        P.op("scalar", lambda e, psz=psz: e.activation(out=mats["sz"][:], in_=psz[:, 0:128], func=AF.Silu), reads=[przr, "sz"], writes=["sz"])
                        sm = self.small[:, self.rot("small", 8), :]
                        sres = ("small", (self._rot_small - 1) % 8)
                        P.op("vector", lambda e, t=t, sm=sm: e.scalar_tensor_tensor(out=mats["on"][:], in0=otm[:, t, :], scalar=1.0, in1=otm[:, t, :], op0=ALU.mult, op1=ALU.mult, accum_out=sm[:, 0:1]),
                             reads=[("otm", t), "cf", "on", sres], writes=["on", sres])
                        P.op("scalar", lambda e, sm=sm: e.activation(out=sm[:, 1:2], in_=sm[:, 0:1], func=AF.Sqrt, bias=1e-6, scale=1.0 / 128.0), reads=[sres], writes=[sres])
                        P.op("vector", lambda e, sm=sm: e.reciprocal(out=sm[:, 2:3], in_=sm[:, 1:2]), reads=[sres], writes=[sres])
                        P.op("vector", lambda e, t=t, sm=sm: e.scalar_tensor_tensor(out=mats["on"][:], in0=otm[:, t, :], scalar=sm[:, 2:3], in1=hb[:, 16:144], op0=ALU.mult, op1=ALU.mult),
                             reads=[("otm", t), "cf", sres, "hb", "on"], writes=["on"])
                        P.op("vector", lambda e: e.tensor_tensor(out=mats["on"][:], in0=mats["on"][:], in1=mats["sz"][:], op=ALU.mult), reads=["on", "sz"], writes=["on"])
                        P.op("tensor", lambda e, psT=psT, j=j: e.transpose(out=psT[:, j * 128:(j + 1) * 128], in_=mats["on"][:], identity=ident), reads=["on", "cm"], writes=[prT])
                    P.op("scalar", lambda e, psT=psT, tb=tb: e.copy(out=ohT[:, tb * 512:(tb + 1) * 512], in_=psT[:]), reads=[prT], writes=[("ohT", tb)])
                self.out_proj_acc(ohT, lambda t: [("ohT", t // 4)], Wo, h, first=(h == 0))
            self.n_slots = 4
        P.barrier()


def host_layout(inputs, b):
    m = {}
    m["x"] = np.ascontiguousarray(inputs["x"][b])
    m["p"] = np.ascontiguousarray(inputs["p"][:, b])
    for n in INPUT_SHAPES:
        if n not in ("x", "p"):
            m[n] = np.ascontiguousarray(inputs[n], dtype=np.float32)
    bc = np.zeros((128, BC_W), np.float32)
    for n, (o, w) in BC_LAYOUT.items():
        if n == "moe_w_routerT":
            v = np.transpose(np.asarray(inputs["moe_w_router"]), (0, 2, 1)).reshape(-1)
        else:
            v = np.asarray(inputs[n]).reshape(-1)
        bc[:, o:o + w] = v[None, :]
    m["bc"] = bc
    col = np.zeros((128, COL_W), np.float32)

    def put(n, arr):
        o, w = COL_LAYOUT[n]
        col[:, o:o + w] = arr.reshape(128, w)
    put("lru_conv_w", np.asarray(inputs["lru_conv_w"])[0].reshape(4, 10, 128).transpose(2, 1, 0))
    for n in ("lru_conv_b", "lru_b_a", "lru_b_x", "lru_lambda"):
        put(n, np.asarray(inputs[n])[0].reshape(10, 128).T)
    put("cv_b_in", np.asarray(inputs["cv_b_in"])[0].reshape(16, 128).T)
    put("cv_dw_w", np.asarray(inputs["cv_dw_w"])[0].reshape(31, 8, 128).transpose(2, 1, 0))
    for n in ("cv_dw_b", "cv_ln_g", "cv_ln_b"):
        put(n, np.asarray(inputs[n])[0].reshape(8, 128).T)
    put("gdn_conv_w", np.asarray(inputs["gdn_conv_w"])[0].reshape(4, 24, 128).transpose(2, 1, 0))
    m["col"] = col
    m["cm"] = host_consts()
    return m


_NC_CACHE = {}


def run(inputs, n_layers=DEPTH, mixers=(0, 1, 2, 3), ffns=(0, 1, 0, 1), cores=8):
    key = (n_layers, tuple(mixers), tuple(ffns))
    if key not in _NC_CACHE:
        _NC_CACHE[key] = KB(n_layers, mixers, ffns).build()
    nc = _NC_CACHE[key]
    in_maps = [host_layout(inputs, b) for b in range(cores)]
    if os.environ.get("KB_TRACE"):
        res = run_bass_kernel_spmd(nc, in_maps, core_ids=list(range(cores)), trace=True)
        print("EXEC_NS", res.exec_time_ns, flush=True)
    else:
        res = run_bass_kernel_spmd(nc, in_maps, core_ids=list(range(cores)))
    return np.stack([r["y"] for r in res.results], axis=0)


def kernel(**inputs):
    inputs = {k: np.asarray(v) for k, v in inputs.items()}
    return run(inputs).astype(np.float32)
```

```python
import os
import numpy as np
from contextlib import ExitStack
import concourse.bass as bass
import concourse.mybir as mybir
from concourse.bass_utils import run_bass_kernel_spmd

F32 = mybir.dt.float32
BF16 = mybir.dt.bfloat16
AF = mybir.ActivationFunctionType
ALU = mybir.AluOpType

ENGS = ("tensor", "vector", "scalar", "gpsimd", "sync")
EPOCH = 16000
N_DMA_SEMS = 24
NO_SAME_ENG_WAIT = bool(int(os.environ.get("KB_NOSAME", "0")))

S = 2048
D = 1024
NT = 16
NB = 4
KC = 8
DEPTH = 4
ALPHA = float((2 * DEPTH) ** 0.25)
LN_EPS = 1e-5
NEG = -30000.0


class Op:
    __slots__ = ("eng", "fn", "reads", "writes", "is_dma", "idx", "sem", "val", "deps", "prev_val", "barrier")

    def __init__(self, eng, fn, reads, writes, is_dma):
        self.eng = eng
        self.fn = fn
        self.reads = reads
        self.writes = writes
        self.is_dma = is_dma
        self.deps = ()
        self.barrier = False


class Prog:
    def __init__(self, nc):
        self.nc = nc
        self.ops = []

    def op(self, eng, fn, reads=(), writes=()):
        o = Op(eng, fn, tuple(reads), tuple(writes), False)
        self.ops.append(o)
        return o

    def dma(self, eng, fn, reads=(), writes=()):
        o = Op(eng, fn, tuple(reads), tuple(writes), True)
        self.ops.append(o)
        return o

    def barrier(self):
        o = Op(None, None, (), (), False)
        o.barrier = True
        self.ops.append(o)

    def emit(self, stack):
        nc = self.nc
        ops = self.ops
        last_w = {}
        readers = {}
        eng_count = {e: 0 for e in ENGS}
        cur_sem = {}
        n_dma = {e: 0 for e in ENGS}
        per_eng = {e: [] for e in ENGS}
        for o in ops:
            if o.barrier:
                snap = dict(cur_sem)
                for e in ENGS:
                    per_eng[e].append(("bar", snap))
                last_w = {}
                readers = {}
                continue
            deps = set()
            for r in o.reads:
                w = last_w.get(r)
                if w is not None:
                    deps.add(w)
            for r in o.writes:
                w = last_w.get(r)
                if w is not None:
                    deps.add(w)
                for rd in readers.get(r, ()):
                    deps.add(rd)
            deps.discard(o)
            o.deps = deps
            for r in o.reads:
                readers.setdefault(r, []).append(o)
            for r in o.writes:
                last_w[r] = o
                readers[r] = []
            if o.is_dma:
                nd = n_dma[o.eng]
                o.sem = ("dma_" + o.eng, nd % N_DMA_SEMS)
                o.val = 16 * (nd // N_DMA_SEMS + 1)
                o.prev_val = o.val - 16
                n_dma[o.eng] = nd + 1
            else:
                eng_count[o.eng] += 1
                s = eng_count[o.eng]
                o.sem = (o.eng, (s - 1) // EPOCH)
                o.val = (s - 1) % EPOCH + 1
            cur_sem[o.sem] = o.val
            per_eng[o.eng].append(o)
        sems = {}
        for o in ops:
            if not o.barrier and o.sem not in sems:
                sems[o.sem] = stack.enter_context(nc.semaphore("s_%s_%d" % o.sem))
        final = dict(cur_sem)

        def make_body(ename):
            def body(e):
                seen = {}

                def wait(s, v):
                    if seen.get(s, 0) >= v:
                        return
                    seen[s] = v
                    e.wait_ge(sems[s], v)

                for o in per_eng[ename]:
                    if isinstance(o, tuple):
                        for s, v in o[1].items():
                            wait(s, v)
                        continue
                    need = {}
                    for d in o.deps:
                        if (not d.is_dma) and d.eng == "tensor" and ename == "tensor" and not o.is_dma:
                            continue
                        if NO_SAME_ENG_WAIT and (not d.is_dma) and (not o.is_dma) and d.eng == ename and ename in ("vector", "scalar"):
                            continue
                        need[d.sem] = max(need.get(d.sem, 0), d.val)
                    if o.is_dma and o.prev_val > 0:
                        need[o.sem] = max(need.get(o.sem, 0), o.prev_val)
                    for s, v in need.items():
                        wait(s, v)
                    ins = o.fn(e)
                    ins.then_inc(sems[o.sem], 16 if o.is_dma else 1)
                if ename == "sync":
                    for s, v in final.items():
                        wait(s, v)
            return body

        with nc.Block() as block:
            for ename in ENGS:
                getattr(block, ename)(make_body(ename))


INPUT_SHAPES = {
    "x": (S, D), "p": (4, S, 256),
    "ple_w": (4, 256, 1024), "ple_gate_w": (4, 1024, 1024),
    "fox_w_in": (1, 1024, 3088), "fox_w_out": (1, 1024, 1024),
    "lru_w_in": (1, 1024, 2560), "lru_w_a": (1, 10, 128, 128), "lru_w_x": (1, 10, 128, 128),
    "lru_w_out": (1, 1280, 1024),
    "cv_w_in": (1, 1024, 2048), "cv_w_out": (1, 1024, 1024),
    "gdn_w_in": (1, 1024, 4112), "gdn_w_out": (1, 1024, 1024),
    "ffn_w_gu": (2, 1024, 5632), "ffn_w_down": (2, 2816, 1024),
    "moe_w_gu": (2, 8, 1024, 7168), "moe_w_down": (2, 8, 3584, 1024),
}

BC_LAYOUT = {}
_off = 0
for _n, _w in [("ln_mix_g", 4096), ("ln_mix_b", 4096), ("ln_ffn_g", 4096), ("ln_ffn_b", 4096),
               ("fox_b_f", 16), ("cv_b_out", 1024), ("gdn_a_log", 8), ("gdn_dt_bias", 8),
               ("gdn_norm_g", 128), ("moe_b_router", 16), ("moe_w_routerT", 2 * 8 * 1024)]:
    BC_LAYOUT[_n] = (_off, _w)
    _off += _w
BC_W = _off
COL_LAYOUT = {}
_off = 0
for _n, _w in [("lru_conv_w", 40), ("lru_conv_b", 10), ("lru_b_a", 10), ("lru_b_x", 10), ("lru_lambda", 10),
               ("cv_b_in", 16), ("cv_dw_w", 8 * 31), ("cv_dw_b", 8), ("cv_ln_g", 8), ("cv_ln_b", 8),
               ("gdn_conv_w", 24 * 4)]:
    COL_LAYOUT[_n] = (_off, _w)
    _off += _w
COL_W = _off
CM_IDENT, CM_U, CM_MLOW, CM_MUP, CM_STRICT, CM_UBIG = 0, 128, 256, 384, 512, 640
CM_W = 640 + 2048
CAP = 640
NS = CAP // 128
CM_IOTA = CM_W
CM_SLOT = CM_W + CAP
CM_W = CM_W + CAP + 8


def host_consts():
    r = np.arange(128)[:, None]
    c = np.arange(128)[None, :]
    cm = np.zeros((128, CM_W), np.float32)
    cm[:, CM_IDENT:CM_IDENT + 128] = (r == c)
    cm[:, CM_U:CM_U + 128] = (r <= c)
    cm[:, CM_MLOW:CM_MLOW + 128] = np.where(c <= r, 0.0, NEG)
    cm[:, CM_MUP:CM_MUP + 128] = np.where(r <= c, 0.0, NEG)
    cm[:, CM_STRICT:CM_STRICT + 128] = (c < r)
    t = np.arange(512)[None, :]
    for jj in range(4):
        cm[:, CM_UBIG + jj * 512:CM_UBIG + (jj + 1) * 512] = ((jj * 128 + r) <= t)
    cm[:, CM_IOTA:CM_IOTA + CAP] = np.arange(CAP)[None, :]
    for s_ in range(8):
        cm[:, CM_SLOT + s_] = np.arange(128) + 128 * s_
    return cm


class KB:
    def __init__(self, n_layers, mixers=(0, 1, 2, 3), ffns=(0, 1, 0, 1)):
        self.n_layers = n_layers
        self.mixers = mixers
        self.ffns = ffns
        self.nc = bass.Bass("TRN2", target_bir_lowering=False)
        nc = self.nc
        self.dr = {}
        for n, shp in INPUT_SHAPES.items():
            self.dr[n] = nc.dram_tensor(n, list(shp), F32, kind="ExternalInput").ap()
        self.dr["bc"] = nc.dram_tensor("bc", [128, BC_W], F32, kind="ExternalInput").ap()
        self.dr["col"] = nc.dram_tensor("col", [128, COL_W], F32, kind="ExternalInput").ap()
        self.dr["cm"] = nc.dram_tensor("cm", [128, CM_W], F32, kind="ExternalInput").ap()
        self.y = nc.dram_tensor("y", [S, D], F32, kind="ExternalOutput").ap()
        self.P = Prog(nc)
        self.ps_rr = 0
        self.slot_rr = 0
        self.uid = 0

    def T(self, st, name, shape, dt=F32):
        self.uid += 1
        return st.enter_context(self.nc.sbuf_tensor("sb%d_%s" % (self.uid, name), list(shape), dt))

    def psum(self):
        i = self.ps_rr % self.n_rot
        self.ps_rr += 1
        return self.ps[i], ("ps", i)

    def rot(self, key, n):
        v = getattr(self, "_rot_" + key, 0)
        setattr(self, "_rot_" + key, v + 1)
        return v % n

    def wload(self, src3, nk, ncol):
        i = self.slot_rr % self.n_slots
        self.slot_rr += 1
        sl = self.slots[i]
        dst = sl[:, 0:nk * ncol].rearrange("p (k c) -> p k c", k=nk)
        res = ("slot", i)
        self.P.dma("gpsimd", lambda e: e.dma_start(out=dst, in_=src3), writes=[res])
        return dst, res

    def wload_narrow(self, st, name, W, c0, ncol):
        stg = self.T(st, name + "_stg", [128, KC, ncol])
        wb = self.T(st, name + "_bf", [128, KC, ncol], BF16)
        src = W[:, c0:c0 + ncol].rearrange("(k p) c -> p k c", p=128)
        self.P.dma("sync", lambda e: e.dma_start(out=stg[:], in_=src), writes=[name + "_stg"])
        self.P.op("vector", lambda e: e.tensor_copy(out=wb[:], in_=stg[:]), reads=[name + "_stg"], writes=[name + "_bf"])
        return wb, name + "_bf"

    def wview(self, W, r0, nk, c0, ncol):
        return W[r0:r0 + nk * 128, c0:c0 + ncol].rearrange("(k p) c -> p k c", p=128)

    def mm(self, out_ap, out_res, pairs, reads):
        pairs = list(pairs)

        def fn(e):
            n = len(pairs)
            ins = None
            for i, (l, r) in enumerate(pairs):
                ins = e.matmul(out_ap, lhsT=l, rhs=r, start=(i == 0), stop=(i == n - 1))
            return ins
        self.P.op("tensor", fn, reads=reads, writes=[out_res])

    def xb_res(self, tb):
        return [("xb", c, tb) for c in range(KC)]

    def build(self):
        nc, P = self.nc, self.P
        with ExitStack() as st:
            self.st = st
            self.ps = [st.enter_context(nc.psum_tensor("ps%d" % i, [128, 512], F32)) for i in range(8)]
            self.n_rot = 8
            self.xtm = self.T(st, "xtm", [128, NT, D])
            self.xb = self.T(st, "xb", [128, KC, S], BF16)
            self.n_slots = 4
            self.slots = [self.T(st, "slot%d" % i, [128, 4096], BF16) for i in range(self.n_slots)]
            self.cm = self.T(st, "cm", [128, 640])
            self.ones_f = self.T(st, "ones_f", [128, 128])
            self.ones_b = self.T(st, "ones_b", [128, 128], BF16)
            self.lnw = self.T(st, "lnw", [128, 2, D])
            self.small = self.T(st, "small", [128, 8, 32])
            self.ln_st6 = self.T(st, "ln_st6", [128, NT, 12])
            self.ln_mv = self.T(st, "ln_mv", [128, NT, 2])
            self.ln_sd = self.T(st, "ln_sd", [128, 2, NT])
            self.ident = self.cm[:, CM_IDENT:CM_IDENT + 128]
            xtm = self.xtm
            P.dma("sync", lambda e: e.dma_start(out=self.cm[:], in_=self.dr["cm"][:, 0:640]), writes=["cm"])
            P.op("vector", lambda e: e.memset(self.ones_f[:], 1.0), writes=["ones_f"])
            P.op("vector", lambda e: e.memset(self.ones_b[:], 1.0), writes=["ones_b"])
            xsrc = self.dr["x"].rearrange("(t p) d -> p t d", p=128)
            for t in range(NT):
                P.dma("sync", lambda e, t=t: e.dma_start(out=xtm[:, t, :], in_=xsrc[:, t, :]), writes=[("xtm", t)])
            self.rebuild_xb()
            for l in range(self.n_layers):
                self.layer(l)
            for t in range(NT):
                ysrc = self.y.rearrange("(t p) d -> p t d", p=128)
                P.dma("sync", lambda e, t=t: e.dma_start(out=ysrc[:, t, :], in_=xtm[:, t, :]), reads=[("xtm", t)], writes=[("y", t)])
            P.emit(st)
        return nc

    def rebuild_xb(self):
        P = self.P
        xtm, xb = self.xtm, self.xb
        for tb in range(NB):
            for c in range(KC):
                ps, pr = self.psum()

                def fn(e, ps=ps, tb=tb, c=c):
                    ins = None
                    for j in range(4):
                        ins = e.transpose(out=ps[:, j * 128:(j + 1) * 128], in_=xtm[:, tb * 4 + j, c * 128:(c + 1) * 128], identity=self.ident)
                    return ins
                P.op("tensor", fn, reads=[("xtm", tb * 4 + j) for j in range(4)] + ["cm"], writes=[pr])
                dst = xb[:, c, tb * 512:(tb + 1) * 512]
                if (tb * KC + c) % 2 == 0:
                    P.op("scalar", lambda e, ps=ps, dst=dst: e.copy(out=dst, in_=ps[:]), reads=[pr], writes=[("xb", c, tb)])
                else:
                    P.op("vector", lambda e, ps=ps, dst=dst: e.tensor_copy(out=dst, in_=ps[:]), reads=[pr], writes=[("xb", c, tb)])

    def layer_norm(self, which):
        P = self.P
        xtm = self.xtm
        g = self.lnw[:, 0, :]
        b = self.lnw[:, 1, :]
        bc = self.dr["bc"]
        st6, mv, sd = self.ln_st6, self.ln_mv, self.ln_sd
        for i, n in enumerate([["ln_mix_g", "ln_mix_b"], ["ln_ffn_g", "ln_ffn_b"]][which]):
            o0 = BC_LAYOUT[n][0] + self.cur_layer * D
            P.dma("sync", lambda e, i=i, o0=o0: e.dma_start(out=self.lnw[:, i, :], in_=bc[:, o0:o0 + D]), reads=["lnw"], writes=["lnw"])
        for t in range(NT):
            xt = xtm[:, t, :]
            P.op("vector", lambda e, t=t, xt=xt: e.bn_stats(out=st6[:, t, 0:6], in_=xt[:, 0:512]), reads=[("xtm", t), ("lnst", t)], writes=[("lnst", t)])
            P.op("vector", lambda e, t=t, xt=xt: e.bn_stats(out=st6[:, t, 6:12], in_=xt[:, 512:1024]), reads=[("xtm", t), ("lnst", t)], writes=[("lnst", t)])
            P.op("vector", lambda e, t=t: e.bn_aggr(out=mv[:, t, :], in_=st6[:, t, :]), reads=[("lnst", t), "lnmv"], writes=["lnmv"])
        P.op("scalar", lambda e: e.activation(out=sd[:, 0, :], in_=mv[:, :, 1], func=AF.Sqrt, bias=LN_EPS, scale=1.0), reads=["lnmv", "lnsd"], writes=["lnsd"])
        P.op("vector", lambda e: e.reciprocal(out=sd[:, 1, :], in_=sd[:, 0, :]), reads=["lnsd"], writes=["lnsd"])
        for t in range(NT):
            xt = xtm[:, t, :]
            P.op("vector", lambda e, t=t, xt=xt: e.scalar_tensor_tensor(out=xt, in0=xt, scalar=mv[:, t, 0:1], in1=g, op0=ALU.subtract, op1=ALU.mult),
                 reads=[("xtm", t), "lnmv", "lnw"], writes=[("xtm", t)])
            P.op("scalar", lambda e, t=t, xt=xt: e.activation(out=xt, in_=xt, func=AF.Copy, scale=sd[:, 1, t:t + 1]), reads=[("xtm", t), "lnsd"], writes=[("xtm", t)])
            P.op("gpsimd", lambda e, xt=xt: e.tensor_tensor(out=xt, in0=xt, in1=b, op=ALU.add), reads=[("xtm", t), "lnw"], writes=[("xtm", t)])

    def out_proj_acc(self, yT, yres, W, cc, first=False, nk=1):
        P = self.P
        xtm = self.xtm
        for f in range(2):
            wv, wr = self.wload(self.wview(W, cc * 128, nk, f * 512, 512), nk, 512)
            for t in range(NT):
                ps, pr = self.psum()
                if nk == 1:
                    pairs = [(yT[:, t * 128:(t + 1) * 128], wv[:, 0, :])]
                else:
                    pairs = [(yT[:, k, t * 128:(t + 1) * 128], wv[:, k, :]) for k in range(nk)]
                self.mm(ps[:, :], pr, pairs, reads=[wr] + list(yres(t)))
                xs = xtm[:, t, f * 512:(f + 1) * 512]
                if first:
                    P.op("vector", lambda e, xs=xs, ps=ps: e.scalar_tensor_tensor(out=xs, in0=xs, scalar=ALPHA, in1=ps[:], op0=ALU.mult, op1=ALU.add),
                         reads=[pr, ("xtm", t)], writes=[("xtm", t)])
                else:
                    P.op("vector", lambda e, xs=xs, ps=ps: e.tensor_tensor(out=xs, in0=xs, in1=ps[:], op=ALU.add),
                         reads=[pr, ("xtm", t)], writes=[("xtm", t)])

    def out_proj_full(self, yT, nk, yres, W):
        P = self.P
        xtm = self.xtm
        for f in range(2):
            wv, wr = self.wload(self.wview(W, 0, nk, f * 512, 512), nk, 512)
            for t in range(NT):
                ps, pr = self.psum()
                self.mm(ps[:, :], pr, [(yT[:, k, t * 128:(t + 1) * 128], wv[:, k, :]) for k in range(nk)], reads=[wr] + list(yres(t)))
                xs = xtm[:, t, f * 512:(f + 1) * 512]
                P.op("vector", lambda e, xs=xs, ps=ps: e.scalar_tensor_tensor(out=xs, in0=xs, scalar=ALPHA, in1=ps[:], op0=ALU.mult, op1=ALU.add),
                     reads=[pr, ("xtm", t)], writes=[("xtm", t)])

    def layer(self, l):
        P = self.P
        self.cur_layer = l
        m = self.mixers[l]
        if m == 0:
            self.fox()
        elif m == 1:
            self.lru()
        elif m == 2:
            self.conformer()
        elif m == 3:
            self.gdn()
        if m >= 0:
            self.layer_norm(0)
            self.rebuild_xb()
        P.barrier()
        fk = self.ffns[l]
        if fk == 0:
            with ExitStack() as st:
                self.ffn_alloc(st)
                self.ffn(self.dr["ffn_w_gu"][l // 2], self.dr["ffn_w_down"][l // 2], 2816, None)
            self.layer_norm(1)
            self.rebuild_xb()
        elif fk == 2:
            self.scale_x()
            self.layer_norm(1)
            self.rebuild_xb()
        elif fk == 1:
            self.moe(l // 2)
            self.layer_norm(1)
            self.rebuild_xb()
        P.barrier()
        self.ple(l)
        if l != self.n_layers - 1:
            self.rebuild_xb()
        P.barrier()

    def scale_x(self):
        for t in range(NT):
            xt = self.xtm[:, t, :]
            self.P.op("gpsimd", lambda e, xt=xt: e.tensor_scalar(out=xt, in0=xt, scalar1=ALPHA, scalar2=None, op0=ALU.mult),
                      reads=[("xtm", t)], writes=[("xtm", t)])

    def ffn_alloc(self, st):
        self.hT = [self.T(st, "hT%d" % i, [128, 4, S], BF16) for i in range(2)]
        self.sg = [self.T(st, "sg%d" % i, [128, 512]) for i in range(3)]

    def ffn(self, Wgu, Wd, F, gate):
        P = self.P
        xb, xtm = self.xb, self.xtm
        ng = (F + 511) // 512
        for g in range(ng):
            hc = min(512, F - g * 512)
            nj = hc // 128
            wg, wgr = self.wload(self.wview(Wgu, 0, 8, g * 512, hc), 8, hc)
            wu, wur = self.wload(self.wview(Wgu, 0, 8, F + g * 512, hc), 8, hc)
            wd, wdr = self.wload(self.wview(Wd, g * 512, nj, 0, 1024), nj, 1024)
            hi = self.rot("hT", 2)
            hT = self.hT[hi]
            for j in range(nj):
                for n in range(NB):
                    pg, pgr = self.psum()
                    self.mm(pg[:, :], pgr, [(wg[:, k, j * 128:(j + 1) * 128], xb[:, k, n * 512:(n + 1) * 512]) for k in range(KC)],
                            reads=[wgr] + self.xb_res(n))
                    pu, pur = self.psum()
                    self.mm(pu[:, :], pur, [(wu[:, k, j * 128:(j + 1) * 128], xb[:, k, n * 512:(n + 1) * 512]) for k in range(KC)],
                            reads=[wur] + self.xb_res(n))
                    si = self.rot("sg", 3)
                    sg = self.sg[si]
                    P.op("scalar", lambda e, sg=sg, pg=pg: e.activation(out=sg[:], in_=pg[:], func=AF.Silu), reads=[pgr], writes=[("sg", si)])
                    hd = hT[:, j, n * 512:(n + 1) * 512]
                    P.op("vector", lambda e, hd=hd, sg=sg, pu=pu: e.tensor_tensor(out=hd, in0=sg[:], in1=pu[:], op=ALU.mult),
                         reads=[("sg", si), pur], writes=[("hT", hi, j, n)])
            for t in range(NT):
                for f in range(2):
                    po, por = self.psum()
                    self.mm(po[:, :], por, [(hT[:, j, t * 128:(t + 1) * 128], wd[:, j, f * 512:(f + 1) * 512]) for j in range(nj)],
                            reads=[wdr] + [("hT", hi, j, t // 4) for j in range(nj)])
                    xs = xtm[:, t, f * 512:(f + 1) * 512]
                    if gate is None and g == 0:
                        P.op("vector", lambda e, xs=xs, po=po: e.scalar_tensor_tensor(out=xs, in0=xs, scalar=ALPHA, in1=po[:], op0=ALU.mult, op1=ALU.add),
                             reads=[por, ("xtm", t)], writes=[("xtm", t)])
                    elif gate is None:
                        P.op("vector", lambda e, xs=xs, po=po: e.tensor_tensor(out=xs, in0=xs, in1=po[:], op=ALU.add),
                             reads=[por, ("xtm", t)], writes=[("xtm", t)])
                    else:
                        ga, gr = gate(t)
                        P.op("vector", lambda e, xs=xs, po=po, ga=ga: e.scalar_tensor_tensor(out=xs, in0=po[:], scalar=ga, in1=xs, op0=ALU.mult, op1=ALU.add),
                             reads=[por, ("xtm", t), gr], writes=[("xtm", t)])

    def moe(self, li):
        P = self.P
        xtm = self.xtm
        bc = self.dr["bc"]
        U = self.cm[:, CM_U:CM_U + 128]
        with ExitStack() as st0:
            gates = self.T(st0, "gates", [128, NT, 8])
            maskt = self.T(st0, "maskt", [128, NT, 8])
            posm = self.T(st0, "posm", [128, NT, 8])
            with ExitStack() as st:
                wr = self.T(st, "wrT", [128, 8, D])
                junk = self.T(st, "junk", [128, D])
                lg = self.T(st, "lg", [128, NT, 8])
                rs = self.T(st, "rs", [128, NT, 32])
                br = self.T(st, "br", [128, 8])
                o0 = BC_LAYOUT["moe_w_routerT"][0] + li * 8 * D
                P.dma("sync", lambda e: e.dma_start(out=wr[:], in_=bc[:, o0:o0 + 8 * D].rearrange("p (e d) -> p e d", e=8)), writes=["wrT"])
                o1 = BC_LAYOUT["moe_b_router"][0] + li * 8
                P.dma("sync", lambda e: e.dma_start(out=br[:], in_=bc[:, o1:o1 + 8]), writes=["br"])
                for t in range(NT):
                    for ex in range(8):
                        P.op("vector", lambda e, t=t, ex=ex: e.scalar_tensor_tensor(out=junk[:], in0=xtm[:, t, :], scalar=1.0, in1=wr[:, ex, :], op0=ALU.mult, op1=ALU.mult,
                                                                                   accum_out=lg[:, t, ex:ex + 1]),
                             reads=[("xtm", t), "wrT"], writes=["junk", ("lg", t)])
                    r = rs[:, t, :]
                    lt = lg[:, t, :]
                    R = [("lg", t)]
                    P.op("vector", lambda e, lt=lt: e.tensor_tensor(out=lt, in0=lt, in1=br[:], op=ALU.add), reads=R + ["br"], writes=R)
                    P.op("vector", lambda e, lt=lt, r=r: e.max(out=r[:, 0:8], in_=lt), reads=R, writes=R)
                    P.op("vector", lambda e, lt=lt, t=t, r=r: e.tensor_scalar(out=maskt[:, t, :], in0=lt, scalar1=r[:, 1:2], scalar2=None, op0=ALU.is_ge), reads=R, writes=R + [("maskt", t)])
                    P.op("vector", lambda e, r=r: e.tensor_scalar(out=r[:, 16:17], in0=r[:, 0:1], scalar1=-1.0, scalar2=None, op0=ALU.mult), reads=R, writes=R)
                    P.op("scalar", lambda e, lt=lt, r=r: e.activation(out=r[:, 24:32], in_=lt, func=AF.Exp, bias=r[:, 16:17], scale=1.0), reads=R, writes=R)
                    P.op("vector", lambda e, r=r, t=t: e.tensor_tensor(out=r[:, 24:32], in0=r[:, 24:32], in1=maskt[:, t, :], op=ALU.mult), reads=R + [("maskt", t)], writes=R)
                    P.op("vector", lambda e, r=r: e.reduce_sum(out=r[:, 17:18], in_=r[:, 24:32], axis=mybir.AxisListType.X), reads=R, writes=R)
                    P.op("vector", lambda e, r=r: e.reciprocal(out=r[:, 18:19], in_=r[:, 17:18]), reads=R, writes=R)
                    P.op("vector", lambda e, r=r, t=t: e.tensor_scalar(out=gates[:, t, :], in0=r[:, 24:32], scalar1=r[:, 18:19], scalar2=None, op0=ALU.mult),
                         reads=R, writes=[("gates", t)])
                for t in range(NT):
                    ps, pr = self.psum()
                    pairs = [(self.ones_f[:], maskt[:, j, :]) for j in range(t)] + [(U, maskt[:, t, :])]
                    self.mm(ps[:, 0:8], pr, pairs, reads=["ones_f", "cm"] + [("maskt", j) for j in range(t + 1)])
                    P.op("vector", lambda e, ps=ps, t=t: e.tensor_tensor(out=posm[:, t, :], in0=ps[:, 0:8], in1=maskt[:, t, :], op=ALU.mult), reads=[pr, ("maskt", t)], writes=[("posm", t)])
                    P.op("vector", lambda e, t=t: e.tensor_scalar(out=posm[:, t, :], in0=posm[:, t, :], scalar1=-1.0, scalar2=None, op0=ALU.add), reads=[("posm", t)], writes=[("posm", t)])
            P.barrier()
            xtb = self.xb[:, :, :].rearrange("p c s -> p (c s)").rearrange("p (t d) -> p t d", t=NT)
            for t in range(NT):
                if t % 2 == 0:
                    P.op("scalar", lambda e, t=t: e.copy(out=xtb[:, t, :], in_=xtm[:, t, :]), reads=[("xtm", t)], writes=[("xtb", t)])
                else:
                    P.op("vector", lambda e, t=t: e.tensor_copy(out=xtb[:, t, :], in_=xtm[:, t, :]), reads=[("xtm", t)], writes=[("xtb", t)])
            self.scale_x()
            with ExitStack() as st:
                selbuf = self.T(st, "selbuf", [128, NT * CAP], BF16)
                Sel = selbuf[:, :].rearrange("p (t c) -> p t c", t=NT)
                SelT = selbuf[:, :].rearrange("p (s t) -> p s t", s=NS)
                XgT = self.T(st, "XgT", [128, KC, CAP], BF16)
                Yb = XgT[:, :, :].rearrange("p c s -> p (c s)").rearrange("p (s d) -> p s d", s=NS)
                hT = self.T(st, "hTr", [128, 4, CAP], BF16)
                Yacc = self.T(st, "Yacc", [128, NS, D])
                sgl = [self.T(st, "sgr%d" % i, [128, 512]) for i in range(2)]
                iota = self.T(st, "iota", [128, CAP])
                slotid = self.T(st, "slotid", [128, 8])
                dgl = [self.T(st, "dgr%d" % i, [128, 128]) for i in range(2)]
                P.dma("sync", lambda e: e.dma_start(out=iota[:], in_=self.dr["cm"][:, CM_IOTA:CM_IOTA + CAP]), writes=["iota"])
                P.dma("sync", lambda e: e.dma_start(out=slotid[:], in_=self.dr["cm"][:, CM_SLOT:CM_SLOT + 8]), writes=["slotid"])
                blocks = [(0, 512), (512, CAP - 512)]
                ecnt = 0
                for ex in range(8):
                    Wgu = self.dr["moe_w_gu"][li, ex]
                    Wd = self.dr["moe_w_down"][li, ex]
                    F = 3584
                    for t in range(NT):
                        P.op("vector", lambda e, t=t, ex=ex: e.tensor_scalar(out=Sel[:, t, :], in0=iota[:], scalar1=posm[:, t, ex:ex + 1], scalar2=None, op0=ALU.is_equal),
                             reads=["iota", ("posm", t), "selbuf"], writes=["selbuf"])
                    for c in range(KC):
                        for (b0, bn) in blocks:
                            ps, pr = self.psum()
                            self.mm(ps[:, 0:bn], pr, [(xtb[:, t, c * 128:(c + 1) * 128], Sel[:, t, b0:b0 + bn]) for t in range(NT)],
                                    reads=["selbuf"] + [("xtb", t) for t in range(NT)])
                            ecnt += 1
                            if ecnt % 2 == 0:
                                P.op("scalar", lambda e, ps=ps, c=c, b0=b0, bn=bn: e.copy(out=XgT[:, c, b0:b0 + bn], in_=ps[:, 0:bn]), reads=[pr, "XgT"], writes=["XgT"])
                            else:
                                P.op("vector", lambda e, ps=ps, c=c, b0=b0, bn=bn: e.tensor_copy(out=XgT[:, c, b0:b0 + bn], in_=ps[:, 0:bn]), reads=[pr, "XgT"], writes=["XgT"])
                    ng = F // 512
                    for g in range(ng):
                        wg, wgr = self.wload(self.wview(Wgu, 0, 8, g * 512, 512), 8, 512)
                        wu, wur = self.wload(self.wview(Wgu, 0, 8, F + g * 512, 512), 8, 512)
                        wd, wdr = self.wload(self.wview(Wd, g * 512, 4, 0, 1024), 4, 1024)
                        for j in range(4):
                            for (b0, bn) in blocks:
                                pg, pgr = self.psum()
                                self.mm(pg[:, 0:bn], pgr, [(wg[:, k, j * 128:(j + 1) * 128], XgT[:, k, b0:b0 + bn]) for k in range(KC)], reads=[wgr, "XgT"])
                                pu, pur = self.psum()
                                self.mm(pu[:, 0:bn], pur, [(wu[:, k, j * 128:(j + 1) * 128], XgT[:, k, b0:b0 + bn]) for k in range(KC)], reads=[wur, "XgT"])
                                si = self.rot("sgr", 2)
                                sg = sgl[si]
                                P.op("scalar", lambda e, sg=sg, pg=pg, bn=bn: e.activation(out=sg[:, 0:bn], in_=pg[:, 0:bn], func=AF.Silu), reads=[pgr], writes=[("sgr", si)])
                                P.op("vector", lambda e, sg=sg, pu=pu, j=j, b0=b0, bn=bn: e.tensor_tensor(out=hT[:, j, b0:b0 + bn], in0=sg[:, 0:bn], in1=pu[:, 0:bn], op=ALU.mult),
                                     reads=[("sgr", si), pur], writes=[("hTr", j)])
                        for s_ in range(NS):
                            for f in range(2):
                                po, por = self.psum()
                                self.mm(po[:, :], por, [(hT[:, j, s_ * 128:(s_ + 1) * 128], wd[:, j, f * 512:(f + 1) * 512]) for j in range(4)],
                                        reads=[wdr] + [("hTr", j) for j in range(4)])
                                ys = Yacc[:, s_, f * 512:(f + 1) * 512]
                                if g == 0:
                                    P.op("vector", lambda e, ys=ys, po=po: e.tensor_copy(out=ys, in_=po[:]), reads=[por, ("Yacc", s_)], writes=[("Yacc", s_)])
                                else:
                                    P.op("vector", lambda e, ys=ys, po=po: e.tensor_tensor(out=ys, in0=ys, in1=po[:], op=ALU.add), reads=[por, ("Yacc", s_)], writes=[("Yacc", s_)])
                    for s_ in range(NS):
                        P.op("scalar", lambda e, s_=s_: e.copy(out=Yb[:, s_, :], in_=Yacc[:, s_, :]), reads=[("Yacc", s_), "XgT"], writes=["XgT"])
                    for tb in range(NB):
                        ps, pr = self.psum()
                        for j in range(4):
                            t = tb * 4 + j
                            di = self.rot("dgr", 2)
                            dg = dgl[di]
                            P.op("scalar", lambda e, dg=dg, t=t, ex=ex: e.activation(out=dg[:], in_=self.ident, func=AF.Copy, scale=posm[:, t, ex:ex + 1]),
                                 reads=["cm", ("posm", t), ("dgr", di)], writes=[("dgr", di)])
                            P.op("tensor", lambda e, ps=ps, dg=dg, j=j: e.matmul(ps[:, j * 128:(j + 1) * 128], lhsT=self.ones_f[:], rhs=dg[:], start=True, stop=True),
                                 reads=["ones_f", ("dgr", di)], writes=[pr])
                        for s_ in range(NS):
                            P.op("vector", lambda e, ps=ps, s_=s_, tb=tb: e.tensor_scalar(out=SelT[:, s_, tb * 512:(tb + 1) * 512], in0=ps[:, :], scalar1=slotid[:, s_:s_ + 1], scalar2=None, op0=ALU.is_equal),
                                 reads=[pr, "slotid", "selbuf"], writes=["selbuf"])
                    for t in range(NT):
                        for f in range(2):
                            po, por = self.psum()
                            self.mm(po[:, :], por, [(SelT[:, s_, t * 128:(t + 1) * 128], Yb[:, s_, f * 512:(f + 1) * 512]) for s_ in range(NS)], reads=["selbuf", "XgT"])
                            xs = xtm[:, t, f * 512:(f + 1) * 512]
                            P.op("vector", lambda e, xs=xs, po=po, t=t, ex=ex: e.scalar_tensor_tensor(out=xs, in0=po[:], scalar=gates[:, t, ex:ex + 1], in1=xs, op0=ALU.mult, op1=ALU.add),
                                 reads=[por, ("xtm", t), ("gates", t)], writes=[("xtm", t)])
            P.barrier()

    def ple(self, l):
        P = self.P
        xb, xtm = self.xb, self.xtm
        with ExitStack() as st:
            ptm = self.T(st, "ptm", [128, NT, 256])
            pT = self.T(st, "pT", [128, 2, S], BF16)
            tmp = [self.T(st, "pletmp%d" % i, [128, 512]) for i in range(3)]
            psrc = self.dr["p"][l].rearrange("(t p) f -> p t f", p=128)
            P.dma("sync", lambda e: e.dma_start(out=ptm[:], in_=psrc), writes=["ptm"])
            for tb in range(NB):
                for c in range(2):
                    ps, pr = self.psum()

                    def fn(e, ps=ps, tb=tb, c=c):
                        ins = None
                        for j in range(4):
                            ins = e.transpose(out=ps[:, j * 128:(j + 1) * 128], in_=ptm[:, tb * 4 + j, c * 128:(c + 1) * 128], identity=self.ident)
                        return ins
                    P.op("tensor", fn, reads=["ptm", "cm"], writes=[pr])
                    dst = pT[:, c, tb * 512:(tb + 1) * 512]
                    P.op("scalar", lambda e, ps=ps, dst=dst: e.copy(out=dst, in_=ps[:]), reads=[pr], writes=[("pT", c, tb)])
            wp, wpr = self.wload(self.wview(self.dr["ple_w"][l], 0, 2, 0, 1024), 2, 1024)
            for f in range(2):
                wg, wgr = self.wload(self.wview(self.dr["ple_gate_w"][l], 0, 8, f * 512, 512), 8, 512)
                for t in range(NT):
                    pg, pgr = self.psum()
                    self.mm(pg[:, :], pgr, [(xb[:, k, t * 128:(t + 1) * 128], wg[:, k, :]) for k in range(KC)], reads=[wgr] + self.xb_res(t // 4))
                    pp, ppr = self.psum()
                    self.mm(pp[:, :], ppr, [(pT[:, c, t * 128:(t + 1) * 128], wp[:, c, f * 512:(f + 1) * 512]) for c in range(2)],
                            reads=[wpr, ("pT", 0, t // 4), ("pT", 1, t // 4)])
                    ti = self.rot("pletmp", 3)
                    tm = tmp[ti]
                    P.op("scalar", lambda e, tm=tm, pg=pg: e.activation(out=tm[:], in_=pg[:], func=AF.Sigmoid), reads=[pgr], writes=[("pletmp", ti)])
                    P.op("vector", lambda e, tm=tm, pp=pp: e.tensor_tensor(out=tm[:], in0=tm[:], in1=pp[:], op=ALU.mult), reads=[("pletmp", ti), ppr], writes=[("pletmp", ti)])
                    xs = xtm[:, t, f * 512:(f + 1) * 512]
                    P.op("vector", lambda e, xs=xs, tm=tm: e.tensor_tensor(out=xs, in0=xs, in1=tm[:], op=ALU.add), reads=[("pletmp", ti), ("xtm", t)], writes=[("xtm", t)])

    def fox(self):
        P = self.P
        xb, xtm = self.xb, self.xtm
        W = self.dr["fox_w_in"][0]
        bc = self.dr["bc"]
        self.n_rot = 4
        with ExitStack() as st:
            oT = self.T(st, "oT", [128, 2, S], BF16)
            self.ubig = self.T(st, "ubig", [128, 4, 512], BF16)
            P.dma("gpsimd", lambda e: e.dma_start(out=self.ubig[:], in_=self.dr["cm"][:, CM_UBIG:CM_UBIG + 2048].rearrange("p (j t) -> p j t", j=4)), writes=["ubig"])
            vpad = self.T(st, "vpad", [128, NT, 2, 128], BF16)
            qT = self.T(st, "qT", [128, 2, S], BF16)
            kT = self.T(st, "kT", [128, S], BF16)
            P.op("vector", lambda e: e.memset(qT[:], 0.0), writes=["qTz"])
            pT = [self.T(st, "pexp%d" % i, [128, 512], BF16) for i in range(4)]
            rec = [self.T(st, "rec%d" % i, [128, 512]) for i in range(2)]
            lf = self.T(st, "lf", [128, NT, 16])
            negc = self.T(st, "negc", [128, NT, 16])
            kap = self.T(st, "kap", [128, NB, 16])
            biasT = self.T(st, "biasT", [128, NB, NT, 16])
            bfb = self.T(st, "bfb", [128, 16])
            zt = [self.T(st, "zt%d" % i, [128, 16]) for i in range(2)]
            oneslr = self.T(st, "oneslr", [128, 2, 128], BF16)
            o0 = BC_LAYOUT["fox_b_f"][0]
            P.dma("sync", lambda e: e.dma_start(out=bfb[:], in_=bc[:, o0:o0 + 16]), writes=["bfb"])
            P.op("vector", lambda e: e.memset(vpad[:], 0.0), writes=["vpad"])
            P.op("vector", lambda e: e.memset(oneslr[:], 0.0), writes=["oneslr"])
            P.op("vector", lambda e: e.memset(oneslr[:, 0, 0:64], 1.0), reads=["oneslr"], writes=["oneslr"])
            P.op("vector", lambda e: e.memset(oneslr[:, 1, 64:128], 1.0), reads=["oneslr"], writes=["oneslr"])
            wf, wfr = self.wload_narrow(st, "foxwf", W, 3072, 16)
            for t in range(NT):
                ps, pr = self.psum()
                self.mm(ps[:, 0:16], pr, [(xb[:, k, t * 128:(t + 1) * 128], wf[:, k, :]) for k in range(KC)], reads=[wfr] + self.xb_res(t // 4))
                z = zt[t % 2]
                zr = ("zt", t % 2)
                P.op("vector", lambda e, z=z, ps=ps: e.tensor_tensor(out=z[:], in0=ps[:, 0:16], in1=bfb[:], op=ALU.add), reads=[pr, "bfb"], writes=[zr])
                P.op("scalar", lambda e, z=z: e.activation(out=z[:], in_=z[:], func=AF.Exp, scale=-1.0), reads=[zr], writes=[zr])
                P.op("scalar", lambda e, z=z, t=t: e.activation(out=lf[:, t, :], in_=z[:], func=AF.Ln, bias=1.0, scale=1.0), reads=[zr], writes=[("lf", t)])
            STOP = int(os.environ.get("KB_STOP", "99"))
            if STOP <= 1:
                self.n_rot = 8
                P.barrier()
                return
            U = self.cm[:, CM_U:CM_U + 128]
            for t in range(NT):
                ps, pr = self.psum()
                pairs = [(self.ones_f[:], lf[:, j, :]) for j in range(t)] + [(U, lf[:, t, :])]
                self.mm(ps[:, 0:16], pr, pairs, reads=["ones_f", "cm"] + [("lf", j) for j in range(t + 1)])
                P.op("vector", lambda e, ps=ps, t=t: e.tensor_copy(out=negc[:, t, :], in_=ps[:, 0:16]), reads=[pr], writes=[("negc", t)])
            for n in range(NB):
                ps, pr = self.psum()
                pairs = [(self.ones_f[:], lf[:, j, :]) for j in range(4 * n + 2)]
                self.mm(ps[:, 0:16], pr, pairs, reads=["ones_f"] + [("lf", j) for j in range(4 * n + 2)])
                P.op("vector", lambda e, ps=ps, n=n: e.tensor_copy(out=kap[:, n, :], in_=ps[:, 0:16]), reads=[pr], writes=[("kap", n)])
                for j in range(4 * n + 4):
                    P.op("vector", lambda e, n=n, j=j: e.tensor_tensor(out=biasT[:, n, j, :], in0=negc[:, j, :], in1=kap[:, n, :], op=ALU.subtract),
                         reads=[("negc", j), ("kap", n)], writes=[("biasT", n, j)])
            if STOP <= 2:
                self.n_rot = 8
                P.barrier()
                return
            for c in range(KC):
                if STOP <= 5 and c >= 1:
                    break
                wq, wqr = self.wload(self.wview(W, 0, 8, c * 128, 128), 8, 128)
                wk, wkr = self.wload(self.wview(W, 0, 8, 1024 + c * 128, 128), 8, 128)
                wv, wvr = self.wload(self.wview(W, 0, 8, 2048 + c * 128, 128), 8, 128)
                for n in range(NB):
                    ps, pr = self.psum()
                    self.mm(ps[:, :], pr, [(wq[:, k, :], xb[:, k, n * 512:(n + 1) * 512]) for k in range(KC)], reads=[wqr] + self.xb_res(n))
                    P.op("vector", lambda e, ps=ps, n=n: e.tensor_scalar(out=qT[0:64, 0, n * 512:(n + 1) * 512], in0=ps[0:64, :], scalar1=0.125, scalar2=None, op0=ALU.mult), reads=[pr, "qTz"], writes=[("qT", n, 0)])
                    P.op("vector", lambda e, ps=ps, n=n: e.tensor_scalar(out=qT[64:128, 1, n * 512:(n + 1) * 512], in0=ps[64:128, :], scalar1=0.125, scalar2=None, op0=ALU.mult), reads=[pr, "qTz"], writes=[("qT", n, 1)])
                    ps, pr = self.psum()
                    self.mm(ps[:, :], pr, [(wk[:, k, :], xb[:, k, n * 512:(n + 1) * 512]) for k in range(KC)], reads=[wkr] + self.xb_res(n))
                    P.op("vector", lambda e, ps=ps, n=n: e.tensor_copy(out=kT[:, n * 512:(n + 1) * 512], in_=ps[:]), reads=[pr], writes=[("kT", n)])
                for t in range(NT):
                    ps, pr = self.psum()
                    self.mm(ps[:, 0:128], pr, [(xb[:, k, t * 128:(t + 1) * 128], wv[:, k, :]) for k in range(KC)], reads=[wvr] + self.xb_res(t // 4))
                    P.op("vector", lambda e, ps=ps, t=t: e.tensor_copy(out=vpad[:, t, 0, 0:64], in_=ps[:, 0:64]), reads=[pr, "vpad"], writes=[("vpad", t, 0)])
                    P.op("vector", lambda e, ps=ps, t=t: e.tensor_copy(out=vpad[:, t, 1, 64:128], in_=ps[:, 64:128]), reads=[pr, "vpad"], writes=[("vpad", t, 1)])
                if STOP <= 3:
                    break
                for n in range(NB):
                    if STOP <= 4 and n >= 1:
                        break
                    pi = self.rot("foxacc", 2)
                    num, numr = self.ps[4 + 2 * pi], ("ps", 4 + 2 * pi)
                    den, denr = self.ps[5 + 2 * pi], ("ps", 5 + 2 * pi)
                    nkt = 4 * n + 4
                    total = 2 * nkt
                    steps = [(hh, j) for hh in range(2) for j in range(nkt)]

                    def emit_S(k, n=n, c=c):
                        hh, j = steps[k]
                        sp, spr = self.psum()
                        self.mm(sp[:, :], spr, [(kT[:, j * 128:(j + 1) * 128], qT[:, hh, n * 512:(n + 1) * 512])], reads=[("kT", j // 4), ("qT", n, hh), "qTz"])
                        ei = self.rot("pexp", 4)
                        pe = pT[ei]
                        per = ("pexp", ei)
                        bcol = biasT[:, n, j, 2 * c + hh:2 * c + hh + 1]
                        P.op("scalar", lambda e, pe=pe, sp=sp, bcol=bcol: e.activation(out=pe[:], in_=sp[:], func=AF.Exp, bias=bcol, scale=1.0),
                             reads=[spr, ("biasT", n, j)], writes=[per])
                        if j >= 4 * n:
                            P.op("vector", lambda e, pe=pe, jj=j - 4 * n: e.tensor_tensor(out=pe[:], in0=pe[:], in1=self.ubig[:, jj, :], op=ALU.mult),
                                 reads=[per, "ubig"], writes=[per])
                        return pe, per
                    LA = 2
                    q_ = [emit_S(k) for k in range(min(LA, total))]
                    for idx in range(total):
                        if idx + LA < total:
                            q_.append(emit_S(idx + LA))
                        pe, per = q_[idx]
                        hh, j = steps[idx]
                        first, last = (idx == 0), (idx == total - 1)

                        def fn(e, num=num, den=den, pe=pe, j=j, hh=hh, first=first, last=last):
                            e.matmul(num[:, :], lhsT=vpad[:, j, hh, :], rhs=pe[:], start=first, stop=last)
                            return e.matmul(den[:, :], lhsT=oneslr[:, hh, :], rhs=pe[:], start=first, stop=last)
                        P.op("tensor", fn, reads=[per, ("vpad", j, hh), "vpad", "oneslr"], writes=[numr, denr])
                    rc = rec[pi]
                    P.op("vector", lambda e, rc=rc, den=den: e.reciprocal(out=rc[:], in_=den[:]), reads=[denr], writes=[("rec", pi)])
                    P.op("vector", lambda e, rc=rc, num=num, c=c, n=n: e.tensor_tensor(out=oT[:, c % 2, n * 512:(n + 1) * 512], in0=num[:], in1=rc[:], op=ALU.mult),
                         reads=[numr, ("rec", pi)], writes=[("oT", c % 2, n)])
                if c % 2 == 1:
                    self.out_proj_acc(oT, lambda t: [("oT", 0, t // 4), ("oT", 1, t // 4)], self.dr["fox_w_out"][0], c - 1, first=(c == 1), nk=2)
            self.n_rot = 8
        P.barrier()

    def lru(self):
        P = self.P
        xb = self.xb
        W = self.dr["lru_w_in"][0]
        Wo = self.dr["lru_w_out"][0]
        col = self.dr["col"]
        B4 = lambda nm: [(nm, n) for n in range(NB)]
        with ExitStack() as st:
            cv = self.T(st, "lrucol", [128, 80])
            sp8 = self.T(st, "sp8", [128, 10])
            recp = self.T(st, "recp", [128, 3 + S])
            u = self.T(st, "lru_u", [128, S])
            ub = self.T(st, "lru_ub", [128, S], BF16)
            ra = self.T(st, "lru_ra", [128, S])
            ib = self.T(st, "lru_ib", [128, S])
            y2 = self.T(st, "lru_y", [128, 2, S], BF16)
            wab = self.T(st, "lru_wab", [128, 2, 128], BF16)
            o0 = COL_LAYOUT["lru_conv_w"][0]
            P.dma("sync", lambda e: e.dma_start(out=cv[:], in_=col[:, o0:o0 + 80]), writes=["lrucol"])
            P.op("scalar", lambda e: e.activation(out=sp8[:], in_=cv[:, 70:80], func=AF.Exp, scale=-1.0), reads=["lrucol"], writes=["sp8"])
            P.op("scalar", lambda e: e.activation(out=sp8[:], in_=sp8[:], func=AF.Ln, bias=1.0, scale=1.0), reads=["sp8"], writes=["sp8"])
            P.op("vector", lambda e: e.tensor_scalar(out=sp8[:], in0=sp8[:], scalar1=-8.0, scalar2=None, op0=ALU.mult), reads=["sp8"], writes=["sp8"])
            P.op("vector", lambda e: e.memset(recp[:, 0:3], 0.0), writes=["recpad"])
            for cc in range(10):
                y = y2[:, cc % 2, :]
                yk = "y%d" % (cc % 2)
                wg, wgr = self.wload(self.wview(W, 0, 8, cc * 128, 128), 8, 128)
                wr_, wrr = self.wload(self.wview(W, 0, 8, 1280 + cc * 128, 128), 8, 128)
                P.dma("gpsimd", lambda e, cc=cc: e.dma_start(out=wab[:, 0, :], in_=self.dr["lru_w_a"][0, cc]), writes=[("wab", 0)])
                P.dma("gpsimd", lambda e, cc=cc: e.dma_start(out=wab[:, 1, :], in_=self.dr["lru_w_x"][0, cc]), writes=[("wab", 1)])
                for n in range(NB):
                    bs = slice(n * 512, (n + 1) * 512)
                    ps, pr = self.psum()
                    self.mm(ps[:, :], pr, [(wg[:, k, :], xb[:, k, bs]) for k in range(KC)], reads=[wgr] + self.xb_res(n))
                    P.op("scalar", lambda e, ps=ps, bs=bs, y=y: e.activation(out=y[:, bs], in_=ps[:], func=AF.Gelu_apprx_tanh), reads=[pr], writes=[(yk, n)])
                    ps, pr = self.psum()
                    self.mm(ps[:, :], pr, [(wr_[:, k, :], xb[:, k, bs]) for k in range(KC)], reads=[wrr] + self.xb_res(n))
                    P.op("vector", lambda e, ps=ps, n=n: e.tensor_copy(out=recp[:, 3 + n * 512:3 + (n + 1) * 512], in_=ps[:]), reads=[pr], writes=[("recp", n)])
                cw = lambda k: cv[:, cc * 4 + k:cc * 4 + k + 1]
                cb = cv[:, 40 + cc:41 + cc]
                P.op("vector", lambda e, c0=cw(0), cb=cb: e.tensor_scalar(out=u[:], in0=recp[:, 0:S], scalar1=c0, scalar2=cb, op0=ALU.mult, op1=ALU.add),
                     reads=B4("recp") + ["recpad", "lrucol"], writes=["u"])
                for k in range(1, 4):
                    P.op("vector", lambda e, k=k, ck=cw(k): e.scalar_tensor_tensor(out=u[:], in0=recp[:, k:k + S], scalar=ck, in1=u[:], op0=ALU.mult, op1=ALU.add),
                         reads=B4("recp") + ["recpad", "lrucol", "u"], writes=["u"])
                P.op("scalar", lambda e: e.copy(out=ub[:], in_=u[:]), reads=["u"], writes=["ub"])
                for n in range(NB):
                    bs = slice(n * 512, (n + 1) * 512)
                    ps, pr = self.psum()
                    self.mm(ps[:, :], pr, [(wab[:, 0, :], ub[:, bs])], reads=[("wab", 0), "ub"])
                    P.op("scalar", lambda e, ps=ps, bs=bs, b=cv[:, 50 + cc:51 + cc]: e.activation(out=ra[:, bs], in_=ps[:], func=AF.Sigmoid, bias=b, scale=1.0),
                         reads=[pr, "lrucol"], writes=[("ra", n)])
                    ps, pr = self.psum()
                    self.mm(ps[:, :], pr, [(wab[:, 1, :], ub[:, bs])], reads=[("wab", 1), "ub"])
                    P.op("scalar", lambda e, ps=ps, bs=bs, b=cv[:, 60 + cc:61 + cc]: e.activation(out=ib[:, bs], in_=ps[:], func=AF.Sigmoid, bias=b, scale=1.0),
                         reads=[pr, "lrucol"], writes=[("ib", n)])
                P.op("scalar", lambda e, sc=sp8[:, cc:cc + 1]: e.activation(out=ra[:], in_=ra[:], func=AF.Exp, scale=sc), reads=B4("ra") + ["sp8"], writes=B4("ra"))
                P.op("vector", lambda e: e.tensor_tensor(out=ib[:], in0=ib[:], in1=u[:], op=ALU.mult), reads=B4("ib") + ["u"], writes=B4("ib"))
                P.op("vector", lambda e: e.tensor_tensor(out=u[:], in0=ra[:], in1=ra[:], op=ALU.mult), reads=B4("ra") + ["u"], writes=["u"])
                P.op("scalar", lambda e: e.activation(out=u[:], in_=u[:], func=AF.Sqrt, bias=1.0, scale=-1.0), reads=["u"], writes=["u"])
                P.op("vector", lambda e: e.tensor_tensor(out=ib[:], in0=ib[:], in1=u[:], op=ALU.mult), reads=B4("ib") + ["u"], writes=B4("ib"))
                P.op("vector", lambda e: e.tensor_tensor_scan(out=recp[:, 3:3 + S], data0=ra[:], data1=ib[:], initial=0.0, op0=ALU.mult, op1=ALU.add),
                     reads=B4("ra") + B4("ib") + B4("recp"), writes=B4("recp"))
                P.op("vector", lambda e, y=y: e.tensor_tensor(out=y, in0=y, in1=recp[:, 3:3 + S], op=ALU.mult), reads=B4(yk) + B4("recp"), writes=B4(yk))
                if cc % 2 == 1:
                    self.out_proj_acc(y2, lambda t: [("y0", t // 4), ("y1", t // 4)], Wo, cc - 1, first=(cc == 1), nk=2)
        P.barrier()

    def conformer(self):
        P = self.P
        xb, xtm = self.xb, self.xtm
        W = self.dr["cv_w_in"][0]
        Wo = self.dr["cv_w_out"][0]
        col = self.dr["col"]
        bc = self.dr["bc"]
        B4 = lambda nm: [(nm, n) for n in range(NB)]
        with ExitStack() as st:
            cvc = self.T(st, "cvcol", [128, 288])
            hpad = self.T(st, "hpad", [128, 30 + S], BF16)
            dgk = self.T(st, "dgk", [128, 31, 128], BF16)
            sgt = [self.T(st, "cvsg%d" % i, [128, 512]) for i in range(2)]
            sq = [self.T(st, "cvsq%d" % i, [128, 512], BF16) for i in range(2)]
            convb = self.T(st, "convb", [128, KC, S], BF16)
            mean = self.T(st, "cvmean", [128, 512])
            rstd = self.T(st, "cvrstd", [128, 512])
            bo = self.T(st, "cvbo", [128, D])
            o0 = COL_LAYOUT["cv_b_in"][0]
            P.dma("sync", lambda e: e.dma_start(out=cvc[:], in_=col[:, o0:o0 + 288]), writes=["cvcol"])
            o1 = BC_LAYOUT["cv_b_out"][0]
            P.dma("sync", lambda e: e.dma_start(out=bo[:], in_=bc[:, o1:o1 + D]), writes=["cvbo"])
            P.op("vector", lambda e: e.memset(hpad[:, 0:30], 0.0), writes=["hpadz"])
            for cc in range(KC):
                wv, wvr = self.wload(self.wview(W, 0, 8, cc * 128, 128), 8, 128)
                wg, wgr = self.wload(self.wview(W, 0, 8, 1024 + cc * 128, 128), 8, 128)
                for n in range(NB):
                    bs = slice(n * 512, (n + 1) * 512)
                    pv, pvr = self.psum()
                    self.mm(pv[:, :], pvr, [(wv[:, k, :], xb[:, k, bs]) for k in range(KC)], reads=[wvr] + self.xb_res(n))
                    pg, pgr = self.psum()
                    self.mm(pg[:, :], pgr, [(wg[:, k, :], xb[:, k, bs]) for k in range(KC)], reads=[wgr] + self.xb_res(n))
                    si = self.rot("cvsg", 2)
                    sg = sgt[si]
                    P.op("scalar", lambda e, sg=sg, pg=pg, b=cvc[:, 8 + cc:9 + cc]: e.activation(out=sg[:], in_=pg[:], func=AF.Sigmoid, bias=b, scale=1.0),
                         reads=[pgr, "cvcol"], writes=[("cvsg", si)])
                    P.op("vector", lambda e, sg=sg, pv=pv, n=n, b=cvc[:, cc:cc + 1]: e.scalar_tensor_tensor(out=hpad[:, 30 + n * 512:30 + (n + 1) * 512], in0=pv[:], scalar=b, in1=sg[:], op0=ALU.add, op1=ALU.mult),
                         reads=[pvr, ("cvsg", si), "cvcol"], writes=[("hpad", n)])
                dw = lambda k: cvc[:, 16 + cc * 31 + k:16 + cc * 31 + k + 1]
                db = cvc[:, 264 + cc:265 + cc]
                for k in range(31):
                    eng = "vector" if k % 2 == 0 else "gpsimd"
                    P.op(eng, lambda e, k=k, dk=dw(k): e.tensor_scalar(out=dgk[:, k, :], in0=self.ident, scalar1=dk, scalar2=None, op0=ALU.mult),
                         reads=["cm", "cvcol", ("dgk", k)], writes=[("dgk", k)])
                for n in range(NB):
                    ps, pr = self.psum()
                    self.mm(ps[:, :], pr, [(dgk[:, k, :], hpad[:, k + n * 512:k + (n + 1) * 512]) for k in range(31)],
                            reads=[("dgk", k) for k in range(31)] + B4("hpad") + ["hpadz"])
                    P.op("vector", lambda e, ps=ps, n=n, cc=cc, db=db: e.tensor_scalar(out=convb[:, cc, n * 512:(n + 1) * 512], in0=ps[:], scalar1=db, scalar2=None, op0=ALU.add),
                         reads=[pr, "cvcol"], writes=[("convb", cc, n)])
            for n in range(NB):
                bs = slice(n * 512, (n + 1) * 512)
                psum_, psr = self.psum()
                self.mm(psum_[:, :], psr, [(self.ones_b[:], convb[:, cc, bs]) for cc in range(KC)], reads=["ones_b"] + [("convb", cc, n) for cc in range(KC)])
                psq, pqr = self.psum()
                for cc in range(KC):
                    qi = self.rot("cvsq", 2)
                    P.op("scalar", lambda e, qi=qi, cc=cc, bs=bs: e.activation(out=sq[qi][:], in_=convb[:, cc, bs], func=AF.Square), reads=[("convb", cc, n)], writes=[("cvsq", qi)])
                    P.op("tensor", lambda e, qi=qi, cc=cc, psq=psq: e.matmul(psq[:, :], lhsT=self.ones_b[:], rhs=sq[qi][:], start=(cc == 0), stop=(cc == KC - 1)),
                         reads=["ones_b", ("cvsq", qi)], writes=[pqr])
                P.op("scalar", lambda e, psum_=psum_: e.activation(out=mean[:], in_=psum_[:], func=AF.Copy, scale=1.0 / D), reads=[psr], writes=["cvmean"])
                P.op("vector", lambda e: e.tensor_tensor(out=rstd[:], in0=mean[:], in1=mean[:], op=ALU.mult), reads=["cvmean"], writes=["cvrstd"])
                P.op("vector", lambda e, psq=psq: e.scalar_tensor_tensor(out=rstd[:], in0=psq[:], scalar=1.0 / D, in1=rstd[:], op0=ALU.mult, op1=ALU.subtract),
                     reads=[pqr, "cvrstd"], writes=["cvrstd"])
                P.op("scalar", lambda e: e.activation(out=rstd[:], in_=rstd[:], func=AF.Sqrt, bias=LN_EPS, scale=1.0), reads=["cvrstd"], writes=["cvrstd"])
                P.op("vector", lambda e: e.reciprocal(out=rstd[:], in_=rstd[:]), reads=["cvrstd"], writes=["cvrstd"])
                for cc in range(KC):
                    si = self.rot("cvsg", 2)
                    tm = sgt[si]
                    P.op("vector", lambda e, tm=tm, cc=cc, bs=bs: e.tensor_tensor(out=tm[:], in0=convb[:, cc, bs], in1=mean[:], op=ALU.subtract),
                         reads=[("convb", cc, n), "cvmean"], writes=[("cvsg", si)])
                    P.op("vector", lambda e, tm=tm: e.tensor_tensor(out=tm[:], in0=tm[:], in1=rstd[:], op=ALU.mult), reads=[("cvsg", si), "cvrstd"], writes=[("cvsg", si)])
                    P.op("scalar", lambda e, tm=tm, cc=cc, bs=bs: e.activation(out=convb[:, cc, bs], in_=tm[:], func=AF.Silu, bias=cvc[:, 280 + cc:281 + cc], scale=cvc[:, 272 + cc:273 + cc]),
                         reads=[("cvsg", si), "cvcol"], writes=[("convb", cc, n)])
            self.out_proj_full(convb, KC, lambda t: [("convb", cc, t // 4) for cc in range(KC)], Wo)
            for t in range(NT):
                xt = xtm[:, t, :]
                P.op("gpsimd", lambda e, xt=xt: e.tensor_tensor(out=xt, in0=xt, in1=bo[:], op=ALU.add), reads=[("xtm", t), "cvbo"], writes=[("xtm", t)])
        P.barrier()

    def gdn(self):
        P = self.P
        xb, xtm = self.xb, self.xtm
        W = self.dr["gdn_w_in"][0]
        Wo = self.dr["gdn_w_out"][0]
        col = self.dr["col"]
        bc = self.dr["bc"]
        cm = self.cm
        ident = self.ident
        U = cm[:, CM_U:CM_U + 128]
        mlow = cm[:, CM_MLOW:CM_MLOW + 128]
        mup = cm[:, CM_MUP:CM_MUP + 128]
        strict = cm[:, CM_STRICT:CM_STRICT + 128]
        ones_f = self.ones_f
        B4 = lambda nm: [(nm, n) for n in range(NB)]
        with ExitStack() as st:
            T = lambda nm, shp, dt=F32: self.T(st, "g_" + nm, shp, dt)
            gcol = T("col", [128, 96])
            hb = T("hb", [128, 144])
            beta = T("beta", [128, NT, 8])
            gt = T("gt", [128, NT, 8])
            gc = T("gc", [128, NT, 8])
            ngc = T("ngc", [128, NT, 8])
            egc = T("egc", [128, NT, 8])
            bexp = T("bexp", [128, NT, 8])
            glast = T("glast", [128, NT, 8])
            kendc = T("kendc", [128, NT, 8])
            eglast = T("eglast", [128, NT, 8])
            nea = T("nea", [128, 8])
            cpad = T("cpad", [128, 3 + S])
            cf = T("cf", [128, S])
            otm = cf[:, :].rearrange("p (t d) -> p t d", t=NT)
            qT = T("qT", [128, S])
            kT = T("kT", [128, S])
            vtm = T("vtm", [128, NT, 128], BF16)
            ktm = T("ktm", [128, NT, 128], BF16)
            ohT = T("ohT", [128, S], BF16)
            ssr = T("ssr", [128, 512])
            sqf = T("sqf", [128, 512])
            Sst = T("S", [128, 128])
            mats = {}
            for nm in ["dg", "ndg", "t1", "Dl", "L", "t2", "Du", "R0", "R1", "P0", "P1", "Y0", "Y1",
                       "vb", "kbg", "kend", "nwT", "vnew", "tmpo", "sz", "on"]:
                mats[nm] = T("m_" + nm, [128, 128])
            self.n_slots = 2
            self.slot_rr = 0
            l1a = self.slots[2][:, :].bitcast(F32)
            l1b = self.slots[3][:, :].bitcast(F32)
            lane1 = {}
            for q_, nm in enumerate(["dg", "ndg", "t1", "Dl", "L", "t2", "Du", "R0", "R1", "P0", "P1", "Y0", "Y1"]):
                src_ = l1a if q_ < 8 else l1b
                q2 = q_ % 8
                lane1[nm] = src_[:, q2 * 128:(q2 + 1) * 128]
            TTs = l1b[:, 5 * 128:9 * 128].rearrange("p (a b d) -> p a b d", a=2, b=2)
            ATs = l1b[:, 9 * 128:13 * 128].rearrange("p (a b d) -> p a b d", a=2, b=2)
            o0 = COL_LAYOUT["gdn_conv_w"][0]
            P.dma("sync", lambda e: e.dma_start(out=gcol[:], in_=col[:, o0:o0 + 96]), writes=["gcol"])
            o1 = BC_LAYOUT["gdn_a_log"][0]
            P.dma("sync", lambda e: e.dma_start(out=hb[:], in_=bc[:, o1:o1 + 144]), writes=["hb"])
            P.op("scalar", lambda e: e.activation(out=nea[:], in_=hb[:, 0:8], func=AF.Exp), reads=["hb"], writes=["nea"])
            P.op("vector", lambda e: e.tensor_scalar(out=nea[:], in0=nea[:], scalar1=-1.0, scalar2=None, op0=ALU.mult), reads=["nea"], writes=["nea"])
            P.op("vector", lambda e: e.memset(cpad[:, 0:3], 0.0), writes=["cpadz"])
            wba, wbar = self.wload_narrow(st, "gdnwba", W, 4096, 16)
            for t in range(NT):
                ps, pr = self.psum()
                self.mm(ps[:, 0:16], pr, [(xb[:, k, t * 128:(t + 1) * 128], wba[:, k, :]) for k in range(KC)], reads=[wbar] + self.xb_res(t // 4))
                R = [("gsc", t)]
                P.op("vector", lambda e, ps=ps, t=t: e.tensor_copy(out=beta[:, t, :], in_=ps[:, 0:8]), reads=[pr], writes=R)
                P.op("vector", lambda e, ps=ps, t=t: e.tensor_tensor(out=gt[:, t, :], in0=ps[:, 8:16], in1=hb[:, 8:16], op=ALU.add), reads=[pr, "hb"] + R, writes=R)
                P.op("scalar", lambda e, t=t: e.activation(out=beta[:, t, :], in_=beta[:, t, :], func=AF.Sigmoid), reads=R, writes=R)
                P.op("scalar", lambda e, t=t: e.activation(out=gt[:, t, :], in_=gt[:, t, :], func=AF.Exp), reads=R, writes=R)
                P.op("scalar", lambda e, t=t: e.activation(out=gt[:, t, :], in_=gt[:, t, :], func=AF.Ln, bias=1.0, scale=1.0), reads=R, writes=R)
                P.op("vector", lambda e, t=t: e.tensor_tensor(out=gt[:, t, :], in0=gt[:, t, :], in1=nea[:], op=ALU.mult), reads=R + ["nea"], writes=R)
                ps, pr = self.psum()
                self.mm(ps[:, 0:8], pr, [(U, gt[:, t, :])], reads=R + ["cm"])
                ps2, pr2 = self.psum()
                self.mm(ps2[:, 0:8], pr2, [(ones_f[:], gt[:, t, :])], reads=R + ["ones_f"])
                P.op("vector", lambda e, ps=ps, t=t: e.tensor_copy(out=gc[:, t, :], in_=ps[:, 0:8]), reads=[pr] + R, writes=R)
                P.op("vector", lambda e, ps2=ps2, t=t: e.tensor_copy(out=glast[:, t, :], in_=ps2[:, 0:8]), reads=[pr2] + R, writes=R)
                P.op("vector", lambda e, t=t: e.tensor_scalar(out=ngc[:, t, :], in0=gc[:, t, :], scalar1=-1.0, scalar2=None, op0=ALU.mult), reads=R, writes=R)
                P.op("scalar", lambda e, t=t: e.activation(out=egc[:, t, :], in_=gc[:, t, :], func=AF.Exp), reads=R, writes=R)
                P.op("vector", lambda e, t=t: e.tensor_tensor(out=bexp[:, t, :], in0=egc[:, t, :], in1=beta[:, t, :], op=ALU.mult), reads=R, writes=R)
                P.op("vector", lambda e, t=t: e.tensor_tensor(out=kendc[:, t, :], in0=glast[:, t, :], in1=gc[:, t, :], op=ALU.subtract), reads=R, writes=R)
                P.op("scalar", lambda e, t=t: e.activation(out=kendc[:, t, :], in_=kendc[:, t, :], func=AF.Exp), reads=R, writes=R)
                P.op("scalar", lambda e, t=t: e.activation(out=eglast[:, t, :], in_=glast[:, t, :], func=AF.Exp), reads=R, writes=R)
            for h in range(8):
                for which in range(3):
                    wv, wvr = self.wload(self.wview(W, 0, 8, which * 1024 + h * 128, 128), 8, 128)
                    for n in range(NB):
                        bs = slice(n * 512, (n + 1) * 512)
                        ps, pr = self.psum()
                        self.mm(ps[:, :], pr, [(wv[:, k, :], xb[:, k, bs]) for k in range(KC)], reads=[wvr] + self.xb_res(n))
                        P.op("scalar", lambda e, ps=ps, n=n: e.copy(out=cpad[:, 3 + n * 512:3 + (n + 1) * 512], in_=ps[:]), reads=[pr], writes=[("cpad", n)])
                    cidx = which * 8 + h
                    cw = lambda k: gcol[:, cidx * 4 + k:cidx * 4 + k + 1]
                    P.op("vector", lambda e, c0=cw(0): e.tensor_scalar(out=cf[:], in0=cpad[:, 0:S], scalar1=c0, scalar2=None, op0=ALU.mult),
                         reads=B4("cpad") + ["cpadz", "gcol"], writes=["cf"])
                    for k in range(1, 4):
                        P.op("vector", lambda e, k=k, ck=cw(k): e.scalar_tensor_tensor(out=cf[:], in0=cpad[:, k:k + S], scalar=ck, in1=cf[:], op0=ALU.mult, op1=ALU.add),
                             reads=B4("cpad") + ["cpadz", "gcol", "cf"], writes=["cf"])
                    P.op("scalar", lambda e: e.activation(out=cf[:], in_=cf[:], func=AF.Silu), reads=["cf"], writes=["cf"])
                    if which < 2:
                        dstT = qT if which == 0 else kT
                        dres = "qT" if which == 0 else "kT"
                        for n in range(NB):
                            bs = slice(n * 512, (n + 1) * 512)
                            P.op("vector", lambda e, bs=bs: e.tensor_tensor(out=sqf[:], in0=cf[:, bs], in1=cf[:, bs], op=ALU.mult), reads=["cf"], writes=["sqf"])
                            ps, pr = self.psum()
                            self.mm(ps[:, :], pr, [(ones_f[:], sqf[:])], reads=["ones_f", "sqf"])
                            P.op("scalar", lambda e, ps=ps: e.activation(out=ssr[:], in_=ps[:], func=AF.Sqrt, bias=1e-6, scale=1.0), reads=[pr], writes=["ssr"])
                            P.op("vector", lambda e: e.reciprocal(out=ssr[:], in_=ssr[:]), reads=["ssr"], writes=["ssr"])
                            sc = (128.0 ** -0.5) if which == 0 else 1.0
                            P.op("vector", lambda e, bs=bs, dstT=dstT, sc=sc: e.scalar_tensor_tensor(out=dstT[:, bs], in0=cf[:, bs], scalar=sc, in1=ssr[:], op0=ALU.mult, op1=ALU.mult),
                                 reads=["cf", "ssr"], writes=[(dres, n)])
                        if which == 1:
                            for tb in range(NB):
                                ps, pr = self.psum()

                                def fn(e, ps=ps, tb=tb):
                                    ins = None
                                    for j in range(4):
                                        ins = e.transpose(out=ps[:, j * 128:(j + 1) * 128], in_=kT[:, (tb * 4 + j) * 128:(tb * 4 + j + 1) * 128], identity=ident)
                                    return ins
                                P.op("tensor", fn, reads=[("kT", tb), "cm"], writes=[pr])
                                P.op("vector", lambda e, ps=ps, tb=tb: e.tensor_copy(out=ktm[:, tb * 4:(tb + 1) * 4, :], in_=ps[:].rearrange("p (j d) -> p j d", j=4)),
                                     reads=[pr], writes=[("ktm", tb)])
                    else:
                        for tb in range(NB):
                            ps, pr = self.psum()

                            def fn(e, ps=ps, tb=tb):
                                ins = None
                                for j in range(4):
                                    ins = e.transpose(out=ps[:, j * 128:(j + 1) * 128], in_=cf[:, (tb * 4 + j) * 128:(tb * 4 + j + 1) * 128], identity=ident)
                                return ins
                            P.op("tensor", fn, reads=["cf", "cm"], writes=[pr])
                            P.op("vector", lambda e, ps=ps, tb=tb: e.tensor_copy(out=vtm[:, tb * 4:(tb + 1) * 4, :], in_=ps[:].rearrange("p (j d) -> p j d", j=4)),
                                 reads=[pr], writes=[("vtm", tb)])
                P.op("vector", lambda e: e.memset(Sst[:], 0.0), reads=["S"], writes=["S"])
                PREP = ["dg", "ndg", "t1", "Dl", "L", "t2", "Du", "R0", "R1", "P0", "P1", "Y0", "Y1"]

                def lane_mat(lane, nm):
                    if lane == 0:
                        return mats[nm], nm
                    return lane1[nm], nm + "_l1"

                def make_ops(i, lane, par, banks):
                    prep, tail = [], []
                    bctr = [0]

                    def psb():
                        b = banks[bctr[0] % len(banks)]
                        bctr[0] += 1
                        return self.ps[b], ("ps", b)
                    tctr = [0]

                    def pst():
                        b = (6, 7)[tctr[0] % 2]
                        tctr[0] += 1
                        return self.ps[b], ("ps", b)

                    def MM(lst, out_ap, out_res, pairs, reads):
                        pairs = list(pairs)

                        def fn(e):
                            n = len(pairs)
                            ins = None
                            for q_, (l_, r_) in enumerate(pairs):
                                ins = e.matmul(out_ap, lhsT=l_, rhs=r_, start=(q_ == 0), stop=(q_ == n - 1))
                            return ins
                        lst.append(("tensor", fn, list(reads), [out_res]))
                    cs = slice(i * 128, (i + 1) * 128)
                    SC = [("gsc", i)]
                    colh = lambda tl: tl[:, i, h:h + 1]
                    M = {}
                    RK = {}
                    for nm in PREP:
                        M[nm], RK[nm] = lane_mat(lane, nm)
                    TTp = TTs[:, par, lane, :]
                    ATp = ATs[:, par, lane, :]
                    TTr = ("TT", par, lane)
                    ATr = ("AT", par, lane)
                    A = lambda eng, fn, reads, writes: prep.append((eng, fn, list(reads), list(writes)))
                    psK, prK = psb()
                    MM(prep, psK[:, 0:128], prK, [(kT[:, cs], kT[:, cs])], [("kT", i // 4)])
                    psQ, prQ = psb()
                    MM(prep, psQ[:, 0:128], prQ, [(kT[:, cs], qT[:, cs])], [("kT", i // 4), ("qT", i // 4)])
                    A("vector", lambda e, a=colh(gc): e.tensor_scalar(out=M["dg"][:], in0=ident, scalar1=a, scalar2=None, op0=ALU.mult), SC + ["cm", RK["dg"]], [RK["dg"]])
                    A("gpsimd", lambda e, a=colh(ngc): e.tensor_scalar(out=M["ndg"][:], in0=ident, scalar1=a, scalar2=None, op0=ALU.mult), SC + ["cm", RK["ndg"]], [RK["ndg"]])
                    psG, prG = psb()
                    MM(prep, psG[:, 0:128], prG, [(M["dg"][:], ones_f[:]), (ones_f[:], M["ndg"][:])], [RK["dg"], RK["ndg"], "ones_f"])
                    A("vector", lambda e, psG=psG: e.tensor_tensor(out=M["t1"][:], in0=psG[:, 0:128], in1=mlow, op=ALU.add), [prG, "cm", RK["t1"]], [RK["t1"]])
                    A("scalar", lambda e: e.activation(out=M["Dl"][:], in_=M["t1"][:], func=AF.Exp), [RK["t1"], RK["Dl"]], [RK["Dl"]])
                    A("vector", lambda e, psK=psK, a=colh(beta): e.scalar_tensor_tensor(out=M["L"][:], in0=psK[:, 0:128], scalar=a, in1=M["Dl"][:], op0=ALU.mult, op1=ALU.mult),
                      [prK, RK["Dl"], RK["L"]] + SC, [RK["L"]])
                    A("gpsimd", lambda e: e.tensor_tensor(out=M["L"][:], in0=M["L"][:], in1=strict, op=ALU.mult), [RK["L"], "cm"], [RK["L"]])
                    A("vector", lambda e, psG=psG: e.scalar_tensor_tensor(out=M["t2"][:], in0=psG[:, 0:128], scalar=-1.0, in1=mup, op0=ALU.mult, op1=ALU.add), [prG, "cm", RK["t2"]], [RK["t2"]])
                    A("scalar", lambda e: e.activation(out=M["Du"][:], in_=M["t2"][:], func=AF.Exp), [RK["t2"], RK["Du"]], [RK["Du"]])
                    A("vector", lambda e, psQ=psQ: e.tensor_tensor(out=ATp, in0=psQ[:, 0:128], in1=M["Du"][:], op=ALU.mult), [prQ, RK["Du"], ATr], [ATr])
                    psM, prM = psb()
                    prep.append(("tensor", lambda e, psM=psM: e.transpose(out=psM[:, 0:128], in_=M["L"][:], identity=ident), [RK["L"], "cm"], [prM]))
                    A("vector", lambda e, psM=psM: e.tensor_copy(out=M["R0"][:], in_=psM[:, 0:128]), [prM, RK["R0"]], [RK["R0"]])
                    A("vector", lambda e, psM=psM: e.scalar_tensor_tensor(out=M["Y0"][:], in0=psM[:, 0:128], scalar=-1.0, in1=ident, op0=ALU.mult, op1=ALU.add), [prM, "cm", RK["Y0"]], [RK["Y0"]])
                    Pn, Rn, Yn = "L", "R0", "Y0"
                    for lev in range(1, 7):
                        Pnew, Rnew, Ynew = "P%d" % (lev % 2), "R%d" % (lev % 2), "Y%d" % (lev % 2)
                        psP, prP = psb()
                        MM(prep, psP[:, 0:128], prP, [(M[Rn][:], M[Pn][:])], [RK[Rn], RK[Pn]])
                        if lev < 6:
                            psR, prR = psb()
                            MM(prep, psR[:, 0:128], prR, [(M[Pn][:], M[Rn][:])], [RK[Rn], RK[Pn]])
                        A("scalar", lambda e, psP=psP, Pnew=Pnew: e.copy(out=M[Pnew][:], in_=psP[:, 0:128]), [prP, RK[Pnew]], [RK[Pnew]])
                        if lev < 6:
                            A("vector", lambda e, psR=psR, Rnew=Rnew: e.tensor_copy(out=M[Rnew][:], in_=psR[:, 0:128]), [prR, RK[Rnew]], [RK[Rnew]])
                        psY, prY = psb()
                        MM(prep, psY[:, 0:128], prY, [(M[Pnew][:], M[Yn][:])], [RK[Pnew], RK[Yn]])
                        if lev < 6:
                            A("vector", lambda e, psY=psY, Yn=Yn, Ynew=Ynew: e.tensor_tensor(out=M[Ynew][:], in0=psY[:, 0:128], in1=M[Yn][:], op=ALU.add), [prY, RK[Yn], RK[Ynew]], [RK[Ynew]])
                        else:
                            A("vector", lambda e, psY=psY, Yn=Yn: e.tensor_tensor(out=TTp, in0=psY[:, 0:128], in1=M[Yn][:], op=ALU.add), [prY, RK[Yn], TTr], [TTr])
                        Pn, Rn, Yn = Pnew, Rnew, Ynew
                    m = mats
                    B = lambda eng, fn, reads, writes: tail.append((eng, fn, list(reads), list(writes)))
                    B("vector", lambda e, a=colh(beta): e.tensor_scalar(out=m["vb"][:], in0=vtm[:, i, :], scalar1=a, scalar2=None, op0=ALU.mult), [("vtm", i // 4), "vb"] + SC, ["vb"])
                    B("vector", lambda e, a=colh(bexp): e.tensor_scalar(out=m["kbg"][:], in0=ktm[:, i, :], scalar1=a, scalar2=None, op0=ALU.mult), [("ktm", i // 4), "kbg"] + SC, ["kbg"])
                    B("gpsimd", lambda e, a=colh(kendc): e.tensor_scalar(out=m["kend"][:], in0=ktm[:, i, :], scalar1=a, scalar2=None, op0=ALU.mult), [("ktm", i // 4), "kend"] + SC, ["kend"])
                    psW, prW = pst()
                    MM(tail, psW[:, 0:128], prW, [(m["kbg"][:], TTp)], ["kbg", TTr])
                    B("scalar", lambda e, psW=psW: e.activation(out=m["nwT"][:], in_=psW[:, 0:128], func=AF.Copy, scale=-1.0), [prW, "nwT"], ["nwT"])
                    psV, prV = pst()
                    MM(tail, psV[:, 0:128], prV, [(TTp, m["vb"][:]), (m["nwT"][:], Sst[:])], [TTr, "vb", "nwT", "S"])
                    B("vector", lambda e, psV=psV: e.tensor_copy(out=m["vnew"][:], in_=psV[:, 0:128]), [prV, "vnew"], ["vnew"])
                    psA, prA = pst()
                    MM(tail, psA[:, 0:128], prA, [(qT[:, cs], Sst[:])], [("qT", i // 4), "S"])
                    B("scalar", lambda e, psA=psA, a=colh(egc): e.activation(out=m["tmpo"][:], in_=psA[:, 0:128], func=AF.Copy, scale=a), [prA, "tmpo"] + SC, ["tmpo"])
                    psB, prB = pst()
                    MM(tail, psB[:, 0:128], prB, [(ATp, m["vnew"][:])], [ATr, "vnew"])
                    B("vector", lambda e, psB=psB: e.tensor_tensor(out=otm[:, i, :], in0=psB[:, 0:128], in1=m["tmpo"][:], op=ALU.add), [prB, "tmpo"], [("otm", i), "cf"])
                    psS, prS = pst()
                    MM(tail, psS[:, 0:128], prS, [(m["kend"][:], m["vnew"][:])], ["kend", "vnew"])
                    B("vector", lambda e, psS=psS, a=colh(eglast): e.scalar_tensor_tensor(out=Sst[:], in0=Sst[:], scalar=a, in1=psS[:, 0:128], op0=ALU.mult, op1=ALU.add),
                      [prS, "S"] + SC, ["S"])
                    return prep, tail

                def emit_merged(streams):
                    n = max(len(s_) for s_ in streams) if streams else 0
                    for k in range(n):
                        for s_ in streams:
                            if k < len(s_):
                                eng, fn, reads, writes = s_[k]
                                P.op(eng, fn, reads=reads, writes=writes)
                pending_tail = []
                for pr_ in range(NT // 2):
                    par = pr_ % 2
                    p0, t0 = make_ops(2 * pr_, 0, par, (0, 1, 2))
                    p1, t1 = make_ops(2 * pr_ + 1, 1, par, (3, 4, 5))
                    emit_merged([p0, p1, pending_tail])
                    pending_tail = t0 + t1
                emit_merged([pending_tail])

                wz, wzr = self.wload(self.wview(W, 0, 8, 3072 + h * 128, 128), 8, 128)
                for tb in range(NB):
                    psT, prT = self.psum()
                    for j in range(4):
                        t = tb * 4 + j
                        psz, przr = self.psum()
                        self.mm(psz[:, 0:128], przr, [(xb[:, k, t * 128:(t + 1) * 128], wz[:, k, :]) for k in range(KC)], reads=[wzr] + self.xb_res(tb))
                        P.op("scalar", lambda e, psz=psz: e.activation(out=mats["sz"][:], in_=psz[:, 0:128], func=AF.Silu), reads=[przr, "sz"], writes=["sz"])
                        sm = self.small[:, self.rot("small", 8), :]
                        sres = ("small", (self._rot_small - 1) % 8)
                        P.op("vector", lambda e, t=t, sm=sm: e.scalar_tensor_tensor(out=mats["on"][:], in0=otm[:, t, :], scalar=1.0, in1=otm[:, t, :], op0=ALU.mult, op1=ALU.mult, accum_out=sm[:, 0:1]),
                             reads=[("otm", t), "cf", "on", sres], writes=["on", sres])
                        P.op("scalar", lambda e, sm=sm: e.activation(out=sm[:, 1:2], in_=sm[:, 0:1], func=AF.Sqrt, bias=1e-6, scale=1.0 / 128.0), reads=[sres], writes=[sres])
                        P.op("vector", lambda e, sm=sm: e.reciprocal(out=sm[:, 2:3], in_=sm[:, 1:2]), reads=[sres], writes=[sres])
                        P.op("vector", lambda e, t=t, sm=sm: e.scalar_tensor_tensor(out=mats["on"][:], in0=otm[:, t, :], scalar=sm[:, 2:3], in1=hb[:, 16:144], op0=ALU.mult, op1=ALU.mult),
                             reads=[("otm", t), "cf", sres, "hb", "on"], writes=["on"])
                        P.op("vector", lambda e: e.tensor_tensor(out=mats["on"][:], in0=mats["on"][:], in1=mats["sz"][:], op=ALU.mult), reads=["on", "sz"], writes=["on"])
                        P.op("tensor", lambda e, psT=psT, j=j: e.transpose(out=psT[:, j * 128:(j + 1) * 128], in_=mats["on"][:], identity=ident), reads=["on", "cm"], writes=[prT])
                    P.op("scalar", lambda e, psT=psT, tb=tb: e.copy(out=ohT[:, tb * 512:(tb + 1) * 512], in_=psT[:]), reads=[prT], writes=[("ohT", tb)])
                self.out_proj_acc(ohT, lambda t: [("ohT", t // 4)], Wo, h, first=(h == 0))
            self.n_slots = 4
        P.barrier()


def host_layout(inputs, b):
    m = {}
    m["x"] = np.ascontiguousarray(inputs["x"][b])
    m["p"] = np.ascontiguousarray(inputs["p"][:, b])
    for n in INPUT_SHAPES:
        if n not in ("x", "p"):
            m[n] = np.ascontiguousarray(inputs[n], dtype=np.float32)
    bc = np.zeros((128, BC_W), np.float32)
    for n, (o, w) in BC_LAYOUT.items():
        if n == "moe_w_routerT":
            v = np.transpose(np.asarray(inputs["moe_w_router"]), (0, 2, 1)).reshape(-1)
        else:
            v = np.asarray(inputs[n]).reshape(-1)
        bc[:, o:o + w] = v[None, :]
    m["bc"] = bc
    col = np.zeros((128, COL_W), np.float32)

    def put(n, arr):
        o, w = COL_LAYOUT[n]
        col[:, o:o + w] = arr.reshape(128, w)
    put("lru_conv_w", np.asarray(inputs["lru_conv_w"])[0].reshape(4, 10, 128).transpose(2, 1, 0))
    for n in ("lru_conv_b", "lru_b_a", "lru_b_x", "lru_lambda"):
        put(n, np.asarray(inputs[n])[0].reshape(10, 128).T)
    put("cv_b_in", np.asarray(inputs["cv_b_in"])[0].reshape(16, 128).T)
    put("cv_dw_w", np.asarray(inputs["cv_dw_w"])[0].reshape(31, 8, 128).transpose(2, 1, 0))
    for n in ("cv_dw_b", "cv_ln_g", "cv_ln_b"):
        put(n, np.asarray(inputs[n])[0].reshape(8, 128).T)
    put("gdn_conv_w", np.asarray(inputs["gdn_conv_w"])[0].reshape(4, 24, 128).transpose(2, 1, 0))
    m["col"] = col
    m["cm"] = host_consts()
    return m


_NC_CACHE = {}


def run(inputs, n_layers=DEPTH, mixers=(0, 1, 2, 3), ffns=(0, 1, 0, 1), cores=8):
    key = (n_layers, tuple(mixers), tuple(ffns))
    if key not in _NC_CACHE:
        _NC_CACHE[key] = KB(n_layers, mixers, ffns).build()
    nc = _NC_CACHE[key]
    in_maps = [host_layout(inputs, b) for b in range(cores)]
    if os.environ.get("KB_TRACE"):
        res = run_bass_kernel_spmd(nc, in_maps, core_ids=list(range(cores)), trace=True)
        print("EXEC_NS", res.exec_time_ns, flush=True)
    else:
        res = run_bass_kernel_spmd(nc, in_maps, core_ids=list(range(cores)))
    return np.stack([r["y"] for r in res.results], axis=0)


def kernel(**inputs):
    inputs = {k: np.asarray(v) for k, v in inputs.items()}
    return run(inputs).astype(np.float32)
```
